# Optimizing a Trainium2 kernel written in Bass

```python
import jax, jax.numpy as jnp
from jax import lax
import numpy as np

D_MODEL = 2048
BATCH = 2
SEQ = 8192
DEPTH = 1
DEC_BATCH = 32
DEC_SEQ = 64
PAST_LEN = 2048

CHUNK = 64
N_META = 16
Q_BLOCK = 128
SB_HEADS = 8
SB_HEAD_DIM = 128
SB_WIDTH = SB_HEADS * SB_HEAD_DIM
RET_HEADS = 8
RET_DK = 128
RET_DV = 256
RET_QK_WIDTH = RET_HEADS * RET_DK
RET_V_WIDTH = RET_HEADS * RET_DV
IN_WIDTH = 3 * SB_WIDTH + 2 * RET_QK_WIDTH + 2 * RET_V_WIDTH + 2 * D_MODEL
N_GROUPS = 4
EXPERTS_PER_GROUP = 4
N_EXPERTS = N_GROUPS * EXPERTS_PER_GROUP
TOP_K_IN_GROUP = 2
D_EXPERT = 512
ROPE_BASE = 10000.0
EPS = 1e-6
F32 = jnp.float32

kernel_name = "stickbreak_retention_hmoe_stream_step"


def _rmsnorm(x, g):
    xf = x.astype(F32)
    xf = xf * lax.rsqrt(jnp.mean(xf * xf, axis=-1, keepdims=True) + EPS)
    return xf.astype(x.dtype) * g


def _rotary(x, pos):
    d = x.shape[-1]
    inv_freq = 1.0 / (ROPE_BASE ** (jnp.arange(0, d, 2, dtype=F32) / d))
    ang = pos[:, None] * inv_freq[None, :]
    cos = jnp.cos(ang)[None, :, None, :]
    sin = jnp.sin(ang)[None, :, None, :]
    xf = x.astype(F32)
    x1, x2 = xf[..., : d // 2], xf[..., d // 2:]
    return jnp.concatenate([x1 * cos - x2 * sin, x1 * sin + x2 * cos], axis=-1).astype(x.dtype)


def _mixer_inputs(u, w_in, pos):
    b, t, _ = u.shape
    sizes = (SB_WIDTH, SB_WIDTH, SB_WIDTH, RET_QK_WIDTH, RET_QK_WIDTH, RET_V_WIDTH, RET_V_WIDTH, D_MODEL, D_MODEL)
    cuts = [int(c) for c in np.cumsum(sizes)[:-1]]
    sb_q, sb_k, sb_v, r_q, r_k, r_v, r_g, g_sb, g_ret = jnp.split(u @ w_in, cuts, axis=-1)
    sb_q = sb_q.reshape(b, t, SB_HEADS, SB_HEAD_DIM)
    sb_k = sb_k.reshape(b, t, SB_HEADS, SB_HEAD_DIM)
    sb_v = sb_v.reshape(b, t, SB_HEADS, SB_HEAD_DIM)
    r_q = _rotary(r_q.reshape(b, t, RET_HEADS, RET_DK), pos)
    r_k = _rotary(r_k.reshape(b, t, RET_HEADS, RET_DK), pos) * (RET_DK ** -0.5)
    r_v = r_v.reshape(b, t, RET_HEADS, RET_DV)
    return sb_q, sb_k, sb_v, r_q, r_k, r_v, r_g, g_sb, g_ret


def _sb_block(q, q_pos, k, v, k_pos):
    z = jnp.einsum("bqhd,bkhd->bhqk", q, k, preferred_element_type=F32) * (SB_HEAD_DIM ** -0.5)
    valid = (k_pos[None, :] < q_pos[:, None])[None, None]
    log_fail = jnp.where(valid, jax.nn.log_sigmoid(-z), 0.0)
    later = lax.cumsum(log_fail, axis=3, reverse=True) - log_fail
    a = jnp.where(valid, jnp.exp(jax.nn.log_sigmoid(z) + later), 0.0)
    return jnp.einsum("bhqk,bkhd->bqhd", a.astype(v.dtype), v)


def _stick_breaking(q, k, v, q_offset):
    n = q.shape[1]
    outs = []
    for start in range(0, n, Q_BLOCK):
        stop = min(start + Q_BLOCK, n)
        kend = q_offset + stop
        q_pos = q_offset + jnp.arange(start, stop)
        outs.append(_sb_block(q[:, start:stop], q_pos, k[:, :kend], v[:, :kend], jnp.arange(kend)))
    return jnp.concatenate(outs, axis=1)


def _ret_log_decay():
    return jnp.log1p(-jnp.power(2.0, -5.0 - jnp.arange(RET_HEADS, dtype=F32)))


def _retention_chunk(q, k, v, s):
    n = q.shape[1]
    log_g = _ret_log_decay()
    t = jnp.arange(n, dtype=F32)
    rel = t[:, None] - t[None, :]
    dmask = jnp.where(rel[None] >= 0, jnp.exp(jnp.maximum(rel, 0.0)[None] * log_g[:, None, None]), 0.0)
    scores = jnp.einsum("bthd,bshd->bhts", q, k) * dmask[None]
    o_inner = jnp.einsum("bhts,bshv->bthv", scores, v)
    o_cross = jnp.einsum("bthd,bhdv->bthv", q, s) * jnp.exp((t + 1.0)[:, None] * log_g[None, :])[None, :, :, None]
    k_dec = k * jnp.exp((n - 1.0 - t)[:, None] * log_g[None, :])[None, :, :, None]
    s_new = jnp.exp(n * log_g)[None, :, None, None] * s + jnp.einsum("bshd,bshv->bhdv", k_dec, v)
    return o_inner + o_cross, s_new


def _retention_from_start(q, k, v):
    b, t, h, dk = q.shape
    pad = (-t) % CHUNK
    nc = (t + pad) // CHUNK

    def to_chunks(a):
        a = jnp.pad(a.astype(F32), ((0, 0), (pad, 0), (0, 0), (0, 0)))
        return jnp.moveaxis(a.reshape(b, nc, CHUNK, h, a.shape[-1]), 1, 0)

    def step(s, qkv):
        o, s = _retention_chunk(qkv[0], qkv[1], qkv[2], s)
        return s, o

    s_fin, o = lax.scan(step, jnp.zeros((b, h, dk, RET_DV), F32), (to_chunks(q), to_chunks(k), to_chunks(v)))
    o = jnp.moveaxis(o, 0, 1).reshape(b, nc * CHUNK, h, RET_DV)[:, pad:]
    return o, s_fin


def _branch_merge(o_sb, o_ret, r_g, g_sb, g_ret, w_sb_o, w_ret_o, w_out):
    b, t = o_sb.shape[:2]
    o_ret = o_ret * lax.rsqrt(jnp.mean(o_ret * o_ret, axis=-1, keepdims=True) + EPS)
    o_ret = o_ret.reshape(b, t, RET_V_WIDTH).astype(r_g.dtype) * jax.nn.silu(r_g)
    sb_branch = o_sb.reshape(b, t, SB_WIDTH) @ w_sb_o
    ret_branch = o_ret @ w_ret_o
    merged = jax.nn.sigmoid(g_sb) * sb_branch + jax.nn.sigmoid(g_ret) * ret_branch
    return merged @ w_out


def _hier_moe(u, w_grp, b_grp, w_exp, b_exp, w_gate, w_up, w_down):
    shp = u.shape
    x = u.reshape(-1, D_MODEL)
    xf = x.astype(F32)
    g_prob = jax.nn.softmax(xf @ w_grp.astype(F32) + b_grp.astype(F32), axis=-1)
    g_top, g_idx = lax.top_k(g_prob, 1)
    e_logits = (xf @ w_exp.astype(F32) + b_exp.astype(F32)).reshape(-1, N_GROUPS, EXPERTS_PER_GROUP)
    e_in_group = jnp.take_along_axis(e_logits, g_idx[:, :, None], axis=1)[:, 0]
    e_top, e_idx = lax.top_k(e_in_group, TOP_K_IN_GROUP)
    e_w = jax.nn.softmax(e_top, axis=-1) * g_top
    expert_id = g_idx * EXPERTS_PER_GROUP + e_idx
    combine = jnp.sum(jax.nn.one_hot(expert_id, N_EXPERTS, dtype=F32) * e_w[..., None], axis=1).astype(x.dtype)
    y = jnp.zeros_like(x)
    for e in range(N_EXPERTS):
        hid = jax.nn.silu(x @ w_gate[e]) * (x @ w_up[e])
        y = y + combine[:, e:e + 1] * (hid @ w_down[e])
    return y.reshape(shp)


def _layer(h, pos, past_k, past_v, past_state, norm1_g, w_in, w_sb_o, w_ret_o, w_out, norm2_g,
           w_grp, b_grp, w_exp, b_exp, w_gate, w_up, w_down):
    u = _rmsnorm(h, norm1_g)
    sb_q, sb_k, sb_v, r_q, r_k, r_v, r_g, g_sb, g_ret = _mixer_inputs(u, w_in, pos)
    if past_k is None:
        o_sb = _stick_breaking(sb_q, sb_k, sb_v, 0)
        o_ret, new_state = _retention_from_start(r_q, r_k, r_v)
    else:
        past = past_k.shape[1]
        k_all = jnp.concatenate([past_k.astype(sb_k.dtype), sb_k], axis=1)
        v_all = jnp.concatenate([past_v.astype(sb_v.dtype), sb_v], axis=1)
        o_sb = _stick_breaking(sb_q, k_all, v_all, past)
        o_ret, new_state = _retention_chunk(r_q.astype(F32), r_k.astype(F32), r_v.astype(F32), past_state.astype(F32))
    h = h + _branch_merge(o_sb, o_ret, r_g, g_sb, g_ret, w_sb_o, w_ret_o, w_out)
    h = h + _hier_moe(_rmsnorm(h, norm2_g), w_grp, b_grp, w_exp, b_exp, w_gate, w_up, w_down)
    return h, sb_k, sb_v, new_state.astype(h.dtype)


def setup_inputs(seed: int = 0) -> dict:
    key = jax.random.key(seed)
    ks = jax.random.split(key, 24)

    def nrm(k, shape, scale):
        return jax.random.normal(k, shape, F32) * scale

    return {
        "x_prompt": nrm(ks[0], (BATCH, SEQ, D_MODEL), 1.0),
        "x_sample": nrm(ks[1], (DEC_BATCH, DEC_SEQ, D_MODEL), 1.0),
        "cache_sb_k": nrm(ks[2], (DEPTH, DEC_BATCH, PAST_LEN, SB_HEADS, SB_HEAD_DIM), 1.0),
        "cache_sb_v": nrm(ks[3], (DEPTH, DEC_BATCH, PAST_LEN, SB_HEADS, SB_HEAD_DIM), 1.0),
        "state_ret": nrm(ks[4], (DEPTH, DEC_BATCH, RET_HEADS, RET_DK, RET_DV), 1.0),
        "meta": nrm(ks[5], (N_META, D_MODEL), 1.0),
        "norm1_g": 1.0 + nrm(ks[6], (DEPTH, D_MODEL), 0.02),
        "w_in": nrm(ks[7], (DEPTH, D_MODEL, IN_WIDTH), D_MODEL ** -0.5),
        "w_sb_o": nrm(ks[8], (DEPTH, SB_WIDTH, D_MODEL), SB_WIDTH ** -0.5),
        "w_ret_o": nrm(ks[9], (DEPTH, RET_V_WIDTH, D_MODEL), RET_V_WIDTH ** -0.5),
        "w_out": nrm(ks[10], (DEPTH, D_MODEL, D_MODEL), D_MODEL ** -0.5),
        "norm2_g": 1.0 + nrm(ks[11], (DEPTH, D_MODEL), 0.02),
        "w_grp": nrm(ks[12], (DEPTH, D_MODEL, N_GROUPS), D_MODEL ** -0.5),
        "b_grp": nrm(ks[13], (DEPTH, N_GROUPS), 0.01),
        "w_exp": nrm(ks[14], (DEPTH, D_MODEL, N_EXPERTS), D_MODEL ** -0.5),
        "b_exp": nrm(ks[15], (DEPTH, N_EXPERTS), 0.01),
        "w_gate": nrm(ks[16], (DEPTH, N_EXPERTS, D_MODEL, D_EXPERT), D_MODEL ** -0.5),
        "w_up": nrm(ks[17], (DEPTH, N_EXPERTS, D_MODEL, D_EXPERT), D_MODEL ** -0.5),
        "w_down": nrm(ks[18], (DEPTH, N_EXPERTS, D_EXPERT, D_MODEL), D_EXPERT ** -0.5),
        "normf_g": 1.0 + nrm(ks[19], (D_MODEL,), 0.02),
    }


def reference(x_prompt, x_sample, cache_sb_k, cache_sb_v, state_ret, meta, norm1_g, w_in, w_sb_o, w_ret_o,
              w_out, norm2_g, w_grp, b_grp, w_exp, b_exp, w_gate, w_up, w_down, normf_g):
    b = x_prompt.shape[0]
    h_p = jnp.concatenate([jnp.broadcast_to(meta[None].astype(x_prompt.dtype), (b, N_META, D_MODEL)), x_prompt], axis=1)
    pos_p = jnp.arange(h_p.shape[1], dtype=F32) - N_META
    past = cache_sb_k.shape[2]
    h_s = x_sample
    pos_s = past + jnp.arange(x_sample.shape[1], dtype=F32)
    kp, vp, sp, ksm, vsm, ssm = [], [], [], [], [], []
    for i in range(DEPTH):
        lw = (norm1_g[i], w_in[i], w_sb_o[i], w_ret_o[i], w_out[i], norm2_g[i],
              w_grp[i], b_grp[i], w_exp[i], b_exp[i], w_gate[i], w_up[i], w_down[i])
        h_p, k_new, v_new, s_new = _layer(h_p, pos_p, None, None, None, *lw)
        kp.append(k_new)
        vp.append(v_new)
        sp.append(s_new)
        h_s, k_new, v_new, s_new = _layer(h_s, pos_s, cache_sb_k[i], cache_sb_v[i], state_ret[i], *lw)
        ksm.append(k_new)
        vsm.append(v_new)
        ssm.append(s_new)
    y_prompt = _rmsnorm(h_p, normf_g)[:, N_META:]
    y_sample = _rmsnorm(h_s, normf_g)
    return (y_prompt, y_sample, jnp.stack(kp, 0), jnp.stack(vp, 0), jnp.stack(sp, 0),
            jnp.stack(ksm, 0), jnp.stack(vsm, 0), jnp.stack(ssm, 0))
```

```python
import numpy as np
import concourse.bass as bass
import concourse.mybir as mybir
from concourse.bass_utils import run_bass_kernel_spmd

F32 = mybir.dt.float32
BF16 = mybir.dt.bfloat16
AF = mybir.ActivationFunctionType
ALU = mybir.AluOpType

D = 2048
SEQ = 8192
NMETA = 16
TP = SEQ + NMETA
DEC_B = 32
DEC_T = 64
PAST = 2048
NSC = 4
EPS = 1e-6
NEG = -30000.0
SEM_LIMIT = 30000
WKV = 640


class Buf:
    __slots__ = ("name", "w", "r", "dsem", "dcnt", "excl")

    def __init__(self, name, excl=False):
        self.name = name
        self.excl = excl
        self.w = None
        self.r = {}
        self.dsem = None
        self.dcnt = 0


class Ctx:
    def __init__(self, nc):
        self.nc = nc
        self.eng = {"pe": nc.tensor, "act": nc.scalar, "dve": nc.vector,
                    "pool": nc.gpsimd, "sp": nc.sync}
        self.sem = {}
        self.cnt = {}
        self.waited = {}
        self.pend_r = {}
        self.pend_w = {}
        self.nsem = 0
        for k in self.eng:
            self.sem[k] = self._newsem("e_" + k)
            self.cnt[k] = 0
            self.waited[k] = {}
            self.pend_r[k] = []
            self.pend_w[k] = []
        self.out_tokens = {}
        self.dtoks = {}
        self.ninst = 0

    def _newsem(self, name):
        self.nsem += 1
        return self.nc.alloc_semaphore(f"{name}_{self.nsem}")

    def _wait(self, en, tok):
        if tok is None:
            return
        sem, val = tok
        w = self.waited[en]
        if w.get(sem.num, 0) >= val:
            return
        self.eng[en].wait_ge(sem, val)
        w[sem.num] = val

    def _deps(self, en, reads, writes):
        skip = self.sem[en].num if en == "pe" else None
        for b in reads:
            if b.w is not None and b.w[0].num != skip:
                self._wait(en, b.w)
        for b in writes:
            if b.w is not None and b.w[0].num != skip:
                self._wait(en, b.w)
            for t in b.r.values():
                if t[0].num != skip:
                    self._wait(en, t)

    def _commit(self, tok, reads, writes):
        sem, val = tok
        for b in reads:
            b.r[sem.num] = tok
        for b in writes:
            b.w = tok
            b.r = {}

    def op(self, en, fn, reads=(), writes=(), sig=True):
        ex = [b for b in reads if b.excl]
        if ex:
            reads = [b for b in reads if not b.excl]
            writes = list(writes) + ex
        self._deps(en, reads, writes)
        ins = fn(self.eng[en])
        self.ninst += 1
        if not sig:
            self.pend_r[en].extend(reads)
            self.pend_w[en].extend(writes)
            return None
        if self.cnt[en] >= SEM_LIMIT:
            self.sem[en] = self._newsem("e_" + en)
            self.cnt[en] = 0
        self.cnt[en] += 1
        ins.then_inc(self.sem[en], 1)
        tok = (self.sem[en], self.cnt[en])
        self._commit(tok, list(reads) + self.pend_r[en], list(writes) + self.pend_w[en])
        self.pend_r[en] = []
        self.pend_w[en] = []
        return tok

    def dma(self, q, out, in_, reads=(), writes=(), owner=None, is_output=False):
        self._deps(q, reads, writes)
        ins = self.eng[q].dma_start(out=out, in_=in_)
        self.ninst += 1
        if owner.dsem is None or owner.dcnt >= SEM_LIMIT:
            owner.dsem = self._newsem("d_" + owner.name)
            owner.dcnt = 0
        owner.dcnt += 16
        ins.then_inc(owner.dsem, 16)
        tok = (owner.dsem, owner.dcnt)
        self.dtoks[owner.dsem.num] = tok
        self._commit(tok, reads, writes)
        if is_output:
            self.out_tokens[owner.dsem.num] = tok
        return tok

    def barrier(self):
        toks = [(self.sem[k], self.cnt[k]) for k in self.eng if self.cnt[k] > 0] + list(self.dtoks.values())
        for en in self.eng:
            for t in toks:
                if t[0] is self.sem[en]:
                    continue
                self._wait(en, t)

    def finish(self, en="sp"):
        for tok in self.out_tokens.values():
            self._wait(en, tok)


AX = mybir.AxisListType
WALL = 1152
GT = 768


class Ring:
    def __init__(self, aps, name):
        self.t = list(aps)
        self.b = [Buf(f"{name}{i}") for i in range(len(aps))]
        self.i = 0

    def next(self):
        t, b = self.t[self.i], self.b[self.i]
        self.i = (self.i + 1) % len(self.t)
        return t, b


class _Pool:
    def __init__(self, t, size):
        self.t, self.size, self.off = t, size, 0


class Arena:
    def __init__(self, pool, f32):
        self.p, self.f32 = pool, f32

    def reset(self):
        self.p.off = 0

    def get(self, n, b=None):
        p = self.p
        if self.f32:
            p.off += p.off % 2
            a = p.t[:, p.off:p.off + 2 * n].bitcast(F32)
            p.off += 2 * n
        else:
            a = p.t[:, p.off:p.off + n]
            p.off += n
        assert p.off <= p.size, (p.off, p.size)
        if b is not None:
            a = a.rearrange("p (a b) -> p a b", b=b)
        return a


def build(nc, NXT=64, NH=8, NS=NSC, dbg=False):
    c = Ctx(nc)
    dt = nc.dram_tensor
    NL = NXT // 4
    NTOK = NL * 128 + NS * DEC_T
    assert NTOK % 128 == 0
    ntp = NMETA + NXT * 128
    NBLK = max(1 + NXT, 17)
    I = "ExternalInput"
    xall = dt("xall", [NXT * 128, D], F32, kind=I).ap()
    meta = dt("meta", [NMETA, D], F32, kind=I).ap()
    xtok = dt("xtok", [NTOK, D], F32, kind=I).ap()
    g1 = dt("g1", [D], F32, kind=I).ap()
    g2 = dt("g2", [D], F32, kind=I).ap()
    gf = dt("gf", [D], F32, kind=I).ap()
    wh = dt("wh", [NH, D, WALL], F32, kind=I).ap()
    st = dt("st", [NS, NH, 128, 256], F32, kind=I).ap()
    cstf = dt("cstf", [NH, 128, 262], F32, kind=I).ap()
    cstb = dt("cstb", [128, 512], F32, kind=I).ap()
    ropec = dt("ropec", [ntp + DEC_T, 128], F32, kind=I).ap()
    ropes = dt("ropes", [ntp + DEC_T, 128], F32, kind=I).ap()
    ropesmc = dt("ropesmc", [DEC_T, 256], F32, kind=I).ap()
    ropesms = dt("ropesms", [DEC_T, 256], F32, kind=I).ap()
    ropeoc = dt("ropeoc", [NL * 128, 256], F32, kind=I).ap()
    ropeos = dt("ropeos", [NL * 128, 256], F32, kind=I).ap()
    msk = dt("msk", [128, 512], F32, kind=I).ap()
    sel = dt("sel", [128, 4], F32, kind=I).ap()
    ck = dt("ck", [NS, PAST, NH * 128], F32, kind=I).ap()
    cv = dt("cv", [NS, PAST, NH * 128], F32, kind=I).ap()
    wg = dt("wg", [D, 2 * D], F32, kind=I).ap()
    wsbo = dt("wsbo", [NH * 128, D], F32, kind=I).ap()
    wreto = dt("wreto", [NH * 256, D], F32, kind=I).ap()
    wout = dt("wout", [D, D], F32, kind=I).ap()
    wr = dt("wr", [D, 20], F32, kind=I).ap()
    br = dt("br", [20], F32, kind=I).ap()
    wgate = dt("wgate", [16, D, 512], F32, kind=I).ap()
    wup = dt("wup", [16, D, 512], F32, kind=I).ap()
    wdown = dt("wdown", [16, 512, D], F32, kind=I).ap()

    O = "ExternalOutput"
    kp = dt("kp", [ntp, NH * 128], F32, kind=O).ap()
    vp = dt("vp", [ntp, NH * 128], F32, kind=O).ap()
    sp_o = dt("sp_o", [NH, 128, 256], F32, kind=O).ap()
    ks = dt("ks", [NS, DEC_T, NH * 128], F32, kind=O).ap()
    vs = dt("vs", [NS, DEC_T, NH * 128], F32, kind=O).ap()
    ss_o = dt("ss_o", [NS, NH, 128, 256], F32, kind=O).ap()
    yo = dt("yo", [NTOK, D], F32, kind=O).ap()

    SK = O if dbg else "Internal"
    NTL = 1 + NXT + NS
    uts = dt("uts", [NTL, 128, 16 * 128], BF16, kind="Internal").ap()
    uto = dt("uto", [128, 16 * NTOK], BF16, kind="Internal").ap().rearrange("p (k t) -> p k t", t=NTOK)
    osbT = dt("osbT", [NH, 128, NTOK], BF16, kind=SK).ap()
    oretT = dt("oretT", [2 * NH, 128, NTOK], BF16, kind=SK).ap()
    hdbg = dt("hdbg", [NTOK, D], F32, kind=O).ap() if dbg else None
    b_uts = Buf("uts")
    b_osb = Buf("osbT")
    b_oret = Buf("oretT")

    NPOOL = 95000
    pool_ = _Pool(nc.alloc_sbuf_tensor("arena", [128, NPOOL], BF16), NPOOL)
    AFa, ABa = Arena(pool_, True), Arena(pool_, False)

    pT = nc.alloc_psum_tensor("pT", [128, 16, 128], BF16); bpT = Buf("pT", True)
    pA = nc.alloc_psum_tensor("pA", [128, 512], F32); bpA = Buf("pA", True)
    pB = nc.alloc_psum_tensor("pB", [128, 512], F32); bpB = Buf("pB", True)
    pC = nc.alloc_psum_tensor("pC", [128, 512], F32); bpC = Buf("pC", True)
    p6 = nc.alloc_psum_tensor("p6", [128, 512], F32); bp6 = Buf("p6", True)
    p7 = nc.alloc_psum_tensor("p7", [128, 512], F32); bp7 = Buf("p7", True)
    pR = nc.alloc_psum_tensor("pR", [128, 8, 128], BF16); bpR = Buf("pR", True)

    def ring(ar, n, cnt, name, b=None):
        return Ring([ar.get(n, b) for _ in range(cnt)], name)

    CB = ABa.get(512); bCB = Buf("CB")
    gbc = AFa.get(D); bg = Buf("gbc")
    c.dma("sp", gbc, g1.partition_broadcast(128), writes=[bg], owner=bg)
    c.dma("pool", CB, cstb, writes=[bCB], owner=bCB)
    MK = ABa.get(512, 128); bMK = Buf("MK")
    c.dma("pool", MK, msk.rearrange("p (r q) -> p r q", q=128), writes=[bMK], owner=bMK)
    SEL = AFa.get(4); bSEL = Buf("SEL")
    c.dma("sp", SEL, sel, writes=[bSEL], owner=bSEL)
    IDN = CB[:, 0:128]
    NEGM = CB[:, 128:256]
    TRIN = CB[:, 256:384]
    ONESN = CB[:, 384:512]
    NTI = {128: 0, 64: 1, 16: 2}

    xr = ring(AFa, D, 2, "xt")
    st_r = ring(AFa, 4, 2, "ss")
    ur = ring(ABa, D, 2, "u")
    uTr = ring(ABa, 2048, 2, "uT", 128)
    ccr = ring(AFa, 256, 2, "cc")
    ssr = ring(AFa, 256, 2, "sn")
    kvr = ring(AFa, 256, 3, "kvst")
    rfr = ring(AFa, 256, 2, "rf")
    tmr = ring(AFa, 256, 2, "tm")
    swr = ring(AFa, 256, 2, "sw")
    sgr = ring(AFa, 256, 2, "sg")
    vbr = ring(ABa, 256, 2, "vbf")
    kdr = ring(ABa, 128, 2, "kdec")
    k16r = ring(ABa, 128, 2, "k16")
    qkr = ring(ABa, 384, 2, "qk16")
    trr = ring(ABa, 256, 2, "trT", 128)
    scr = ring(ABa, 128, 2, "scm")
    qdr = ring(ABa, 128, 2, "qdec")
    ogr = ring(ABa, 256, 2, "og")
    ogTr = ring(ABa, 256, 2, "ogT", 128)
    Wh = ABa.get(16 * WALL, WALL); bWh = Buf("Wh")
    CF = AFa.get(262); bCF = Buf("CF")
    S = AFa.get(256); bS = Buf("S")
    Sb = ABa.get(256); bSb = Buf("Sb")
    Ssel = ABa.get(max(NL, 1) * 256, 256); bSsel = Buf("Ssel")
    KT = ABa.get(NBLK * 128)
    VA = ABa.get(NBLK * 128, 128)
    QT = ABa.get(max(NL, 1) * 128)
    bKT = [Buf(f"KT{i}") for i in range(NBLK)]
    bVA = [Buf(f"VA{i}") for i in range(NBLK)]
    bQT = [Buf(f"QT{i}") for i in range(max(NL, 1))]
    Er = ring(AFa, 512, 2, "E")
    SPr = ring(ABa, 512, 2, "SP")
    ATr = ring(ABa, 512, 2, "AT")
    Sacc = AFa.get(512); bSacc = Buf("Sacc")
    Saccb = ABa.get(512); bSaccb = Buf("Saccb")
    osr = ring(ABa, 512, 2, "oso")
    ckst = ABa.get(2048, 128); bckst = Buf("ckst")

    def norm_tile(xsrc, nt, dsts):
        xt, bx = xr.next()
        c.dma("sp", xt[:nt, :], xsrc, writes=[bx], owner=bx)
        stt, bst = st_r.next()
        u, bu = ur.next()
        c.op("pool", lambda e: e.memset(stt[:, :], 0.0), writes=[bst])
        c.op("act", lambda e: e.activation(out=u[:nt, :], in_=xt[:nt, :], func=AF.Square,
                                           accum_out=stt[:nt, 0:1]), reads=[bx], writes=[bu, bst])
        c.op("act", lambda e: e.activation(out=stt[:nt, 1:2], in_=stt[:nt, 0:1], func=AF.Ln,
                                           scale=1.0 / D, bias=EPS), reads=[bst], writes=[bst])
        c.op("act", lambda e: e.activation(out=stt[:nt, 1:2], in_=stt[:nt, 1:2], func=AF.Exp,
                                           scale=-0.5), reads=[bst], writes=[bst])
        c.op("dve", lambda e: e.scalar_tensor_tensor(out=u[:nt, :], in0=xt[:nt, :], scalar=stt[:nt, 1:2],
                                                     in1=gbc[:nt, :], op0=ALU.mult, op1=ALU.mult),
             reads=[bx, bst, bg], writes=[bu])
        for j in range(16):
            c.op("pe", lambda e, j=j: e.transpose(out=pT[:, j, :nt], in_=u[:nt, j * 128:(j + 1) * 128],
                                                  identity=IDN[:nt, :nt]),
                 reads=[bu, bCB], writes=[bpT], sig=(j == 15))
        uT, buT = uTr.next()
        c.op("act", lambda e: e.copy(out=uT[:, :, :nt], in_=pT[:, :, :nt]), reads=[bpT], writes=[buT])
        for dst in dsts:
            c.dma("pool", dst, uT[:, :, :nt], reads=[buT], owner=b_uts)

    def uts_ap(idx, nt):
        return uts[idx].rearrange("p (k t) -> p k t", t=128)[:, :, :nt]

    norm_tile(meta, NMETA, [uts_ap(0, NMETA)])
    for i in range(NXT):
        norm_tile(xall[i * 128:(i + 1) * 128, :], 128, [uts_ap(1 + i, 128)])
    for l in range(NL):
        norm_tile(xtok[l * 128:(l + 1) * 128, :], 128, [uto[:, :, l * 128:(l + 1) * 128]])
    for s in range(NS):
        t0 = NL * 128 + s * DEC_T
        norm_tile(xtok[t0:t0 + DEC_T, :], DEC_T, [uts_ap(1 + NXT + s, DEC_T), uto[:, :, t0:t0 + DEC_T]])
    b_uts.w = (b_uts.dsem, b_uts.dcnt)

    def load_head(h):
        for k4 in range(4):
            c.dma("pool", Wh[:, 4 * k4:4 * k4 + 4, :],
                  wh[h, 512 * k4:512 * (k4 + 1), :].rearrange("(k p) n -> p k n", p=128),
                  writes=[bWh], owner=bWh)
        c.dma("sp", CF, cstf[h], writes=[bCF], owner=bCF)

    def scan_tile(idx, nt, rope_row, kout, vout, blk, sel_l=None, sel_r=None):
        uT, buT = uTr.next()
        c.dma("sp", uT[:, :, :nt], uts_ap(idx, nt), reads=[b_uts], writes=[buT], owner=buT)
        cc, bcc = ccr.next()
        sn, bsn = ssr.next()
        c.dma("sp", cc[:nt, 0:128], ropec[rope_row:rope_row + nt, :], writes=[bcc], owner=bcc)
        c.dma("sp", sn[:nt, 0:128], ropes[rope_row:rope_row + nt, :], writes=[bsn], owner=bsn)
        for (ps, bps, c0, cn) in ((pA, bpA, 0, 256), (pB, bpB, 512, 384)):
            for k in range(16):
                c.op("pe", lambda e, ps=ps, c0=c0, cn=cn, k=k: e.matmul(
                    ps[:nt, :cn], lhsT=uT[:, k, :nt], rhs=Wh[:, k, c0:c0 + cn],
                    start=(k == 0), stop=(k == 15)),
                    reads=[buT, bWh], writes=[bps], sig=(k == 15))
        kv, bkv = kvr.next()
        c.op("act", lambda e: e.copy(out=kv[:nt, 0:128], in_=pA[:nt, 0:128]), reads=[bpA], writes=[bkv])
        c.op("act", lambda e: e.copy(out=kv[:nt, 128:256], in_=pA[:nt, 128:256]), reads=[bpA], writes=[bkv])
        c.dma("pool", kout, kv[:nt, 0:128], reads=[bkv], owner=bkv, is_output=True)
        c.dma("pool", vout, kv[:nt, 128:256], reads=[bkv], owner=bkv, is_output=True)
        k16, bk16 = k16r.next()
        c.op("act", lambda e: e.copy(out=k16[:nt, :], in_=pA[:nt, 0:128]), reads=[bpA], writes=[bk16])
        c.op("dve", lambda e: e.tensor_copy(out=VA[:nt, blk, :], in_=pA[:nt, 128:256]), reads=[bpA], writes=[bVA[blk]])
        c.op("pe", lambda e: e.transpose(out=pR[:, 7, :nt], in_=k16[:nt, :], identity=IDN[:nt, :nt]),
             reads=[bk16, bCB], writes=[bpR])
        c.op("act", lambda e: e.mul(out=KT[:, blk * 128:blk * 128 + nt], in_=pR[:, 7, :nt], mul=128.0 ** -0.5),
             reads=[bpR], writes=[bKT[blk]])
        rf, brf = rfr.next()
        c.op("dve", lambda e: e.tensor_copy(out=rf[:nt, 0:128], in_=pB[:nt, 0:128]), reads=[bpB], writes=[brf])
        vb, bvb = vbr.next()
        c.op("act", lambda e: e.copy(out=vb[:nt, :], in_=pB[:nt, 128:384]), reads=[bpB], writes=[bvb])
        tm, btm = tmr.next()
        sw, bsw = swr.next()
        c.op("dve", lambda e: e.tensor_tensor(out=tm[:nt, 0:128], in0=rf[:nt, 0:128], in1=cc[:nt, 0:128], op=ALU.mult),
             reads=[brf, bcc], writes=[btm])
        c.op("pool", lambda e: e.tensor_tensor(out=sw[:nt, 0:64], in0=rf[:nt, 64:128], in1=sn[:nt, 0:64],
                                               op=ALU.mult), reads=[brf, bsn], writes=[bsw])
        c.op("pool", lambda e: e.tensor_tensor(out=sw[:nt, 64:128], in0=rf[:nt, 0:64], in1=sn[:nt, 64:128],
                                               op=ALU.mult), reads=[brf, bsn], writes=[bsw])
        c.op("dve", lambda e: e.tensor_tensor(out=tm[:nt, 0:128], in0=tm[:nt, 0:128], in1=sw[:nt, 0:128], op=ALU.add),
             reads=[btm, bsw], writes=[btm])
        kd, bkd = kdr.next()
        gk = CF[:nt, 256 + NTI[nt]:257 + NTI[nt]]
        c.op("dve", lambda e: e.tensor_scalar(out=kd[:nt, :], in0=tm[:nt, 0:128], scalar1=gk, scalar2=None,
                                              op0=ALU.mult), reads=[btm, bCF], writes=[bkd])
        if sel_l is not None:
            sc1 = SEL[:, sel_r:sel_r + 1]
            if sel_r == 0:
                c.op("dve", lambda e: e.tensor_scalar(out=Ssel[:, sel_l, :], in0=S, scalar1=sc1, scalar2=None,
                                                      op0=ALU.mult), reads=[bS, bSEL], writes=[bSsel])
            else:
                c.op("dve", lambda e: e.scalar_tensor_tensor(out=Ssel[:, sel_l, :], in0=S, scalar=sc1,
                                                             in1=Ssel[:, sel_l, :], op0=ALU.mult, op1=ALU.add),
                     reads=[bS, bSEL, bSsel], writes=[bSsel])
        c.op("pe", lambda e: e.matmul(p6[:, 0:256], lhsT=kd[:nt, :], rhs=vb[:nt, :], start=True, stop=True),
             reads=[bkd, bvb], writes=[bp6])
        gn = CF[:, 259 + NTI[nt]:260 + NTI[nt]]
        c.op("dve", lambda e: e.scalar_tensor_tensor(out=S, in0=S, scalar=gn, in1=p6[:, 0:256],
                                                     op0=ALU.mult, op1=ALU.add),
             reads=[bS, bCF, bp6], writes=[bS])

    def own_tile(h, usrc, nt, rc_ap, rs_ap, qdst, bqd, Sb_ap, bSb_, tok0):
        uT, buT = uTr.next()
        c.dma("sp", uT[:, :, :nt], usrc, reads=[b_uts], writes=[buT], owner=buT)
        cc, bcc = ccr.next()
        sn, bsn = ssr.next()
        c.dma("sp", cc[:nt, :], rc_ap, writes=[bcc], owner=bcc)
        c.dma("sp", sn[:nt, :], rs_ap, writes=[bsn], owner=bsn)
        for (ps, bps, c0, cn) in ((pA, bpA, 256, 384), (pB, bpB, 640, 512)):
            for k in range(16):
                c.op("pe", lambda e, ps=ps, c0=c0, cn=cn, k=k: e.matmul(
                    ps[:nt, :cn], lhsT=uT[:, k, :nt], rhs=Wh[:, k, c0:c0 + cn],
                    start=(k == 0), stop=(k == 15)),
                    reads=[buT, bWh], writes=[bps], sig=(k == 15))
        qk, bqk = qkr.next()
        c.op("act", lambda e: e.copy(out=qk[:nt, 0:128], in_=pA[:nt, 0:128]), reads=[bpA], writes=[bqk])
        rf, brf = rfr.next()
        c.op("dve", lambda e: e.tensor_copy(out=rf[:nt, :], in_=pA[:nt, 128:384]), reads=[bpA], writes=[brf])
        vb, bvb = vbr.next()
        c.op("act", lambda e: e.copy(out=vb[:nt, :], in_=pB[:nt, 0:256]), reads=[bpB], writes=[bvb])
        sg, bsg = sgr.next()
        c.op("act", lambda e: e.activation(out=sg[:nt, :], in_=pB[:nt, 256:512], func=AF.Exp, scale=-1.0),
             reads=[bpB], writes=[bsg])
        c.op("dve", lambda e: e.tensor_scalar(out=sg[:nt, :], in0=sg[:nt, :], scalar1=1.0, scalar2=None,
                                              op0=ALU.add), reads=[bsg], writes=[bsg])
        c.op("dve", lambda e: e.reciprocal(out=sg[:nt, :], in_=sg[:nt, :]), reads=[bsg], writes=[bsg])
        c.op("dve", lambda e: e.tensor_tensor(out=sg[:nt, :], in0=sg[:nt, :], in1=pB[:nt, 256:512], op=ALU.mult),
             reads=[bsg, bpB], writes=[bsg])
        tm, btm = tmr.next()
        sw, bsw = swr.next()
        c.op("dve", lambda e: e.tensor_tensor(out=tm[:nt, :], in0=rf[:nt, :], in1=cc[:nt, :], op=ALU.mult),
             reads=[brf, bcc], writes=[btm])
        rf4 = rf[:nt, :].rearrange("p (a h d) -> p a h d", a=2, h=2)
        sw4 = sw[:nt, :].rearrange("p (a h d) -> p a h d", a=2, h=2)
        sn4 = sn[:nt, :].rearrange("p (a h d) -> p a h d", a=2, h=2)
        c.op("pool", lambda e: e.tensor_tensor(out=sw4[:, :, 0, :], in0=rf4[:, :, 1, :], in1=sn4[:, :, 0, :],
                                               op=ALU.mult), reads=[brf, bsn], writes=[bsw])
        c.op("pool", lambda e: e.tensor_tensor(out=sw4[:, :, 1, :], in0=rf4[:, :, 0, :], in1=sn4[:, :, 1, :],
                                               op=ALU.mult), reads=[brf, bsn], writes=[bsw])
        c.op("dve", lambda e: e.tensor_tensor(out=qk[:nt, 128:384], in0=tm[:nt, :], in1=sw[:nt, :], op=ALU.add),
             reads=[btm, bsw], writes=[bqk])
        for j in range(3):
            c.op("pe", lambda e, j=j: e.transpose(out=pR[:, j, :nt], in_=qk[:nt, j * 128:(j + 1) * 128],
                                                  identity=IDN[:nt, :nt]),
                 reads=[bqk, bCB], writes=[bpR], sig=(j == 2))
        c.op("act", lambda e: e.copy(out=qdst, in_=pR[:, 0, :nt]), reads=[bpR], writes=[bqd])
        tr, btr = trr.next()
        c.op("dve", lambda e: e.tensor_copy(out=tr[:, :, :nt], in_=pR[:, 1:3, :nt]), reads=[bpR], writes=[btr])
        c.op("pe", lambda e: e.matmul(p6[:nt, 0:nt], lhsT=tr[:, 1, :nt], rhs=tr[:, 0, :nt], start=True, stop=True),
             reads=[btr], writes=[bp6])
        sc, bsc = scr.next()
        c.op("dve", lambda e: e.tensor_tensor(out=sc[:nt, :nt], in0=p6[:nt, 0:nt], in1=CF[:nt, 0:nt], op=ALU.mult),
             reads=[bp6, bCF], writes=[bsc])
        qd, bqdc = qdr.next()
        c.op("pool", lambda e: e.tensor_tensor(out=qd[:, :nt], in0=tr[:, 0, :nt], in1=CF[:, 128:128 + nt], op=ALU.mult),
             reads=[btr, bCF], writes=[bqdc])
        c.op("pe", lambda e: e.matmul(p7[:nt, 0:256], lhsT=sc[:nt, :nt], rhs=vb[:nt, :], start=True, stop=False),
             reads=[bsc, bvb], writes=[bp7], sig=False)
        c.op("pe", lambda e: e.matmul(p7[:nt, 0:256], lhsT=qd[:, :nt], rhs=Sb_ap, start=False, stop=True),
             reads=[bqdc, bSb_], writes=[bp7])
        stt, bst = st_r.next()
        og, bog = ogr.next()
        c.op("pool", lambda e: e.memset(stt[:, :], 0.0), writes=[bst])
        c.op("act", lambda e: e.activation(out=og[:nt, :], in_=p7[:nt, 0:256], func=AF.Square,
                                           accum_out=stt[:nt, 2:3]), reads=[bp7], writes=[bog, bst])
        c.op("act", lambda e: e.activation(out=stt[:nt, 3:4], in_=stt[:nt, 2:3], func=AF.Ln,
                                           scale=1.0 / 256, bias=EPS), reads=[bst], writes=[bst])
        c.op("act", lambda e: e.activation(out=stt[:nt, 3:4], in_=stt[:nt, 3:4], func=AF.Exp,
                                           scale=-0.5), reads=[bst], writes=[bst])
        c.op("dve", lambda e: e.scalar_tensor_tensor(out=og[:nt, :], in0=p7[:nt, 0:256], scalar=stt[:nt, 3:4],
                                                     in1=sg[:nt, :], op0=ALU.mult, op1=ALU.mult),
             reads=[bp7, bst, bsg], writes=[bog])
        for j in range(2):
            c.op("pe", lambda e, j=j: e.transpose(out=pR[:, 4 + j, :nt], in_=og[:nt, j * 128:(j + 1) * 128],
                                                  identity=IDN[:nt, :nt]),
                 reads=[bog, bCB], writes=[bpR], sig=(j == 1))
        ogT, bogT = ogTr.next()
        c.op("act", lambda e: e.copy(out=ogT[:, :, :nt], in_=pR[:, 4:6, :nt]), reads=[bpR], writes=[bogT])
        c.dma("pool", oretT[2 * h:2 * h + 2, :, tok0:tok0 + nt].rearrange("c p t -> p c t"), ogT[:, :, :nt],
              reads=[bogT], owner=b_oret)

    banksZ = [(pA, bpA), (pB, bpB)]
    banksA = [(pC, bpC), (p6, bp6)]
    zi = [0]

    def attn_run(qcols, rdq, keys, ncb, cw, dst):
        NA = ncb * cw
        c.op("pool", lambda e: e.memset(Sacc[:, :NA], 0.0), writes=[bSacc])
        c.op("pool", lambda e: e.memset(Saccb[:, :], 0.0), writes=[bSaccb])
        c.op("pe", lambda e: e.matmul(p7[:, :NA], lhsT=Saccb[:, 0:128], rhs=Saccb[:, :NA], start=True, stop=False),
             reads=[bSaccb], writes=[bp7])
        started = [True] * ncb
        for idx, (ktap, bkt, vaap, bva, nk, cb0, mask) in enumerate(keys):
            last = idx == len(keys) - 1
            c0 = cb0 * cw
            N = NA - c0
            (pz, bpz) = banksZ[zi[0] % 2]
            (pa, bpa) = banksA[zi[0] % 2]
            zi[0] += 1

            def zmm(ps, bps, fin):
                c.op("pe", lambda e: e.matmul(ps[:nk, :N], lhsT=ktap, rhs=qcols[:, c0:NA], start=True,
                                              stop=(mask is None and fin)),
                     reads=[bkt] + rdq, writes=[bps], sig=(mask is None and fin))
                if mask is not None:
                    c.op("pe", lambda e: e.matmul(ps[:nk, 0:cw], lhsT=IDN[:nk, :nk], rhs=mask, start=False, stop=fin),
                         reads=[bCB, bMK], writes=[bps], sig=fin)
            zmm(pz, bpz, True)
            E, bE = Er.next()
            c.op("act", lambda e: e.activation(out=E[:nk, :N], in_=pz[:nk, :N], func=AF.Exp),
                 reads=[bpz], writes=[bE])
            SPt, bSP = SPr.next()
            c.op("act", lambda e: e.activation(out=SPt[:nk, :N], in_=E[:nk, :N], func=AF.Ln, bias=1.0),
                 reads=[bE], writes=[bSP])
            zmm(pa, bpa, False)
            c.op("pe", lambda e: e.matmul(pa[:nk, :N], lhsT=TRIN[:nk, :nk], rhs=SPt[:nk, :N],
                                          start=False, stop=(idx == 0)),
                 reads=[bCB, bSP], writes=[bpa], sig=(idx == 0))
            if idx > 0:
                c.op("pe", lambda e: e.matmul(pa[:nk, :N], lhsT=ONESN[:, :nk], rhs=Saccb[:, c0:NA],
                                              start=False, stop=True),
                     reads=[bCB, bSaccb], writes=[bpa])
            if not last:
                c.op("dve", lambda e: e.tensor_tensor(out=Sacc[:nk, c0:NA], in0=Sacc[:nk, c0:NA],
                                                      in1=SPt[:nk, :N], op=ALU.add),
                     reads=[bSacc, bSP], writes=[bSacc])
                c.op("dve", lambda e: e.tensor_copy(out=Saccb[:, c0:NA], in_=Sacc[:, c0:NA]),
                     reads=[bSacc], writes=[bSaccb])
            AT, bAT = ATr.next()
            c.op("act", lambda e: e.activation(out=AT[:nk, :N], in_=pa[:nk, :N], func=AF.Exp),
                 reads=[bpa], writes=[bAT])
            for cb in range(cb0, ncb):
                a0 = (cb - cb0) * cw
                c.op("pe", lambda e, cb=cb, a0=a0: e.matmul(
                    p7[:, cb * cw:(cb + 1) * cw], lhsT=vaap, rhs=AT[:nk, a0:a0 + cw],
                    start=(not started[cb]), stop=last),
                    reads=[bAT, bva], writes=[bp7], sig=(cb == ncb - 1))
                started[cb] = True
        oso, boso = osr.next()
        c.op("dve", lambda e: e.tensor_copy(out=oso[:, :NA], in_=p7[:, :NA]), reads=[bp7], writes=[boso])
        c.dma("pool", dst, oso[:, :NA], reads=[boso], owner=b_osb)

    for h in range(NH):
        load_head(h)
        hc = slice(h * 128, (h + 1) * 128)
        c.op("pool", lambda e: e.memset(S, 0.0), writes=[bS])
        scan_tile(0, NMETA, 0, kp[0:NMETA, hc], vp[0:NMETA, hc], 0)
        for i in range(NXT):
            r0 = NMETA + i * 128
            scan_tile(1 + i, 128, r0, kp[r0:r0 + 128, hc], vp[r0:r0 + 128, hc], 1 + i, sel_l=i // 4, sel_r=i % 4)
        c.dma("pool", sp_o[h], S, reads=[bS], owner=bS, is_output=True)
        for l in range(NL):
            own_tile(h, uto[:, :, l * 128:(l + 1) * 128], 128, ropeoc[l * 128:(l + 1) * 128, :],
                     ropeos[l * 128:(l + 1) * 128, :], QT[:, l * 128:(l + 1) * 128], bQT[l],
                     Ssel[:, l, :], bSsel, l * 128)
        for l0 in range(0, NL, 4):
            l1 = min(l0 + 4, NL)
            keys = []
            for kt in range(4 * (l1 - 1) + 3, -1, -1):
                lp, r = kt // 4, kt % 4
                blk = 1 + kt
                keys.append((KT[:, blk * 128:(blk + 1) * 128], bKT[blk], VA[:, blk, :], bVA[blk], 128,
                             max(lp - l0, 0), MK[:, r, :] if lp >= l0 else None))
            keys.append((KT[:, 0:NMETA], bKT[0], VA[:NMETA, 0, :], bVA[0], NMETA, 0, None))
            attn_run(QT[:, l0 * 128:l1 * 128], [bQT[i] for i in range(l0, l1)], keys, l1 - l0, 128,
                     osbT[h, :, l0 * 128:l1 * 128])
        for s in range(NS):
            tok0 = NL * 128 + s * DEC_T
            c.dma("pool", VA[:, 0:16, :], cv[s, :, hc].rearrange("(a p) d -> p a d", p=128),
                  writes=[bVA[i] for i in range(16)], owner=bVA[0])
            c.dma("pool", ckst, ck[s, :, hc].rearrange("(a p) d -> p a d", p=128), writes=[bckst], owner=bckst)
            for j in range(16):
                c.op("pe", lambda e, j=j: e.transpose(out=pT[:, j, :], in_=ckst[:, j, :], identity=IDN),
                     reads=[bckst, bCB], writes=[bpT], sig=(j == 15))
            c.op("act", lambda e: e.mul(out=KT[:, 0:2048], in_=pT[:].rearrange("p a d -> p (a d)"), mul=128.0 ** -0.5),
                 reads=[bpT], writes=[bKT[i] for i in range(16)])
            c.dma("sp", S, st[s, h], writes=[bS], owner=bS)
            c.op("pool", lambda e: e.tensor_copy(out=Sb, in_=S), reads=[bS], writes=[bSb])
            own_tile(h, uto[:, :, tok0:tok0 + DEC_T], DEC_T, ropesmc[:, :], ropesms[:, :],
                     QT[:, 0:DEC_T], bQT[0], Sb, bSb, tok0)
            scan_tile(1 + NXT + s, DEC_T, ntp, ks[s, :, hc], vs[s, :, hc], 16)
            c.dma("pool", ss_o[s, h], S, reads=[bS], owner=bS, is_output=True)
            keys = [(KT[:, 2048:2048 + DEC_T], bKT[16], VA[:DEC_T, 16, :], bVA[16], DEC_T, 0, NEGM[:DEC_T, :DEC_T])]
            for kb in range(15, -1, -1):
                keys.append((KT[:, kb * 128:(kb + 1) * 128], bKT[kb], VA[:, kb, :], bVA[kb], 128, 0, None))
            attn_run(QT[:, 0:DEC_T], [bQT[0]], keys, 1, DEC_T, osbT[h, :, tok0:tok0 + DEC_T])
    b_osb.w = (b_osb.dsem, b_osb.dcnt)
    b_oret.w = (b_oret.dsem, b_oret.dcnt)

    hs = dt("hs", [NTOK, D], F32, kind=SK).ap()
    b_hs = Buf("hs")
    c.barrier()
    AFa.reset(); ABa.reset()
    CB2 = ABa.get(512); IDN = CB2[:, 0:128]
    GC = 384
    xcr = ring(AFa, 512, 2, "xc")
    gsr = ring(AFa, 512, 3, "gs")
    hcr = ring(AFa, 512, 3, "hc")
    slots = ring(ABa, 8192, 3, "slot")
    actU = ABa.get(16 * GC, GC); bactU = Buf("actU")
    actS = ABa.get(8 * GC, GC); bactS = Buf("actS")
    actR = ABa.get(16 * GC, GC); bactR = Buf("actR")
    MT = ABa.get(16 * GC, GC); bMT = Buf("MT")

    for t0 in range(0, NTOK, GC):
        T = min(GC, NTOK - t0)
        ntl = T // 128
        c.dma("sp", actU[:, :, :T], uto[:, :, t0:t0 + T], reads=[b_uts], writes=[bactU], owner=bactU)
        c.dma("sp", actS[:, :NH, :T], osbT[:, :, t0:t0 + T].rearrange("h p t -> p h t"), reads=[b_osb],
              writes=[bactS], owner=bactS)
        c.dma("sp", actR[:, :2 * NH, :T], oretT[:, :, t0:t0 + T].rearrange("h p t -> p h t"), reads=[b_oret],
              writes=[bactR], owner=bactR)
        for fc in range(16):
            sl, bsl = slots.next()
            fcs = slice(fc * 128, (fc + 1) * 128)
            w1 = sl[:, 0:2048].rearrange("p (a b) -> p a b", b=128)
            w2 = sl[:, 2048:4096].rearrange("p (a b) -> p a b", b=128)
            w3 = sl[:, 4096:4096 + NH * 128].rearrange("p (a b) -> p a b", b=128)
            w4 = sl[:, 6144:6144 + 2 * NH * 128].rearrange("p (a b) -> p a b", b=128)
            c.dma("pool", w1, wg[:, fc * 128:(fc + 1) * 128].rearrange("(k p) n -> p k n", p=128), writes=[bsl], owner=bsl)
            c.dma("pool", w2, wg[:, D + fc * 128:D + (fc + 1) * 128].rearrange("(k p) n -> p k n", p=128), writes=[bsl], owner=bsl)
            c.dma("pool", w3, wsbo[:, fcs].rearrange("(k p) n -> p k n", p=128), writes=[bsl], owner=bsl)
            c.dma("pool", w4, wreto[:, fcs].rearrange("(k p) n -> p k n", p=128), writes=[bsl], owner=bsl)
            n = T
            for (ps, bps, w, act, bact, nk_) in ((pA, bpA, w1, actU, bactU, 16), (pB, bpB, w2, actU, bactU, 16),
                                                 (pC, bpC, w3, actS, bactS, NH), (p6, bp6, w4, actR, bactR, 2 * NH)):
                for k in range(nk_):
                    c.op("pe", lambda e, ps=ps, w=w, act=act, k=k, nk_=nk_: e.matmul(
                        ps[:, :n], lhsT=w[:, k, :], rhs=act[:, k, :n], start=(k == 0), stop=(k == nk_ - 1)),
                        reads=[bsl, bact], writes=[bps], sig=(k == nk_ - 1))
            ga, bga = gsr.next()
            gb, bgb = gsr.next()
            c.op("act", lambda e: e.activation(out=ga[:, :n], in_=pA[:, :n], func=AF.Sigmoid), reads=[bpA], writes=[bga])
            c.op("act", lambda e: e.activation(out=gb[:, :n], in_=pB[:, :n], func=AF.Sigmoid), reads=[bpB], writes=[bgb])
            c.op("dve", lambda e: e.tensor_tensor(out=ga[:, :n], in0=ga[:, :n], in1=pC[:, :n], op=ALU.mult),
                 reads=[bga, bpC], writes=[bga])
            c.op("dve", lambda e: e.tensor_tensor(out=gb[:, :n], in0=gb[:, :n], in1=p6[:, :n], op=ALU.mult),
                 reads=[bgb, bp6], writes=[bgb])
            c.op("dve", lambda e, fc=fc: e.tensor_tensor(out=MT[:, fc, :n], in0=ga[:, :n], in1=gb[:, :n], op=ALU.add),
                 reads=[bga, bgb], writes=[bMT])
        for oc in range(4):
            sl, bsl = slots.next()
            wo = sl[:, 0:8192].rearrange("p (a b) -> p a b", b=512)
            for k4 in range(4):
                c.dma("pool", wo[:, 4 * k4:4 * k4 + 4, :],
                      wout[512 * k4:512 * (k4 + 1), oc * 512:(oc + 1) * 512].rearrange("(k p) n -> p k n", p=128),
                      writes=[bsl], owner=bsl)
            for ti in range(ntl):
                xc, bxc = xcr.next()
                c.dma("sp", xc, xtok[t0 + ti * 128:t0 + (ti + 1) * 128, oc * 512:(oc + 1) * 512], writes=[bxc], owner=bxc)
                (ps, bps) = ((pA, bpA), (pB, bpB))[(oc * ntl + ti) % 2]
                for k in range(16):
                    c.op("pe", lambda e, k=k, ti=ti, ps=ps: e.matmul(ps[:, :], lhsT=MT[:, k, ti * 128:(ti + 1) * 128], rhs=wo[:, k, :],
                                                                     start=(k == 0), stop=(k == 15)),
                         reads=[bMT, bsl], writes=[bps], sig=(k == 15))
                hc_, bhc = hcr.next()
                c.op("dve", lambda e, ps=ps: e.tensor_tensor(out=hc_, in0=ps[:, :], in1=xc, op=ALU.add),
                     reads=[bps, bxc], writes=[bhc])
                c.dma("pool", hs[t0 + ti * 128:t0 + (ti + 1) * 128, oc * 512:(oc + 1) * 512], hc_, reads=[bhc], owner=b_hs,
                      is_output=dbg)
    b_hs.w = (b_hs.dsem, b_hs.dcnt)

    c.barrier()
    AFa.reset(); ABa.reset()
    CB2 = ABa.get(512); IDN = CB2[:, 0:128]
    NTG = GT // 128
    gv = AFa.get(D); bgv = Buf("gv")
    brc = AFa.get(20); bbr = Buf("brc")
    c.dma("sp", brc, br.partition_broadcast(128), writes=[bbr], owner=bbr)
    wrf = AFa.get(320, 20); bwrf = Buf("wrf")
    c.dma("sp", wrf, wr.rearrange("(k p) n -> p k n", p=128), writes=[bwrf], owner=bwrf)
    wrh = ABa.get(320, 20); bwrh = Buf("wrh")
    wrl = ABa.get(320, 20); bwrl = Buf("wrl")
    c.op("act", lambda e: e.copy(out=wrh, in_=wrf), reads=[bwrf], writes=[bwrh])
    c.op("dve", lambda e: e.tensor_tensor(out=wrl, in0=wrf, in1=wrh, op=ALU.subtract), reads=[bwrf, bwrh], writes=[bwrl])
    H = AFa.get(NTG * D, D); bH = [Buf(f"H{i}") for i in range(NTG)]
    Cmb = AFa.get(NTG * 16, 16); bCmb = Buf("Cmb")
    gsr = ring(AFa, 512, 2, "gs2")
    rt = ring(AFa, 64, 2, "rt")
    st2 = ring(AFa, 8, 2, "st2")
    u2f = AFa.get(D); bu2f = Buf("u2f")
    slots = ring(ABa, 8192, 3, "eslot")
    actU = ABa.get(16 * GT, GT); bactU = Buf("u2T")
    u2h = ABa.get(D); bu2h = Buf("u2h")
    u2l = ABa.get(D); bu2l = Buf("u2l")
    loT = ABa.get(D, 128); bloT = Buf("loT")
    hT = ABa.get(4 * GT, GT); bhT = Buf("hT")

    def subgroups(T):
        out, o = [], 0
        while o < T:
            n = min(512, T - o)
            out.append((o, n))
            o += n
        return out

    def rms_tile(ti, gain_buf):
        stt, bst = st2.next()
        c.op("pool", lambda e: e.memset(stt, 0.0), writes=[bst])
        c.op("act", lambda e: e.activation(out=u2f, in_=H[:, ti, :], func=AF.Square, accum_out=stt[:, 0:1]),
             reads=[bH[ti]], writes=[bu2f, bst])
        c.op("act", lambda e: e.activation(out=stt[:, 1:2], in_=stt[:, 0:1], func=AF.Ln, scale=1.0 / D, bias=EPS),
             reads=[bst], writes=[bst])
        c.op("act", lambda e: e.activation(out=stt[:, 1:2], in_=stt[:, 1:2], func=AF.Exp, scale=-0.5),
             reads=[bst], writes=[bst])
        c.op("dve", lambda e: e.scalar_tensor_tensor(out=u2f, in0=H[:, ti, :], scalar=stt[:, 1:2], in1=gv,
                                                     op0=ALU.mult, op1=ALU.mult),
             reads=[bH[ti], bst, bgv], writes=[bu2f])

    for t0 in range(0, NTOK, GT):
        T = min(GT, NTOK - t0)
        ntl = T // 128
        for ti in range(ntl):
            c.dma("sp", H[:, ti, :], hs[t0 + ti * 128:t0 + (ti + 1) * 128, :], reads=[b_hs], writes=[bH[ti]], owner=bH[ti])
        c.dma("sp", gv, g2.partition_broadcast(128), writes=[bgv], owner=bgv)
        for ti in range(ntl):
            rms_tile(ti, gv)
            c.op("act", lambda e: e.copy(out=u2h, in_=u2f), reads=[bu2f], writes=[bu2h])
            c.op("dve", lambda e: e.tensor_tensor(out=u2l, in0=u2f, in1=u2h, op=ALU.subtract),
                 reads=[bu2f, bu2h], writes=[bu2l])
            for j in range(16):
                c.op("pe", lambda e, j=j: e.transpose(out=pT[:, j, :], in_=u2h[:, j * 128:(j + 1) * 128], identity=IDN),
                     reads=[bu2h], writes=[bpT], sig=(j == 15))
            c.op("act", lambda e, ti=ti: e.copy(out=actU[:, :, ti * 128:(ti + 1) * 128], in_=pT[:, :, :]),
                 reads=[bpT], writes=[bactU])
            for j in range(16):
                c.op("pe", lambda e, j=j: e.transpose(out=pT[:, j, :], in_=u2l[:, j * 128:(j + 1) * 128], identity=IDN),
                     reads=[bu2l], writes=[bpT], sig=(j == 15))
            c.op("act", lambda e: e.copy(out=loT, in_=pT[:, :, :]), reads=[bpT], writes=[bloT])
            mm = []
            for k in range(16):
                mm.append((actU[:, k, ti * 128:(ti + 1) * 128], wrh[:, k, :]))
            for k in range(16):
                mm.append((loT[:, k, :], wrh[:, k, :]))
            for k in range(16):
                mm.append((actU[:, k, ti * 128:(ti + 1) * 128], wrl[:, k, :]))
            for i_, (l_, r_) in enumerate(mm):
                c.op("pe", lambda e, l_=l_, r_=r_, i_=i_: e.matmul(pB[:, 0:20], lhsT=l_, rhs=r_, start=(i_ == 0), stop=(i_ == 47)),
                     reads=[bactU, bloT, bwrh, bwrl], writes=[bpB], sig=(i_ == 47))
            R_, bR = rt.next()
            L = R_[:, 0:20]
            c.op("dve", lambda e: e.tensor_tensor(out=L, in0=pB[:, 0:20], in1=brc, op=ALU.add), reads=[bpB, bbr], writes=[bR])
            gmax, gsum, m1, m2 = R_[:, 20:21], R_[:, 21:22], R_[:, 22:23], R_[:, 23:24]
            oh, pen = R_[:, 24:28], R_[:, 28:32]
            elm, mk1 = R_[:, 32:48], R_[:, 48:64]
            R2, bR2 = rt.next()
            elm2, mk2, ge = R2[:, 0:16], R2[:, 16:32], R2[:, 32:36]
            w1_, w2_, e2 = R2[:, 36:37], R2[:, 37:38], R2[:, 38:39]
            rb = [bR, bR2]
            V = lambda fn: c.op("dve", fn, reads=rb, writes=rb)
            V(lambda e: e.tensor_reduce(out=gmax, in_=L[:, 0:4], op=ALU.max, axis=AX.X))
            V(lambda e: e.tensor_scalar(out=oh, in0=L[:, 0:4], scalar1=gmax, scalar2=None, op0=ALU.is_ge))
            V(lambda e: e.tensor_scalar(out=ge, in0=L[:, 0:4], scalar1=gmax, scalar2=None, op0=ALU.subtract))
            c.op("act", lambda e: e.activation(out=ge, in_=ge, func=AF.Exp), reads=rb, writes=rb)
            V(lambda e: e.tensor_reduce(out=gsum, in_=ge, op=ALU.add, axis=AX.X))
            V(lambda e: e.reciprocal(out=gsum, in_=gsum))
            V(lambda e: e.tensor_scalar(out=pen, in0=oh, scalar1=-1.0, scalar2=1e9, op0=ALU.add, op1=ALU.mult))
            for gi in range(4):
                V(lambda e, gi=gi: e.tensor_scalar(out=elm[:, gi * 4:gi * 4 + 4], in0=L[:, 4 + gi * 4:8 + gi * 4],
                                                   scalar1=pen[:, gi:gi + 1], scalar2=None, op0=ALU.add))
            V(lambda e: e.tensor_reduce(out=m1, in_=elm, op=ALU.max, axis=AX.X))
            V(lambda e: e.tensor_scalar(out=mk1, in0=elm, scalar1=m1, scalar2=None, op0=ALU.is_ge))
            V(lambda e: e.scalar_tensor_tensor(out=elm2, in0=mk1, scalar=-1e9, in1=elm, op0=ALU.mult, op1=ALU.add))
            V(lambda e: e.tensor_reduce(out=m2, in_=elm2, op=ALU.max, axis=AX.X))
            V(lambda e: e.tensor_scalar(out=mk2, in0=elm2, scalar1=m2, scalar2=None, op0=ALU.is_ge))
            V(lambda e: e.tensor_tensor(out=e2, in0=m2, in1=m1, op=ALU.subtract))
            c.op("act", lambda e: e.activation(out=e2, in_=e2, func=AF.Exp), reads=rb, writes=rb)
            V(lambda e: e.tensor_scalar(out=w1_, in0=e2, scalar1=1.0, scalar2=None, op0=ALU.add))
            V(lambda e: e.reciprocal(out=w1_, in_=w1_))
            V(lambda e: e.tensor_tensor(out=w1_, in0=w1_, in1=gsum, op=ALU.mult))
            V(lambda e: e.tensor_tensor(out=w2_, in0=w1_, in1=e2, op=ALU.mult))
            V(lambda e: e.tensor_scalar(out=mk1, in0=mk1, scalar1=w1_, scalar2=None, op0=ALU.mult))
            c.op("dve", lambda e, ti=ti: e.scalar_tensor_tensor(out=Cmb[:, ti, :], in0=mk2, scalar=w2_, in1=mk1,
                                                                op0=ALU.mult, op1=ALU.add), reads=rb, writes=rb + [bCmb])
        for ex in range(16):
            wge_, bwg = slots.next()
            wue_, bwu = slots.next()
            wde_, bwd = slots.next()
            wge = wge_.rearrange("p (a b) -> p a b", b=512)
            wue = wue_.rearrange("p (a b) -> p a b", b=512)
            wde = wde_.rearrange("p (a b) -> p a b", b=D)
            for k4 in range(4):
                c.dma("pool", wge[:, 4 * k4:4 * k4 + 4, :], wgate[ex, 512 * k4:512 * (k4 + 1), :].rearrange("(k p) n -> p k n", p=128),
                      writes=[bwg], owner=bwg)
            for k4 in range(4):
                c.dma("pool", wue[:, 4 * k4:4 * k4 + 4, :], wup[ex, 512 * k4:512 * (k4 + 1), :].rearrange("(k p) n -> p k n", p=128),
                      writes=[bwu], owner=bwu)
            c.dma("pool", wde, wdown[ex].rearrange("(k p) n -> p k n", p=128), writes=[bwd], owner=bwd)
            for (o, n) in subgroups(T):
                for fx in range(4):
                    for (ps, bps, w, bw) in ((pA, bpA, wge, bwg), (pB, bpB, wue, bwu)):
                        for k in range(16):
                            c.op("pe", lambda e, ps=ps, w=w, k=k, fx=fx: e.matmul(
                                ps[:, :n], lhsT=w[:, k, fx * 128:(fx + 1) * 128], rhs=actU[:, k, o:o + n],
                                start=(k == 0), stop=(k == 15)),
                                reads=[bw, bactU], writes=[bps], sig=(k == 15))
                    ga, bga = gsr.next()
                    c.op("act", lambda e: e.activation(out=ga[:, :n], in_=pA[:, :n], func=AF.Sigmoid), reads=[bpA], writes=[bga])
                    c.op("dve", lambda e: e.tensor_tensor(out=ga[:, :n], in0=ga[:, :n], in1=pA[:, :n], op=ALU.mult),
                         reads=[bga, bpA], writes=[bga])
                    c.op("dve", lambda e, fx=fx: e.tensor_tensor(out=hT[:, fx, o:o + n], in0=ga[:, :n], in1=pB[:, :n], op=ALU.mult),
                         reads=[bga, bpB], writes=[bhT])
            for ti in range(ntl):
                for oc in range(4):
                    (ps, bps) = ((pC, bpC), (p6, bp6))[(ti * 4 + oc) % 2]
                    for fx in range(4):
                        c.op("pe", lambda e, ps=ps, fx=fx, ti=ti, oc=oc: e.matmul(
                            ps[:, :], lhsT=hT[:, fx, ti * 128:(ti + 1) * 128], rhs=wde[:, fx, oc * 512:(oc + 1) * 512],
                            start=(fx == 0), stop=(fx == 3)),
                            reads=[bhT, bwd], writes=[bps], sig=(fx == 3))
                    c.op("dve", lambda e, ps=ps, ti=ti, oc=oc, ex=ex: e.scalar_tensor_tensor(
                        out=H[:, ti, oc * 512:(oc + 1) * 512], in0=ps[:, :], scalar=Cmb[:, ti, ex:ex + 1],
                        in1=H[:, ti, oc * 512:(oc + 1) * 512], op0=ALU.mult, op1=ALU.add),
                        reads=[bps, bCmb, bH[ti]], writes=[bH[ti]])
        c.dma("sp", gv, gf.partition_broadcast(128), writes=[bgv], owner=bgv)
        for ti in range(ntl):
            rms_tile(ti, gv)
            c.dma("sp", yo[t0 + ti * 128:t0 + (ti + 1) * 128, :], u2f, reads=[bu2f], owner=bu2f, is_output=True)
    c.finish()
    return c


def head_consts(h):
    lg = np.log1p(-np.float32(2.0) ** np.float32(-5.0 - h)).astype(np.float32)
    t = np.arange(128, dtype=np.float32)
    rel = t[None, :] - t[:, None]
    dmt = np.where(rel >= 0, np.exp(np.maximum(rel, 0) * lg), 0.0).astype(np.float32)
    gq = np.broadcast_to(np.exp((t + 1.0) * lg)[None, :], (128, 128)).astype(np.float32)
    cf = np.zeros((128, 262), np.float32)
    cf[:, 0:128] = dmt
    cf[:, 128:256] = gq
    for i, nt in enumerate((128, 64, 16)):
        v = np.zeros(128, np.float32)
        v[:nt] = np.exp((nt - 1.0 - t[:nt]) * lg)
        cf[:, 256 + i] = v
        cf[:, 259 + i] = np.exp(np.float32(nt) * lg)
    return cf


def bf_consts():
    cb = np.zeros((128, 512), np.float32)
    i = np.arange(128)
    cb[:, 0:128] = np.eye(128)
    cb[:, 128:256] = np.where(i[:, None] >= i[None, :], NEG, 0.0)
    cb[:, 256:384] = np.where(i[:, None] >= i[None, :], -1.0, 0.0)
    cb[:, 384:512] = -1.0
    return cb


def rope_tables(ntp):
    pos = np.concatenate([np.arange(ntp, dtype=np.float32) - NMETA, PAST + np.arange(DEC_T, dtype=np.float32)])
    inv = (1.0 / (np.float32(10000.0) ** (np.arange(0, 128, 2, dtype=np.float32) / np.float32(128)))).astype(np.float32)
    ang = (pos[:, None] * inv[None, :]).astype(np.float32)
    cs, sn = np.cos(ang).astype(np.float32), np.sin(ang).astype(np.float32)
    s = np.float32(128.0 ** -0.5)
    cc = np.concatenate([cs, cs, cs * s, cs * s], axis=1).astype(np.float32)
    ss = np.concatenate([-sn, sn, -sn * s, sn * s], axis=1).astype(np.float32)
    return np.ascontiguousarray(cc), np.ascontiguousarray(ss)


def wh_cols(h):
    def r(o, w):
        return np.arange(o + h * w, o + (h + 1) * w)
    return np.concatenate([r(1024, 128), r(2048, 128), r(0, 128), r(3072, 128), r(4096, 128),
                           r(5120, 256), r(7168, 256)])


def make_maps(inp, NXT=64, NH=8, NS=NSC, cores=range(8)):
    ntp = NMETA + NXT * 128
    NL = NXT // 4
    cc, ss = rope_tables(ntp)
    cb = bf_consts()
    cf = np.stack([head_consts(h) for h in range(NH)], 0)
    w_in = inp["w_in"][0]
    wh = np.stack([w_in[:, wh_cols(h)] for h in range(NH)], 0)
    c_ = np.ascontiguousarray
    shared = {
        "meta": c_(inp["meta"]), "g1": c_(inp["norm1_g"][0]), "g2": c_(inp["norm2_g"][0]), "gf": c_(inp["normf_g"]),
        "wh": wh, "cstf": cf, "cstb": cb, "ropec": c_(cc[:, 128:256]), "ropes": c_(ss[:, 128:256]),
        "ropesmc": c_(cc[ntp:ntp + DEC_T]), "ropesms": c_(ss[ntp:ntp + DEC_T]),
        "wg": c_(w_in[:, 9216:13312]), "wsbo": c_(inp["w_sb_o"][0][:NH * 128]), "wreto": c_(inp["w_ret_o"][0][:NH * 256]),
        "wout": c_(inp["w_out"][0]),
        "wr": c_(np.concatenate([inp["w_grp"][0], inp["w_exp"][0]], axis=1)),
        "br": c_(np.concatenate([inp["b_grp"][0], inp["b_exp"][0]], axis=0)),
        "wgate": c_(inp["w_gate"][0]), "wup": c_(inp["w_up"][0]), "wdown": c_(inp["w_down"][0]),
    }
    maps = []
    ii = np.arange(128)
    for cid in cores:
        b, j = cid // 4, cid % 4
        tiles = [4 * l + j for l in range(NL)]
        xp = inp["x_prompt"][b]
        xtok = np.concatenate([xp[t * 128:(t + 1) * 128] for t in tiles]
                              + [inp["x_sample"][NSC * cid + s] for s in range(NS)], axis=0)
        rows = np.concatenate([NMETA + t * 128 + ii for t in tiles]) if NL else np.zeros((0,), np.int64)
        mk = np.zeros((128, 4, 128), np.float32)
        for r in range(4):
            if r == j:
                mk[:, r, :] = np.where(ii[:, None] >= ii[None, :], NEG, 0.0)
            elif r > j:
                mk[:, r, :] = NEG
        sel = np.zeros((128, 4), np.float32)
        sel[:, j] = 1.0
        m = dict(shared)
        m.update({
            "xall": c_(xp[:NXT * 128]), "xtok": c_(xtok),
            "st": c_(inp["state_ret"][0, NSC * cid:NSC * cid + NS, :NH]),
            "ropeoc": c_(cc[rows]), "ropeos": c_(ss[rows]),
            "msk": c_(mk.reshape(128, 512)), "sel": sel,
            "ck": c_(inp["cache_sb_k"][0, NSC * cid:NSC * cid + NS, :, :NH].reshape(NS, PAST, NH * 128)),
            "cv": c_(inp["cache_sb_v"][0, NSC * cid:NSC * cid + NS, :, :NH].reshape(NS, PAST, NH * 128)),
        })
        maps.append(m)
    return maps


def kernel(**inputs):
    inp = {k: np.asarray(v) for k, v in inputs.items()}
    nc = bass.Bass("TRN2", target_bir_lowering=False)
    build(nc)
    maps = make_maps(inp)
    res = run_bass_kernel_spmd(nc, maps, core_ids=list(range(8)))
    R = res.results
    NL = 16
    y_p = np.zeros((2, SEQ, D), np.float32)
    y_s = np.zeros((DEC_B, DEC_T, D), np.float32)
    for cid in range(8):
        b, j = cid // 4, cid % 4
        yo = np.asarray(R[cid]["yo"])
        for l in range(NL):
            t = 4 * l + j
            y_p[b, t * 128:(t + 1) * 128] = yo[l * 128:(l + 1) * 128]
        for s in range(NSC):
            y_s[NSC * cid + s] = yo[NL * 128 + s * DEC_T:NL * 128 + (s + 1) * DEC_T]
    kp = np.stack([np.asarray(R[4 * b]["kp"]).reshape(TP, 8, 128) for b in range(2)], 0)[None]
    vp = np.stack([np.asarray(R[4 * b]["vp"]).reshape(TP, 8, 128) for b in range(2)], 0)[None]
    sp = np.stack([np.asarray(R[4 * b]["sp_o"]) for b in range(2)], 0)[None]
    ks = np.concatenate([np.asarray(R[c]["ks"]).reshape(NSC, DEC_T, 8, 128) for c in range(8)], 0)[None]
    vs = np.concatenate([np.asarray(R[c]["vs"]).reshape(NSC, DEC_T, 8, 128) for c in range(8)], 0)[None]
    ss = np.concatenate([np.asarray(R[c]["ss_o"]) for c in range(8)], 0)[None]
    f = lambda a: np.ascontiguousarray(a, dtype=np.float32)
    return (y_p, y_s, f(kp), f(vp), f(sp), f(ks), f(vs), f(ss))
```

```python
import numpy as np
import concourse.bass as bass
import concourse.mybir as mybir
from concourse.bass_utils import run_bass_kernel_spmd

F32 = mybir.dt.float32
BF16 = mybir.dt.bfloat16
AF = mybir.ActivationFunctionType
ALU = mybir.AluOpType

D = 2048
SEQ = 8192
NMETA = 16
TP = SEQ + NMETA
DEC_B = 32
DEC_T = 64
PAST = 2048
NSC = 4
EPS = 1e-6
NEG = -30000.0
SEM_LIMIT = 30000
WKV = 640


class Buf:
    __slots__ = ("name", "w", "r", "dsem", "dcnt", "excl")

    def __init__(self, name, excl=False):
        self.name = name
        self.excl = excl
        self.w = None
        self.r = {}
        self.dsem = None
        self.dcnt = 0


class Ctx:
    def __init__(self, nc):
        self.nc = nc
        self.eng = {"pe": nc.tensor, "act": nc.scalar, "dve": nc.vector,
                    "pool": nc.gpsimd, "sp": nc.sync}
        self.sem = {}
        self.cnt = {}
        self.waited = {}
        self.pend_r = {}
        self.pend_w = {}
        self.nsem = 0
        for k in self.eng:
            self.sem[k] = self._newsem("e_" + k)
            self.cnt[k] = 0
            self.waited[k] = {}
            self.pend_r[k] = []
            self.pend_w[k] = []
        self.out_tokens = {}
        self.dtoks = {}
        self.ninst = 0

    def _newsem(self, name):
        self.nsem += 1
        return self.nc.alloc_semaphore(f"{name}_{self.nsem}")

    def _wait(self, en, tok):
        if tok is None:
            return
        sem, val = tok
        w = self.waited[en]
        if w.get(sem.num, 0) >= val:
            return
        self.eng[en].wait_ge(sem, val)
        w[sem.num] = val

    def _deps(self, en, reads, writes):
        skip = self.sem[en].num if en == "pe" else None
        for b in reads:
            if b.w is not None and b.w[0].num != skip:
                self._wait(en, b.w)
        for b in writes:
            if b.w is not None and b.w[0].num != skip:
                self._wait(en, b.w)
            for t in b.r.values():
                if t[0].num != skip:
                    self._wait(en, t)

    def _commit(self, tok, reads, writes):
        sem, val = tok
        for b in reads:
            b.r[sem.num] = tok
        for b in writes:
            b.w = tok
            b.r = {}

    def op(self, en, fn, reads=(), writes=(), sig=True):
        ex = [b for b in reads if b.excl]
        if ex:
            reads = [b for b in reads if not b.excl]
            writes = list(writes) + ex
        self._deps(en, reads, writes)
        ins = fn(self.eng[en])
        self.ninst += 1
        if not sig:
            self.pend_r[en].extend(reads)
            self.pend_w[en].extend(writes)
            return None
        if self.cnt[en] >= SEM_LIMIT:
            self.sem[en] = self._newsem("e_" + en)
            self.cnt[en] = 0
        self.cnt[en] += 1
        ins.then_inc(self.sem[en], 1)
        tok = (self.sem[en], self.cnt[en])
        self._commit(tok, list(reads) + self.pend_r[en], list(writes) + self.pend_w[en])
        self.pend_r[en] = []
        self.pend_w[en] = []
        return tok

    def dma(self, q, out, in_, reads=(), writes=(), owner=None, is_output=False):
        self._deps(q, reads, writes)
        ins = self.eng[q].dma_start(out=out, in_=in_)
        self.ninst += 1
        if owner.dsem is None or owner.dcnt >= SEM_LIMIT:
            owner.dsem = self._newsem("d_" + owner.name)
            owner.dcnt = 0
        owner.dcnt += 16
        ins.then_inc(owner.dsem, 16)
        tok = (owner.dsem, owner.dcnt)
        self.dtoks[owner.dsem.num] = tok
        self._commit(tok, reads, writes)
        if is_output:
            self.out_tokens[owner.dsem.num] = tok
        return tok

    def barrier(self):
        toks = [(self.sem[k], self.cnt[k]) for k in self.eng if self.cnt[k] > 0] + list(self.dtoks.values())
        for en in self.eng:
            for t in toks:
                if t[0] is self.sem[en]:
                    continue
                self._wait(en, t)

    def finish(self, en="sp"):
        for tok in self.out_tokens.values():
            self._wait(en, tok)


AX = mybir.AxisListType
WALL = 1152
GT = 768


class Ring:
    def __init__(self, aps, name):
        self.t = list(aps)
        self.b = [Buf(f"{name}{i}") for i in range(len(aps))]
        self.i = 0

    def next(self):
        t, b = self.t[self.i], self.b[self.i]
        self.i = (self.i + 1) % len(self.t)
        return t, b


class _Pool:
    def __init__(self, t, size):
        self.t, self.size, self.off = t, size, 0


class Arena:
    def __init__(self, pool, f32):
        self.p, self.f32 = pool, f32

    def reset(self):
        self.p.off = 0

    def get(self, n, b=None):
        p = self.p
        if self.f32:
            p.off += p.off % 2
            a = p.t[:, p.off:p.off + 2 * n].bitcast(F32)
            p.off += 2 * n
        else:
            a = p.t[:, p.off:p.off + n]
            p.off += n
        assert p.off <= p.size, (p.off, p.size)
        if b is not None:
            a = a.rearrange("p (a b) -> p a b", b=b)
        return a


def build(nc, NXT=64, NH=8, NS=NSC, dbg=False):
    c = Ctx(nc)
    dt = nc.dram_tensor
    NL = NXT // 4
    NTOK = NL * 128 + NS * DEC_T
    assert NTOK % 128 == 0
    ntp = NMETA + NXT * 128
    NBLK = max(1 + NXT, 17)
    I = "ExternalInput"
    xall = dt("xall", [NXT * 128, D], F32, kind=I).ap()
    meta = dt("meta", [NMETA, D], F32, kind=I).ap()
    xtok = dt("xtok", [NTOK, D], F32, kind=I).ap()
    g1 = dt("g1", [D], F32, kind=I).ap()
    g2 = dt("g2", [D], F32, kind=I).ap()
    gf = dt("gf", [D], F32, kind=I).ap()
    wh = dt("wh", [NH, D, WALL], F32, kind=I).ap()
    st = dt("st", [NS, NH, 128, 256], F32, kind=I).ap()
    cstf = dt("cstf", [NH, 128, 262], F32, kind=I).ap()
    cstb = dt("cstb", [128, 512], F32, kind=I).ap()
    ropec = dt("ropec", [ntp + DEC_T, 128], F32, kind=I).ap()
    ropes = dt("ropes", [ntp + DEC_T, 128], F32, kind=I).ap()
    ropesmc = dt("ropesmc", [DEC_T, 256], F32, kind=I).ap()
    ropesms = dt("ropesms", [DEC_T, 256], F32, kind=I).ap()
    ropeoc = dt("ropeoc", [NL * 128, 256], F32, kind=I).ap()
    ropeos = dt("ropeos", [NL * 128, 256], F32, kind=I).ap()
    msk = dt("msk", [128, 512], F32, kind=I).ap()
    sel = dt("sel", [128, 4], F32, kind=I).ap()
    ck = dt("ck", [NS, PAST, NH * 128], F32, kind=I).ap()
    cv = dt("cv", [NS, PAST, NH * 128], F32, kind=I).ap()
    wg = dt("wg", [D, 2 * D], F32, kind=I).ap()
    wsbo = dt("wsbo", [NH * 128, D], F32, kind=I).ap()
    wreto = dt("wreto", [NH * 256, D], F32, kind=I).ap()
    wout = dt("wout", [D, D], F32, kind=I).ap()
    wr = dt("wr", [D, 20], F32, kind=I).ap()
    br = dt("br", [20], F32, kind=I).ap()
    wgate = dt("wgate", [16, D, 512], F32, kind=I).ap()
    wup = dt("wup", [16, D, 512], F32, kind=I).ap()
    wdown = dt("wdown", [16, 512, D], F32, kind=I).ap()

    O = "ExternalOutput"
    kp = dt("kp", [ntp, NH * 128], F32, kind=O).ap()
    vp = dt("vp", [ntp, NH * 128], F32, kind=O).ap()
    sp_o = dt("sp_o", [NH, 128, 256], F32, kind=O).ap()
    ks = dt("ks", [NS, DEC_T, NH * 128], F32, kind=O).ap()
    vs = dt("vs", [NS, DEC_T, NH * 128], F32, kind=O).ap()
    ss_o = dt("ss_o", [NS, NH, 128, 256], F32, kind=O).ap()
    yo = dt("yo", [NTOK, D], F32, kind=O).ap()

    SK = O if dbg else "Internal"
    NTL = 1 + NXT + NS
    uts = dt("uts", [NTL, 128, 16 * 128], BF16, kind="Internal").ap()
    uto = dt("uto", [128, 16 * NTOK], BF16, kind="Internal").ap().rearrange("p (k t) -> p k t", t=NTOK)
    osbT = dt("osbT", [NH, 128, NTOK], BF16, kind=SK).ap()
    oretT = dt("oretT", [2 * NH, 128, NTOK], BF16, kind=SK).ap()
    hdbg = dt("hdbg", [NTOK, D], F32, kind=O).ap() if dbg else None
    b_uts = Buf("uts")
    b_osb = Buf("osbT")
    b_oret = Buf("oretT")

    NPOOL = 95000
    pool_ = _Pool(nc.alloc_sbuf_tensor("arena", [128, NPOOL], BF16), NPOOL)
    AFa, ABa = Arena(pool_, True), Arena(pool_, False)

    pT = nc.alloc_psum_tensor("pT", [128, 16, 128], BF16); bpT = Buf("pT", True)
    pA = nc.alloc_psum_tensor("pA", [128, 512], F32); bpA = Buf("pA", True)
    pB = nc.alloc_psum_tensor("pB", [128, 512], F32); bpB = Buf("pB", True)
    pC = nc.alloc_psum_tensor("pC", [128, 512], F32); bpC = Buf("pC", True)
    p6 = nc.alloc_psum_tensor("p6", [128, 512], F32); bp6 = Buf("p6", True)
    p7 = nc.alloc_psum_tensor("p7", [128, 512], F32); bp7 = Buf("p7", True)
    pR = nc.alloc_psum_tensor("pR", [128, 8, 128], BF16); bpR = Buf("pR", True)

    def ring(ar, n, cnt, name, b=None):
        return Ring([ar.get(n, b) for _ in range(cnt)], name)

    CB = ABa.get(512); bCB = Buf("CB")
    gbc = AFa.get(D); bg = Buf("gbc")
    c.dma("sp", gbc, g1.partition_broadcast(128), writes=[bg], owner=bg)
    c.dma("pool", CB, cstb, writes=[bCB], owner=bCB)
    MK = ABa.get(512, 128); bMK = Buf("MK")
    c.dma("pool", MK, msk.rearrange("p (r q) -> p r q", q=128), writes=[bMK], owner=bMK)
    SEL = AFa.get(4); bSEL = Buf("SEL")
    c.dma("sp", SEL, sel, writes=[bSEL], owner=bSEL)
    IDN = CB[:, 0:128]
    NEGM = CB[:, 128:256]
    TRIN = CB[:, 256:384]
    ONESN = CB[:, 384:512]
    NTI = {128: 0, 64: 1, 16: 2}

    xr = ring(AFa, D, 2, "xt")
    st_r = ring(AFa, 4, 2, "ss")
    ur = ring(ABa, D, 2, "u")
    uTr = ring(ABa, 2048, 2, "uT", 128)
    ccr = ring(AFa, 256, 2, "cc")
    ssr = ring(AFa, 256, 2, "sn")
    kvr = ring(AFa, 256, 3, "kvst")
    rfr = ring(AFa, 256, 2, "rf")
    tmr = ring(AFa, 256, 2, "tm")
    swr = ring(AFa, 256, 2, "sw")
    sgr = ring(AFa, 256, 2, "sg")
    vbr = ring(ABa, 256, 2, "vbf")
    kdr = ring(ABa, 128, 2, "kdec")
    k16r = ring(ABa, 128, 2, "k16")
    qkr = ring(ABa, 384, 2, "qk16")
    trr = ring(ABa, 256, 2, "trT", 128)
    scr = ring(ABa, 128, 2, "scm")
    qdr = ring(ABa, 128, 2, "qdec")
    ogr = ring(ABa, 256, 2, "og")
    ogTr = ring(ABa, 256, 2, "ogT", 128)
    Wh = ABa.get(16 * WALL, WALL); bWh = Buf("Wh")
    CF = AFa.get(262); bCF = Buf("CF")
    S = AFa.get(256); bS = Buf("S")
    Sb = ABa.get(256); bSb = Buf("Sb")
    Ssel = ABa.get(max(NL, 1) * 256, 256); bSsel = Buf("Ssel")
    KT = ABa.get(NBLK * 128)
    VA = ABa.get(NBLK * 128, 128)
    QT = ABa.get(max(NL, 1) * 128)
    bKT = [Buf(f"KT{i}") for i in range(NBLK)]
    bVA = [Buf(f"VA{i}") for i in range(NBLK)]
    bQT = [Buf(f"QT{i}") for i in range(max(NL, 1))]
    Er = ring(AFa, 512, 2, "E")
    SPr = ring(ABa, 512, 2, "SP")
    ATr = ring(ABa, 512, 2, "AT")
    Sacc = AFa.get(512); bSacc = Buf("Sacc")
    Saccb = ABa.get(512); bSaccb = Buf("Saccb")
    osr = ring(ABa, 512, 2, "oso")
    ckst = ABa.get(2048, 128); bckst = Buf("ckst")

    def norm_tile(xsrc, nt, dsts):
        xt, bx = xr.next()
        c.dma("sp", xt[:nt, :], xsrc, writes=[bx], owner=bx)
        stt, bst = st_r.next()
        u, bu = ur.next()
        c.op("pool", lambda e: e.memset(stt[:, :], 0.0), writes=[bst])
        c.op("act", lambda e: e.activation(out=u[:nt, :], in_=xt[:nt, :], func=AF.Square,
                                           accum_out=stt[:nt, 0:1]), reads=[bx], writes=[bu, bst])
        c.op("act", lambda e: e.activation(out=stt[:nt, 1:2], in_=stt[:nt, 0:1], func=AF.Ln,
                                           scale=1.0 / D, bias=EPS), reads=[bst], writes=[bst])
        c.op("act", lambda e: e.activation(out=stt[:nt, 1:2], in_=stt[:nt, 1:2], func=AF.Exp,
                                           scale=-0.5), reads=[bst], writes=[bst])
        c.op("dve", lambda e: e.scalar_tensor_tensor(out=u[:nt, :], in0=xt[:nt, :], scalar=stt[:nt, 1:2],
                                                     in1=gbc[:nt, :], op0=ALU.mult, op1=ALU.mult),
             reads=[bx, bst, bg], writes=[bu])
        for j in range(16):
            c.op("pe", lambda e, j=j: e.transpose(out=pT[:, j, :nt], in_=u[:nt, j * 128:(j + 1) * 128],
                                                  identity=IDN[:nt, :nt]),
                 reads=[bu, bCB], writes=[bpT], sig=(j == 15))
        uT, buT = uTr.next()
        c.op("act", lambda e: e.copy(out=uT[:, :, :nt], in_=pT[:, :, :nt]), reads=[bpT], writes=[buT])
        for dst in dsts:
            c.dma("pool", dst, uT[:, :, :nt], reads=[buT], owner=b_uts)

    def uts_ap(idx, nt):
        return uts[idx].rearrange("p (k t) -> p k t", t=128)[:, :, :nt]

    norm_tile(meta, NMETA, [uts_ap(0, NMETA)])
    for i in range(NXT):
        norm_tile(xall[i * 128:(i + 1) * 128, :], 128, [uts_ap(1 + i, 128)])
    for l in range(NL):
        norm_tile(xtok[l * 128:(l + 1) * 128, :], 128, [uto[:, :, l * 128:(l + 1) * 128]])
    for s in range(NS):
        t0 = NL * 128 + s * DEC_T
        norm_tile(xtok[t0:t0 + DEC_T, :], DEC_T, [uts_ap(1 + NXT + s, DEC_T), uto[:, :, t0:t0 + DEC_T]])
    b_uts.w = (b_uts.dsem, b_uts.dcnt)

    def load_head(h):
        for k4 in range(4):
            c.dma("pool", Wh[:, 4 * k4:4 * k4 + 4, :],
                  wh[h, 512 * k4:512 * (k4 + 1), :].rearrange("(k p) n -> p k n", p=128),
                  writes=[bWh], owner=bWh)
        c.dma("sp", CF, cstf[h], writes=[bCF], owner=bCF)

    pA_, bpA_, pB_, bpB_ = pA, bpA, pB, bpB
    sci = [0]

    def scan_tile(idx, nt, rope_row, kout, vout, blk, sel_l=None, sel_r=None):
        (pA, bpA, pB, bpB) = ((pA_, bpA_, pB_, bpB_), (pC, bpC, p6, bp6))[sci[0] % 2]
        sci[0] += 1
        uT, buT = uTr.next()
        c.dma("sp", uT[:, :, :nt], uts_ap(idx, nt), reads=[b_uts], writes=[buT], owner=buT)
        cc, bcc = ccr.next()
        sn, bsn = ssr.next()
        c.dma("sp", cc[:nt, 0:128], ropec[rope_row:rope_row + nt, :], writes=[bcc], owner=bcc)
        c.dma("sp", sn[:nt, 0:128], ropes[rope_row:rope_row + nt, :], writes=[bsn], owner=bsn)
        for (ps, bps, c0, cn) in ((pA, bpA, 0, 256), (pB, bpB, 512, 384)):
            for k in range(16):
                c.op("pe", lambda e, ps=ps, c0=c0, cn=cn, k=k: e.matmul(
                    ps[:nt, :cn], lhsT=uT[:, k, :nt], rhs=Wh[:, k, c0:c0 + cn],
                    start=(k == 0), stop=(k == 15)),
                    reads=[buT, bWh], writes=[bps], sig=(k == 15))
        kv, bkv = kvr.next()
        c.op("act", lambda e: e.copy(out=kv[:nt, 0:128], in_=pA[:nt, 0:128]), reads=[bpA], writes=[bkv])
        c.op("act", lambda e: e.copy(out=kv[:nt, 128:256], in_=pA[:nt, 128:256]), reads=[bpA], writes=[bkv])
        c.dma("pool", kout, kv[:nt, 0:128], reads=[bkv], owner=bkv, is_output=True)
        c.dma("pool", vout, kv[:nt, 128:256], reads=[bkv], owner=bkv, is_output=True)
        k16, bk16 = k16r.next()
        c.op("act", lambda e: e.copy(out=k16[:nt, :], in_=pA[:nt, 0:128]), reads=[bpA], writes=[bk16])
        c.op("dve", lambda e: e.tensor_copy(out=VA[:nt, blk, :], in_=pA[:nt, 128:256]), reads=[bpA], writes=[bVA[blk]])
        c.op("pe", lambda e: e.transpose(out=pR[:, 7, :nt], in_=k16[:nt, :], identity=IDN[:nt, :nt]),
             reads=[bk16, bCB], writes=[bpR])
        c.op("act", lambda e: e.mul(out=KT[:, blk * 128:blk * 128 + nt], in_=pR[:, 7, :nt], mul=128.0 ** -0.5),
             reads=[bpR], writes=[bKT[blk]])
        rf, brf = rfr.next()
        c.op("dve", lambda e: e.tensor_copy(out=rf[:nt, 0:128], in_=pB[:nt, 0:128]), reads=[bpB], writes=[brf])
        vb, bvb = vbr.next()
        c.op("act", lambda e: e.copy(out=vb[:nt, :], in_=pB[:nt, 128:384]), reads=[bpB], writes=[bvb])
        tm, btm = tmr.next()
        sw, bsw = swr.next()
        c.op("dve", lambda e: e.tensor_tensor(out=tm[:nt, 0:128], in0=rf[:nt, 0:128], in1=cc[:nt, 0:128], op=ALU.mult),
             reads=[brf, bcc], writes=[btm])
        c.op("pool", lambda e: e.tensor_tensor(out=sw[:nt, 0:64], in0=rf[:nt, 64:128], in1=sn[:nt, 0:64],
                                               op=ALU.mult), reads=[brf, bsn], writes=[bsw])
        c.op("pool", lambda e: e.tensor_tensor(out=sw[:nt, 64:128], in0=rf[:nt, 0:64], in1=sn[:nt, 64:128],
                                               op=ALU.mult), reads=[brf, bsn], writes=[bsw])
        c.op("dve", lambda e: e.tensor_tensor(out=tm[:nt, 0:128], in0=tm[:nt, 0:128], in1=sw[:nt, 0:128], op=ALU.add),
             reads=[btm, bsw], writes=[btm])
        kd, bkd = kdr.next()
        gk = CF[:nt, 256 + NTI[nt]:257 + NTI[nt]]
        c.op("dve", lambda e: e.tensor_scalar(out=kd[:nt, :], in0=tm[:nt, 0:128], scalar1=gk, scalar2=None,
                                              op0=ALU.mult), reads=[btm, bCF], writes=[bkd])
        if sel_l is not None:
            sc1 = SEL[:, sel_r:sel_r + 1]
            if sel_r == 0:
                c.op("dve", lambda e: e.tensor_scalar(out=Ssel[:, sel_l, :], in0=S, scalar1=sc1, scalar2=None,
                                                      op0=ALU.mult), reads=[bS, bSEL], writes=[bSsel])
            else:
                c.op("dve", lambda e: e.scalar_tensor_tensor(out=Ssel[:, sel_l, :], in0=S, scalar=sc1,
                                                             in1=Ssel[:, sel_l, :], op0=ALU.mult, op1=ALU.add),
                     reads=[bS, bSEL, bSsel], writes=[bSsel])
        c.op("pe", lambda e: e.matmul(p7[:, 0:256], lhsT=kd[:nt, :], rhs=vb[:nt, :], start=True, stop=True),
             reads=[bkd, bvb], writes=[bp7])
        gn = CF[:, 259 + NTI[nt]:260 + NTI[nt]]
        c.op("dve", lambda e: e.scalar_tensor_tensor(out=S, in0=S, scalar=gn, in1=p7[:, 0:256],
                                                     op0=ALU.mult, op1=ALU.add),
             reads=[bS, bCF, bp7], writes=[bS])

    def own_tile(h, usrc, nt, rc_ap, rs_ap, qdst, bqd, Sb_ap, bSb_, tok0):
        uT, buT = uTr.next()
        c.dma("sp", uT[:, :, :nt], usrc, reads=[b_uts], writes=[buT], owner=buT)
        cc, bcc = ccr.next()
        sn, bsn = ssr.next()
        c.dma("sp", cc[:nt, :], rc_ap, writes=[bcc], owner=bcc)
        c.dma("sp", sn[:nt, :], rs_ap, writes=[bsn], owner=bsn)
        for (ps, bps, c0, cn) in ((pA, bpA, 256, 384), (pB, bpB, 640, 512)):
            for k in range(16):
                c.op("pe", lambda e, ps=ps, c0=c0, cn=cn, k=k: e.matmul(
                    ps[:nt, :cn], lhsT=uT[:, k, :nt], rhs=Wh[:, k, c0:c0 + cn],
                    start=(k == 0), stop=(k == 15)),
                    reads=[buT, bWh], writes=[bps], sig=(k == 15))
        qk, bqk = qkr.next()
        c.op("act", lambda e: e.copy(out=qk[:nt, 0:128], in_=pA[:nt, 0:128]), reads=[bpA], writes=[bqk])
        rf, brf = rfr.next()
        c.op("dve", lambda e: e.tensor_copy(out=rf[:nt, :], in_=pA[:nt, 128:384]), reads=[bpA], writes=[brf])
        vb, bvb = vbr.next()
        c.op("act", lambda e: e.copy(out=vb[:nt, :], in_=pB[:nt, 0:256]), reads=[bpB], writes=[bvb])
        sg, bsg = sgr.next()
        c.op("act", lambda e: e.activation(out=sg[:nt, :], in_=pB[:nt, 256:512], func=AF.Exp, scale=-1.0),
             reads=[bpB], writes=[bsg])
        c.op("dve", lambda e: e.tensor_scalar(out=sg[:nt, :], in0=sg[:nt, :], scalar1=1.0, scalar2=None,
                                              op0=ALU.add), reads=[bsg], writes=[bsg])
        c.op("dve", lambda e: e.reciprocal(out=sg[:nt, :], in_=sg[:nt, :]), reads=[bsg], writes=[bsg])
        c.op("dve", lambda e: e.tensor_tensor(out=sg[:nt, :], in0=sg[:nt, :], in1=pB[:nt, 256:512], op=ALU.mult),
             reads=[bsg, bpB], writes=[bsg])
        tm, btm = tmr.next()
        sw, bsw = swr.next()
        c.op("dve", lambda e: e.tensor_tensor(out=tm[:nt, :], in0=rf[:nt, :], in1=cc[:nt, :], op=ALU.mult),
             reads=[brf, bcc], writes=[btm])
        rf4 = rf[:nt, :].rearrange("p (a h d) -> p a h d", a=2, h=2)
        sw4 = sw[:nt, :].rearrange("p (a h d) -> p a h d", a=2, h=2)
        sn4 = sn[:nt, :].rearrange("p (a h d) -> p a h d", a=2, h=2)
        c.op("pool", lambda e: e.tensor_tensor(out=sw4[:, :, 0, :], in0=rf4[:, :, 1, :], in1=sn4[:, :, 0, :],
                                               op=ALU.mult), reads=[brf, bsn], writes=[bsw])
        c.op("pool", lambda e: e.tensor_tensor(out=sw4[:, :, 1, :], in0=rf4[:, :, 0, :], in1=sn4[:, :, 1, :],
                                               op=ALU.mult), reads=[brf, bsn], writes=[bsw])
        c.op("dve", lambda e: e.tensor_tensor(out=qk[:nt, 128:384], in0=tm[:nt, :], in1=sw[:nt, :], op=ALU.add),
             reads=[btm, bsw], writes=[bqk])
        for j in range(3):
            c.op("pe", lambda e, j=j: e.transpose(out=pR[:, j, :nt], in_=qk[:nt, j * 128:(j + 1) * 128],
                                                  identity=IDN[:nt, :nt]),
                 reads=[bqk, bCB], writes=[bpR], sig=(j == 2))
        c.op("act", lambda e: e.copy(out=qdst, in_=pR[:, 0, :nt]), reads=[bpR], writes=[bqd])
        tr, btr = trr.next()
        c.op("dve", lambda e: e.tensor_copy(out=tr[:, :, :nt], in_=pR[:, 1:3, :nt]), reads=[bpR], writes=[btr])
        c.op("pe", lambda e: e.matmul(p6[:nt, 0:nt], lhsT=tr[:, 1, :nt], rhs=tr[:, 0, :nt], start=True, stop=True),
             reads=[btr], writes=[bp6])
        sc, bsc = scr.next()
        c.op("dve", lambda e: e.tensor_tensor(out=sc[:nt, :nt], in0=p6[:nt, 0:nt], in1=CF[:nt, 0:nt], op=ALU.mult),
             reads=[bp6, bCF], writes=[bsc])
        qd, bqdc = qdr.next()
        c.op("pool", lambda e: e.tensor_tensor(out=qd[:, :nt], in0=tr[:, 0, :nt], in1=CF[:, 128:128 + nt], op=ALU.mult),
             reads=[btr, bCF], writes=[bqdc])
        c.op("pe", lambda e: e.matmul(p7[:nt, 0:256], lhsT=sc[:nt, :nt], rhs=vb[:nt, :], start=True, stop=False),
             reads=[bsc, bvb], writes=[bp7], sig=False)
        c.op("pe", lambda e: e.matmul(p7[:nt, 0:256], lhsT=qd[:, :nt], rhs=Sb_ap, start=False, stop=True),
             reads=[bqdc, bSb_], writes=[bp7])
        stt, bst = st_r.next()
        og, bog = ogr.next()
        c.op("pool", lambda e: e.memset(stt[:, :], 0.0), writes=[bst])
        c.op("act", lambda e: e.activation(out=og[:nt, :], in_=p7[:nt, 0:256], func=AF.Square,
                                           accum_out=stt[:nt, 2:3]), reads=[bp7], writes=[bog, bst])
        c.op("act", lambda e: e.activation(out=stt[:nt, 3:4], in_=stt[:nt, 2:3], func=AF.Ln,
                                           scale=1.0 / 256, bias=EPS), reads=[bst], writes=[bst])
        c.op("act", lambda e: e.activation(out=stt[:nt, 3:4], in_=stt[:nt, 3:4], func=AF.Exp,
                                           scale=-0.5), reads=[bst], writes=[bst])
        c.op("dve", lambda e: e.scalar_tensor_tensor(out=og[:nt, :], in0=p7[:nt, 0:256], scalar=stt[:nt, 3:4],
                                                     in1=sg[:nt, :], op0=ALU.mult, op1=ALU.mult),
             reads=[bp7, bst, bsg], writes=[bog])
        for j in range(2):
            c.op("pe", lambda e, j=j: e.transpose(out=pR[:, 4 + j, :nt], in_=og[:nt, j * 128:(j + 1) * 128],
                                                  identity=IDN[:nt, :nt]),
                 reads=[bog, bCB], writes=[bpR], sig=(j == 1))
        ogT, bogT = ogTr.next()
        c.op("act", lambda e: e.copy(out=ogT[:, :, :nt], in_=pR[:, 4:6, :nt]), reads=[bpR], writes=[bogT])
        c.dma("pool", oretT[2 * h:2 * h + 2, :, tok0:tok0 + nt].rearrange("c p t -> p c t"), ogT[:, :, :nt],
              reads=[bogT], owner=b_oret)

    banksZ = [(pA, bpA), (pB, bpB)]
    banksA = [(pC, bpC), (p6, bp6)]
    zi = [0]

    def attn_run(qcols, rdq, keys, ncb, cw, dst):
        NA = ncb * cw
        c.op("pool", lambda e: e.memset(Sacc[:, :NA], 0.0), writes=[bSacc])
        c.op("pool", lambda e: e.memset(Saccb[:, :], 0.0), writes=[bSaccb])
        c.op("pe", lambda e: e.matmul(p7[:, :NA], lhsT=Saccb[:, 0:128], rhs=Saccb[:, :NA], start=True, stop=False),
             reads=[bSaccb], writes=[bp7])
        started = [True] * ncb
        for idx, (ktap, bkt, vaap, bva, nk, cb0, mask) in enumerate(keys):
            last = idx == len(keys) - 1
            c0 = cb0 * cw
            N = NA - c0
            (pz, bpz) = banksZ[zi[0] % 2]
            (pa, bpa) = banksA[zi[0] % 2]
            zi[0] += 1

            def zmm(ps, bps, fin):
                c.op("pe", lambda e: e.matmul(ps[:nk, :N], lhsT=ktap, rhs=qcols[:, c0:NA], start=True,
                                              stop=(mask is None and fin)),
                     reads=[bkt] + rdq, writes=[bps], sig=(mask is None and fin))
                if mask is not None:
                    c.op("pe", lambda e: e.matmul(ps[:nk, 0:cw], lhsT=IDN[:nk, :nk], rhs=mask, start=False, stop=fin),
                         reads=[bCB, bMK], writes=[bps], sig=fin)
            zmm(pz, bpz, True)
            E, bE = Er.next()
            c.op("act", lambda e: e.activation(out=E[:nk, :N], in_=pz[:nk, :N], func=AF.Exp),
                 reads=[bpz], writes=[bE])
            SPt, bSP = SPr.next()
            c.op("act", lambda e: e.activation(out=SPt[:nk, :N], in_=E[:nk, :N], func=AF.Ln, bias=1.0),
                 reads=[bE], writes=[bSP])
            zmm(pa, bpa, False)
            c.op("pe", lambda e: e.matmul(pa[:nk, :N], lhsT=TRIN[:nk, :nk], rhs=SPt[:nk, :N],
                                          start=False, stop=(idx == 0)),
                 reads=[bCB, bSP], writes=[bpa], sig=(idx == 0))
            if idx > 0:
                c.op("pe", lambda e: e.matmul(pa[:nk, :N], lhsT=ONESN[:, :nk], rhs=Saccb[:, c0:NA],
                                              start=False, stop=True),
                     reads=[bCB, bSaccb], writes=[bpa])
            if not last:
                c.op("dve", lambda e: e.tensor_tensor(out=Sacc[:nk, c0:NA], in0=Sacc[:nk, c0:NA],
                                                      in1=SPt[:nk, :N], op=ALU.add),
                     reads=[bSacc, bSP], writes=[bSacc])
                c.op("dve", lambda e: e.tensor_copy(out=Saccb[:, c0:NA], in_=Sacc[:, c0:NA]),
                     reads=[bSacc], writes=[bSaccb])
            AT, bAT = ATr.next()
            c.op("act", lambda e: e.activation(out=AT[:nk, :N], in_=pa[:nk, :N], func=AF.Exp),
                 reads=[bpa], writes=[bAT])
            for cb in range(cb0, ncb):
                a0 = (cb - cb0) * cw
                c.op("pe", lambda e, cb=cb, a0=a0: e.matmul(
                    p7[:, cb * cw:(cb + 1) * cw], lhsT=vaap, rhs=AT[:nk, a0:a0 + cw],
                    start=(not started[cb]), stop=last),
                    reads=[bAT, bva], writes=[bp7], sig=(cb == ncb - 1))
                started[cb] = True
        oso, boso = osr.next()
        c.op("dve", lambda e: e.tensor_copy(out=oso[:, :NA], in_=p7[:, :NA]), reads=[bp7], writes=[boso])
        c.dma("pool", dst, oso[:, :NA], reads=[boso], owner=b_osb)

    for h in range(NH):
        load_head(h)
        hc = slice(h * 128, (h + 1) * 128)
        c.op("pool", lambda e: e.memset(S, 0.0), writes=[bS])
        scan_tile(0, NMETA, 0, kp[0:NMETA, hc], vp[0:NMETA, hc], 0)
        for i in range(NXT):
            r0 = NMETA + i * 128
            scan_tile(1 + i, 128, r0, kp[r0:r0 + 128, hc], vp[r0:r0 + 128, hc], 1 + i, sel_l=i // 4, sel_r=i % 4)
        c.dma("pool", sp_o[h], S, reads=[bS], owner=bS, is_output=True)
        for l in range(NL):
            own_tile(h, uto[:, :, l * 128:(l + 1) * 128], 128, ropeoc[l * 128:(l + 1) * 128, :],
                     ropeos[l * 128:(l + 1) * 128, :], QT[:, l * 128:(l + 1) * 128], bQT[l],
                     Ssel[:, l, :], bSsel, l * 128)
        for l0 in range(0, NL, 4):
            l1 = min(l0 + 4, NL)
            keys = []
            for kt in range(4 * (l1 - 1) + 3, -1, -1):
                lp, r = kt // 4, kt % 4
                blk = 1 + kt
                keys.append((KT[:, blk * 128:(blk + 1) * 128], bKT[blk], VA[:, blk, :], bVA[blk], 128,
                             max(lp - l0, 0), MK[:, r, :] if lp >= l0 else None))
            keys.append((KT[:, 0:NMETA], bKT[0], VA[:NMETA, 0, :], bVA[0], NMETA, 0, None))
            attn_run(QT[:, l0 * 128:l1 * 128], [bQT[i] for i in range(l0, l1)], keys, l1 - l0, 128,
                     osbT[h, :, l0 * 128:l1 * 128])
        for s in range(NS):
            tok0 = NL * 128 + s * DEC_T
            c.dma("pool", VA[:, 0:16, :], cv[s, :, hc].rearrange("(a p) d -> p a d", p=128),
                  writes=[bVA[i] for i in range(16)], owner=bVA[0])
            c.dma("pool", ckst, ck[s, :, hc].rearrange("(a p) d -> p a d", p=128), writes=[bckst], owner=bckst)
            for j in range(16):
                c.op("pe", lambda e, j=j: e.transpose(out=pT[:, j, :], in_=ckst[:, j, :], identity=IDN),
                     reads=[bckst, bCB], writes=[bpT], sig=(j == 15))
            c.op("act", lambda e: e.mul(out=KT[:, 0:2048], in_=pT[:].rearrange("p a d -> p (a d)"), mul=128.0 ** -0.5),
                 reads=[bpT], writes=[bKT[i] for i in range(16)])
            c.dma("sp", S, st[s, h], writes=[bS], owner=bS)
            c.op("pool", lambda e: e.tensor_copy(out=Sb, in_=S), reads=[bS], writes=[bSb])
            own_tile(h, uto[:, :, tok0:tok0 + DEC_T], DEC_T, ropesmc[:, :], ropesms[:, :],
                     QT[:, 0:DEC_T], bQT[0], Sb, bSb, tok0)
            scan_tile(1 + NXT + s, DEC_T, ntp, ks[s, :, hc], vs[s, :, hc], 16)
            c.dma("pool", ss_o[s, h], S, reads=[bS], owner=bS, is_output=True)
            keys = [(KT[:, 2048:2048 + DEC_T], bKT[16], VA[:DEC_T, 16, :], bVA[16], DEC_T, 0, NEGM[:DEC_T, :DEC_T])]
            for kb in range(15, -1, -1):
                keys.append((KT[:, kb * 128:(kb + 1) * 128], bKT[kb], VA[:, kb, :], bVA[kb], 128, 0, None))
            attn_run(QT[:, 0:DEC_T], [bQT[0]], keys, 1, DEC_T, osbT[h, :, tok0:tok0 + DEC_T])
    b_osb.w = (b_osb.dsem, b_osb.dcnt)
    b_oret.w = (b_oret.dsem, b_oret.dcnt)

    hs = dt("hs", [NTOK, D], F32, kind=SK).ap()
    b_hs = Buf("hs")
    c.barrier()
    AFa.reset(); ABa.reset()
    CB2 = ABa.get(512); IDN = CB2[:, 0:128]
    GC = 768
    xcr = ring(AFa, 512, 2, "xc")
    gsr = ring(AFa, 512, 4, "gs")
    hcr = ring(AFa, 512, 3, "hc")
    slots = ring(ABa, 8192, 3, "slot")
    actU = ABa.get(16 * GC, GC); bactU = Buf("actU")
    actS = ABa.get(8 * GC, GC); bactS = Buf("actS")
    actR = ABa.get(16 * GC, GC); bactR = Buf("actR")
    MT = ABa.get(16 * GC, GC); bMT = Buf("MT")

    for t0 in range(0, NTOK, GC):
        T = min(GC, NTOK - t0)
        ntl = T // 128
        c.dma("sp", actU[:, :, :T], uto[:, :, t0:t0 + T], reads=[b_uts], writes=[bactU], owner=bactU)
        c.dma("sp", actS[:, :NH, :T], osbT[:, :, t0:t0 + T].rearrange("h p t -> p h t"), reads=[b_osb],
              writes=[bactS], owner=bactS)
        c.dma("sp", actR[:, :2 * NH, :T], oretT[:, :, t0:t0 + T].rearrange("h p t -> p h t"), reads=[b_oret],
              writes=[bactR], owner=bactR)
        for fc in range(16):
            sl, bsl = slots.next()
            fcs = slice(fc * 128, (fc + 1) * 128)
            w1 = sl[:, 0:2048].rearrange("p (a b) -> p a b", b=128)
            w2 = sl[:, 2048:4096].rearrange("p (a b) -> p a b", b=128)
            w3 = sl[:, 4096:4096 + NH * 128].rearrange("p (a b) -> p a b", b=128)
            w4 = sl[:, 6144:6144 + 2 * NH * 128].rearrange("p (a b) -> p a b", b=128)
            c.dma("pool", w1, wg[:, fc * 128:(fc + 1) * 128].rearrange("(k p) n -> p k n", p=128), writes=[bsl], owner=bsl)
            c.dma("pool", w2, wg[:, D + fc * 128:D + (fc + 1) * 128].rearrange("(k p) n -> p k n", p=128), writes=[bsl], owner=bsl)
            c.dma("pool", w3, wsbo[:, fcs].rearrange("(k p) n -> p k n", p=128), writes=[bsl], owner=bsl)
            c.dma("pool", w4, wreto[:, fcs].rearrange("(k p) n -> p k n", p=128), writes=[bsl], owner=bsl)
            for (o, n) in [(o_, min(512, T - o_)) for o_ in range(0, T, 512)]:
                for (ps, bps, w, act, bact, nk_) in ((pA, bpA, w1, actU, bactU, 16), (pB, bpB, w2, actU, bactU, 16),
                                                     (pC, bpC, w3, actS, bactS, NH), (p6, bp6, w4, actR, bactR, 2 * NH)):
                    for k in range(nk_):
                        c.op("pe", lambda e, ps=ps, w=w, act=act, k=k, nk_=nk_: e.matmul(
                            ps[:, :n], lhsT=w[:, k, :], rhs=act[:, k, o:o + n], start=(k == 0), stop=(k == nk_ - 1)),
                            reads=[bsl, bact], writes=[bps], sig=(k == nk_ - 1))
                ga, bga = gsr.next()
                gb, bgb = gsr.next()
                c.op("act", lambda e: e.activation(out=ga[:, :n], in_=pA[:, :n], func=AF.Sigmoid), reads=[bpA], writes=[bga])
                c.op("act", lambda e: e.activation(out=gb[:, :n], in_=pB[:, :n], func=AF.Sigmoid), reads=[bpB], writes=[bgb])
                c.op("dve", lambda e: e.tensor_tensor(out=ga[:, :n], in0=ga[:, :n], in1=pC[:, :n], op=ALU.mult),
                     reads=[bga, bpC], writes=[bga])
                c.op("dve", lambda e: e.tensor_tensor(out=gb[:, :n], in0=gb[:, :n], in1=p6[:, :n], op=ALU.mult),
                     reads=[bgb, bp6], writes=[bgb])
                c.op("dve", lambda e, fc=fc: e.tensor_tensor(out=MT[:, fc, o:o + n], in0=ga[:, :n], in1=gb[:, :n], op=ALU.add),
                     reads=[bga, bgb], writes=[bMT])
        for oc in range(4):
            sl, bsl = slots.next()
            wo = sl[:, 0:8192].rearrange("p (a b) -> p a b", b=512)
            for k4 in range(4):
                c.dma("pool", wo[:, 4 * k4:4 * k4 + 4, :],
                      wout[512 * k4:512 * (k4 + 1), oc * 512:(oc + 1) * 512].rearrange("(k p) n -> p k n", p=128),
                      writes=[bsl], owner=bsl)
            for ti in range(ntl):
                xc, bxc = xcr.next()
                c.dma("sp", xc, xtok[t0 + ti * 128:t0 + (ti + 1) * 128, oc * 512:(oc + 1) * 512], writes=[bxc], owner=bxc)
                (ps, bps) = ((pA, bpA), (pB, bpB))[(oc * ntl + ti) % 2]
                for k in range(16):
                    c.op("pe", lambda e, k=k, ti=ti, ps=ps: e.matmul(ps[:, :], lhsT=MT[:, k, ti * 128:(ti + 1) * 128], rhs=wo[:, k, :],
                                                                     start=(k == 0), stop=(k == 15)),
                         reads=[bMT, bsl], writes=[bps], sig=(k == 15))
                hc_, bhc = hcr.next()
                c.op("dve", lambda e, ps=ps: e.tensor_tensor(out=hc_, in0=ps[:, :], in1=xc, op=ALU.add),
                     reads=[bps, bxc], writes=[bhc])
                c.dma("pool", hs[t0 + ti * 128:t0 + (ti + 1) * 128, oc * 512:(oc + 1) * 512], hc_, reads=[bhc], owner=b_hs,
                      is_output=dbg)
    b_hs.w = (b_hs.dsem, b_hs.dcnt)

    c.barrier()
    AFa.reset(); ABa.reset()
    CB2 = ABa.get(512); IDN = CB2[:, 0:128]
    NTG = GT // 128
    gv = AFa.get(D); bgv = Buf("gv")
    brc = AFa.get(20); bbr = Buf("brc")
    c.dma("sp", brc, br.partition_broadcast(128), writes=[bbr], owner=bbr)
    wrf = AFa.get(320, 20); bwrf = Buf("wrf")
    c.dma("sp", wrf, wr.rearrange("(k p) n -> p k n", p=128), writes=[bwrf], owner=bwrf)
    wrh = ABa.get(320, 20); bwrh = Buf("wrh")
    wrl = ABa.get(320, 20); bwrl = Buf("wrl")
    c.op("act", lambda e: e.copy(out=wrh, in_=wrf), reads=[bwrf], writes=[bwrh])
    c.op("dve", lambda e: e.tensor_tensor(out=wrl, in0=wrf, in1=wrh, op=ALU.subtract), reads=[bwrf, bwrh], writes=[bwrl])
    H = AFa.get(NTG * D, D); bH = [Buf(f"H{i}") for i in range(NTG)]
    Cmb = AFa.get(NTG * 16, 16); bCmb = Buf("Cmb")
    gsr = ring(AFa, 512, 2, "gs2")
    rt = ring(AFa, 64, 2, "rt")
    st2 = ring(AFa, 8, 2, "st2")
    u2f = AFa.get(D); bu2f = Buf("u2f")
    slots = ring(ABa, 8192, 3, "eslot")
    actU = ABa.get(16 * GT, GT); bactU = Buf("u2T")
    u2h = ABa.get(D); bu2h = Buf("u2h")
    u2l = ABa.get(D); bu2l = Buf("u2l")
    loT = ABa.get(D, 128); bloT = Buf("loT")
    hT = ABa.get(4 * GT, GT); bhT = Buf("hT")

    def subgroups(T):
        out, o = [], 0
        while o < T:
            n = min(512, T - o)
            out.append((o, n))
            o += n
        return out

    def rms_tile(ti, gain_buf):
        stt, bst = st2.next()
        c.op("pool", lambda e: e.memset(stt, 0.0), writes=[bst])
        c.op("act", lambda e: e.activation(out=u2f, in_=H[:, ti, :], func=AF.Square, accum_out=stt[:, 0:1]),
             reads=[bH[ti]], writes=[bu2f, bst])
        c.op("act", lambda e: e.activation(out=stt[:, 1:2], in_=stt[:, 0:1], func=AF.Ln, scale=1.0 / D, bias=EPS),
             reads=[bst], writes=[bst])
        c.op("act", lambda e: e.activation(out=stt[:, 1:2], in_=stt[:, 1:2], func=AF.Exp, scale=-0.5),
             reads=[bst], writes=[bst])
        c.op("dve", lambda e: e.scalar_tensor_tensor(out=u2f, in0=H[:, ti, :], scalar=stt[:, 1:2], in1=gv,
                                                     op0=ALU.mult, op1=ALU.mult),
             reads=[bH[ti], bst, bgv], writes=[bu2f])

    for t0 in range(0, NTOK, GT):
        T = min(GT, NTOK - t0)
        ntl = T // 128
        for ti in range(ntl):
            c.dma("sp", H[:, ti, :], hs[t0 + ti * 128:t0 + (ti + 1) * 128, :], reads=[b_hs], writes=[bH[ti]], owner=bH[ti])
        c.dma("sp", gv, g2.partition_broadcast(128), writes=[bgv], owner=bgv)
        for ti in range(ntl):
            rms_tile(ti, gv)
            c.op("act", lambda e: e.copy(out=u2h, in_=u2f), reads=[bu2f], writes=[bu2h])
            c.op("dve", lambda e: e.tensor_tensor(out=u2l, in0=u2f, in1=u2h, op=ALU.subtract),
                 reads=[bu2f, bu2h], writes=[bu2l])
            for j in range(16):
                c.op("pe", lambda e, j=j: e.transpose(out=pT[:, j, :], in_=u2h[:, j * 128:(j + 1) * 128], identity=IDN),
                     reads=[bu2h], writes=[bpT], sig=(j == 15))
            c.op("act", lambda e, ti=ti: e.copy(out=actU[:, :, ti * 128:(ti + 1) * 128], in_=pT[:, :, :]),
                 reads=[bpT], writes=[bactU])
            for j in range(16):
                c.op("pe", lambda e, j=j: e.transpose(out=pT[:, j, :], in_=u2l[:, j * 128:(j + 1) * 128], identity=IDN),
                     reads=[bu2l], writes=[bpT], sig=(j == 15))
            c.op("act", lambda e: e.copy(out=loT, in_=pT[:, :, :]), reads=[bpT], writes=[bloT])
            mm = []
            for k in range(16):
                mm.append((actU[:, k, ti * 128:(ti + 1) * 128], wrh[:, k, :]))
            for k in range(16):
                mm.append((loT[:, k, :], wrh[:, k, :]))
            for k in range(16):
                mm.append((actU[:, k, ti * 128:(ti + 1) * 128], wrl[:, k, :]))
            for i_, (l_, r_) in enumerate(mm):
                c.op("pe", lambda e, l_=l_, r_=r_, i_=i_: e.matmul(pB[:, 0:20], lhsT=l_, rhs=r_, start=(i_ == 0), stop=(i_ == 47)),
                     reads=[bactU, bloT, bwrh, bwrl], writes=[bpB], sig=(i_ == 47))
            R_, bR = rt.next()
            L = R_[:, 0:20]
            c.op("dve", lambda e: e.tensor_tensor(out=L, in0=pB[:, 0:20], in1=brc, op=ALU.add), reads=[bpB, bbr], writes=[bR])
            gmax, gsum, m1, m2 = R_[:, 20:21], R_[:, 21:22], R_[:, 22:23], R_[:, 23:24]
            oh, pen = R_[:, 24:28], R_[:, 28:32]
            elm, mk1 = R_[:, 32:48], R_[:, 48:64]
            R2, bR2 = rt.next()
            elm2, mk2, ge = R2[:, 0:16], R2[:, 16:32], R2[:, 32:36]
            w1_, w2_, e2 = R2[:, 36:37], R2[:, 37:38], R2[:, 38:39]
            rb = [bR, bR2]
            V = lambda fn: c.op("dve", fn, reads=rb, writes=rb)
            V(lambda e: e.tensor_reduce(out=gmax, in_=L[:, 0:4], op=ALU.max, axis=AX.X))
            V(lambda e: e.tensor_scalar(out=oh, in0=L[:, 0:4], scalar1=gmax, scalar2=None, op0=ALU.is_ge))
            V(lambda e: e.tensor_scalar(out=ge, in0=L[:, 0:4], scalar1=gmax, scalar2=None, op0=ALU.subtract))
            c.op("act", lambda e: e.activation(out=ge, in_=ge, func=AF.Exp), reads=rb, writes=rb)
            V(lambda e: e.tensor_reduce(out=gsum, in_=ge, op=ALU.add, axis=AX.X))
            V(lambda e: e.reciprocal(out=gsum, in_=gsum))
            V(lambda e: e.tensor_scalar(out=pen, in0=oh, scalar1=-1.0, scalar2=1e9, op0=ALU.add, op1=ALU.mult))
            for gi in range(4):
                V(lambda e, gi=gi: e.tensor_scalar(out=elm[:, gi * 4:gi * 4 + 4], in0=L[:, 4 + gi * 4:8 + gi * 4],
                                                   scalar1=pen[:, gi:gi + 1], scalar2=None, op0=ALU.add))
            V(lambda e: e.tensor_reduce(out=m1, in_=elm, op=ALU.max, axis=AX.X))
            V(lambda e: e.tensor_scalar(out=mk1, in0=elm, scalar1=m1, scalar2=None, op0=ALU.is_ge))
            V(lambda e: e.scalar_tensor_tensor(out=elm2, in0=mk1, scalar=-1e9, in1=elm, op0=ALU.mult, op1=ALU.add))
            V(lambda e: e.tensor_reduce(out=m2, in_=elm2, op=ALU.max, axis=AX.X))
            V(lambda e: e.tensor_scalar(out=mk2, in0=elm2, scalar1=m2, scalar2=None, op0=ALU.is_ge))
            V(lambda e: e.tensor_tensor(out=e2, in0=m2, in1=m1, op=ALU.subtract))
            c.op("act", lambda e: e.activation(out=e2, in_=e2, func=AF.Exp), reads=rb, writes=rb)
            V(lambda e: e.tensor_scalar(out=w1_, in0=e2, scalar1=1.0, scalar2=None, op0=ALU.add))
            V(lambda e: e.reciprocal(out=w1_, in_=w1_))
            V(lambda e: e.tensor_tensor(out=w1_, in0=w1_, in1=gsum, op=ALU.mult))
            V(lambda e: e.tensor_tensor(out=w2_, in0=w1_, in1=e2, op=ALU.mult))
            V(lambda e: e.tensor_scalar(out=mk1, in0=mk1, scalar1=w1_, scalar2=None, op0=ALU.mult))
            c.op("dve", lambda e, ti=ti: e.scalar_tensor_tensor(out=Cmb[:, ti, :], in0=mk2, scalar=w2_, in1=mk1,
                                                                op0=ALU.mult, op1=ALU.add), reads=rb, writes=rb + [bCmb])
        for ex in range(16):
            wge_, bwg = slots.next()
            wue_, bwu = slots.next()
            wde_, bwd = slots.next()
            wge = wge_.rearrange("p (a b) -> p a b", b=512)
            wue = wue_.rearrange("p (a b) -> p a b", b=512)
            wde = wde_.rearrange("p (a b) -> p a b", b=D)
            for k4 in range(4):
                c.dma("pool", wge[:, 4 * k4:4 * k4 + 4, :], wgate[ex, 512 * k4:512 * (k4 + 1), :].rearrange("(k p) n -> p k n", p=128),
                      writes=[bwg], owner=bwg)
            for k4 in range(4):
                c.dma("pool", wue[:, 4 * k4:4 * k4 + 4, :], wup[ex, 512 * k4:512 * (k4 + 1), :].rearrange("(k p) n -> p k n", p=128),
                      writes=[bwu], owner=bwu)
            c.dma("pool", wde, wdown[ex].rearrange("(k p) n -> p k n", p=128), writes=[bwd], owner=bwd)
            for (o, n) in subgroups(T):
                for fx in range(4):
                    for (ps, bps, w, bw) in ((pA, bpA, wge, bwg), (pB, bpB, wue, bwu)):
                        for k in range(16):
                            c.op("pe", lambda e, ps=ps, w=w, k=k, fx=fx: e.matmul(
                                ps[:, :n], lhsT=w[:, k, fx * 128:(fx + 1) * 128], rhs=actU[:, k, o:o + n],
                                start=(k == 0), stop=(k == 15)),
                                reads=[bw, bactU], writes=[bps], sig=(k == 15))
                    ga, bga = gsr.next()
                    c.op("act", lambda e: e.activation(out=ga[:, :n], in_=pA[:, :n], func=AF.Sigmoid), reads=[bpA], writes=[bga])
                    c.op("dve", lambda e: e.tensor_tensor(out=ga[:, :n], in0=ga[:, :n], in1=pA[:, :n], op=ALU.mult),
                         reads=[bga, bpA], writes=[bga])
                    c.op("dve", lambda e, fx=fx: e.tensor_tensor(out=hT[:, fx, o:o + n], in0=ga[:, :n], in1=pB[:, :n], op=ALU.mult),
                         reads=[bga, bpB], writes=[bhT])
            for ti in range(ntl):
                for oc in range(4):
                    (ps, bps) = ((pC, bpC), (p6, bp6))[(ti * 4 + oc) % 2]
                    for fx in range(4):
                        c.op("pe", lambda e, ps=ps, fx=fx, ti=ti, oc=oc: e.matmul(
                            ps[:, :], lhsT=hT[:, fx, ti * 128:(ti + 1) * 128], rhs=wde[:, fx, oc * 512:(oc + 1) * 512],
                            start=(fx == 0), stop=(fx == 3)),
                            reads=[bhT, bwd], writes=[bps], sig=(fx == 3))
                    c.op("dve", lambda e, ps=ps, ti=ti, oc=oc, ex=ex: e.scalar_tensor_tensor(
                        out=H[:, ti, oc * 512:(oc + 1) * 512], in0=ps[:, :], scalar=Cmb[:, ti, ex:ex + 1],
                        in1=H[:, ti, oc * 512:(oc + 1) * 512], op0=ALU.mult, op1=ALU.add),
                        reads=[bps, bCmb, bH[ti]], writes=[bH[ti]])
        c.dma("sp", gv, gf.partition_broadcast(128), writes=[bgv], owner=bgv)
        for ti in range(ntl):
            rms_tile(ti, gv)
            c.dma("sp", yo[t0 + ti * 128:t0 + (ti + 1) * 128, :], u2f, reads=[bu2f], owner=bu2f, is_output=True)
    c.finish()
    return c


def head_consts(h):
    lg = np.log1p(-np.float32(2.0) ** np.float32(-5.0 - h)).astype(np.float32)
    t = np.arange(128, dtype=np.float32)
    rel = t[None, :] - t[:, None]
    dmt = np.where(rel >= 0, np.exp(np.maximum(rel, 0) * lg), 0.0).astype(np.float32)
    gq = np.broadcast_to(np.exp((t + 1.0) * lg)[None, :], (128, 128)).astype(np.float32)
    cf = np.zeros((128, 262), np.float32)
    cf[:, 0:128] = dmt
    cf[:, 128:256] = gq
    for i, nt in enumerate((128, 64, 16)):
        v = np.zeros(128, np.float32)
        v[:nt] = np.exp((nt - 1.0 - t[:nt]) * lg)
        cf[:, 256 + i] = v
        cf[:, 259 + i] = np.exp(np.float32(nt) * lg)
    return cf


def bf_consts():
    cb = np.zeros((128, 512), np.float32)
    i = np.arange(128)
    cb[:, 0:128] = np.eye(128)
    cb[:, 128:256] = np.where(i[:, None] >= i[None, :], NEG, 0.0)
    cb[:, 256:384] = np.where(i[:, None] >= i[None, :], -1.0, 0.0)
    cb[:, 384:512] = -1.0
    return cb


def rope_tables(ntp):
    pos = np.concatenate([np.arange(ntp, dtype=np.float32) - NMETA, PAST + np.arange(DEC_T, dtype=np.float32)])
    inv = (1.0 / (np.float32(10000.0) ** (np.arange(0, 128, 2, dtype=np.float32) / np.float32(128)))).astype(np.float32)
    ang = (pos[:, None] * inv[None, :]).astype(np.float32)
    cs, sn = np.cos(ang).astype(np.float32), np.sin(ang).astype(np.float32)
    s = np.float32(128.0 ** -0.5)
    cc = np.concatenate([cs, cs, cs * s, cs * s], axis=1).astype(np.float32)
    ss = np.concatenate([-sn, sn, -sn * s, sn * s], axis=1).astype(np.float32)
    return np.ascontiguousarray(cc), np.ascontiguousarray(ss)


def wh_cols(h):
    def r(o, w):
        return np.arange(o + h * w, o + (h + 1) * w)
    return np.concatenate([r(1024, 128), r(2048, 128), r(0, 128), r(3072, 128), r(4096, 128),
                           r(5120, 256), r(7168, 256)])


def make_maps(inp, NXT=64, NH=8, NS=NSC, cores=range(8)):
    ntp = NMETA + NXT * 128
    NL = NXT // 4
    cc, ss = rope_tables(ntp)
    cb = bf_consts()
    cf = np.stack([head_consts(h) for h in range(NH)], 0)
    w_in = inp["w_in"][0]
    wh = np.stack([w_in[:, wh_cols(h)] for h in range(NH)], 0)
    c_ = np.ascontiguousarray
    shared = {
        "meta": c_(inp["meta"]), "g1": c_(inp["norm1_g"][0]), "g2": c_(inp["norm2_g"][0]), "gf": c_(inp["normf_g"]),
        "wh": wh, "cstf": cf, "cstb": cb, "ropec": c_(cc[:, 128:256]), "ropes": c_(ss[:, 128:256]),
        "ropesmc": c_(cc[ntp:ntp + DEC_T]), "ropesms": c_(ss[ntp:ntp + DEC_T]),
        "wg": c_(w_in[:, 9216:13312]), "wsbo": c_(inp["w_sb_o"][0][:NH * 128]), "wreto": c_(inp["w_ret_o"][0][:NH * 256]),
        "wout": c_(inp["w_out"][0]),
        "wr": c_(np.concatenate([inp["w_grp"][0], inp["w_exp"][0]], axis=1)),
        "br": c_(np.concatenate([inp["b_grp"][0], inp["b_exp"][0]], axis=0)),
        "wgate": c_(inp["w_gate"][0]), "wup": c_(inp["w_up"][0]), "wdown": c_(inp["w_down"][0]),
    }
    maps = []
    ii = np.arange(128)
    for cid in cores:
        b, j = cid // 4, cid % 4
        tiles = [4 * l + j for l in range(NL)]
        xp = inp["x_prompt"][b]
        xtok = np.concatenate([xp[t * 128:(t + 1) * 128] for t in tiles]
                              + [inp["x_sample"][NSC * cid + s] for s in range(NS)], axis=0)
        rows = np.concatenate([NMETA + t * 128 + ii for t in tiles]) if NL else np.zeros((0,), np.int64)
        mk = np.zeros((128, 4, 128), np.float32)
        for r in range(4):
            if r == j:
                mk[:, r, :] = np.where(ii[:, None] >= ii[None, :], NEG, 0.0)
            elif r > j:
                mk[:, r, :] = NEG
        sel = np.zeros((128, 4), np.float32)
        sel[:, j] = 1.0
        m = dict(shared)
        m.update({
            "xall": c_(xp[:NXT * 128]), "xtok": c_(xtok),
            "st": c_(inp["state_ret"][0, NSC * cid:NSC * cid + NS, :NH]),
            "ropeoc": c_(cc[rows]), "ropeos": c_(ss[rows]),
            "msk": c_(mk.reshape(128, 512)), "sel": sel,
            "ck": c_(inp["cache_sb_k"][0, NSC * cid:NSC * cid + NS, :, :NH].reshape(NS, PAST, NH * 128)),
            "cv": c_(inp["cache_sb_v"][0, NSC * cid:NSC * cid + NS, :, :NH].reshape(NS, PAST, NH * 128)),
        })
        maps.append(m)
    return maps


def kernel(**inputs):
    inp = {k: np.asarray(v) for k, v in inputs.items()}
    nc = bass.Bass("TRN2", target_bir_lowering=False)
    build(nc)
    maps = make_maps(inp)
    res = run_bass_kernel_spmd(nc, maps, core_ids=list(range(8)))
    R = res.results
    NL = 16
    y_p = np.zeros((2, SEQ, D), np.float32)
    y_s = np.zeros((DEC_B, DEC_T, D), np.float32)
    for cid in range(8):
        b, j = cid // 4, cid % 4
        yo = np.asarray(R[cid]["yo"])
        for l in range(NL):
            t = 4 * l + j
            y_p[b, t * 128:(t + 1) * 128] = yo[l * 128:(l + 1) * 128]
        for s in range(NSC):
            y_s[NSC * cid + s] = yo[NL * 128 + s * DEC_T:NL * 128 + (s + 1) * DEC_T]
    kp = np.stack([np.asarray(R[4 * b]["kp"]).reshape(TP, 8, 128) for b in range(2)], 0)[None]
    vp = np.stack([np.asarray(R[4 * b]["vp"]).reshape(TP, 8, 128) for b in range(2)], 0)[None]
    sp = np.stack([np.asarray(R[4 * b]["sp_o"]) for b in range(2)], 0)[None]
    ks = np.concatenate([np.asarray(R[c]["ks"]).reshape(NSC, DEC_T, 8, 128) for c in range(8)], 0)[None]
    vs = np.concatenate([np.asarray(R[c]["vs"]).reshape(NSC, DEC_T, 8, 128) for c in range(8)], 0)[None]
    ss = np.concatenate([np.asarray(R[c]["ss_o"]) for c in range(8)], 0)[None]
    f = lambda a: np.ascontiguousarray(a, dtype=np.float32)
    return (y_p, y_s, f(kp), f(vp), f(sp), f(ks), f(vs), f(ss))
```

```python
import numpy as np
import concourse.bass as bass
import concourse.mybir as mybir
from concourse.bass_utils import run_bass_kernel_spmd

F32 = mybir.dt.float32
BF16 = mybir.dt.bfloat16
AF = mybir.ActivationFunctionType
ALU = mybir.AluOpType

D = 2048
SEQ = 8192
NMETA = 16
TP = SEQ + NMETA
DEC_B = 32
DEC_T = 64
PAST = 2048
NSC = 4
EPS = 1e-6
NEG = -30000.0
SEM_LIMIT = 30000
WKV = 640


class Buf:
    __slots__ = ("name", "w", "r", "dsem", "dcnt", "excl")

    def __init__(self, name, excl=False):
        self.name = name
        self.excl = excl
        self.w = None
        self.r = {}
        self.dsem = None
        self.dcnt = 0


class Ctx:
    def __init__(self, nc):
        self.nc = nc
        self.eng = {"pe": nc.tensor, "act": nc.scalar, "dve": nc.vector,
                    "pool": nc.gpsimd, "sp": nc.sync}
        self.sem = {}
        self.cnt = {}
        self.waited = {}
        self.pend_r = {}
        self.pend_w = {}
        self.nsem = 0
        for k in self.eng:
            self.sem[k] = self._newsem("e_" + k)
            self.cnt[k] = 0
            self.waited[k] = {}
            self.pend_r[k] = []
            self.pend_w[k] = []
        self.out_tokens = {}
        self.dtoks = {}
        self.ninst = 0

    def _newsem(self, name):
        self.nsem += 1
        return self.nc.alloc_semaphore(f"{name}_{self.nsem}")

    def _wait(self, en, tok):
        if tok is None:
            return
        sem, val = tok
        w = self.waited[en]
        if w.get(sem.num, 0) >= val:
            return
        self.eng[en].wait_ge(sem, val)
        w[sem.num] = val

    def _deps(self, en, reads, writes):
        skip = self.sem[en].num if en == "pe" else None
        for b in reads:
            if b.w is not None and b.w[0].num != skip:
                self._wait(en, b.w)
        for b in writes:
            if b.w is not None and b.w[0].num != skip:
                self._wait(en, b.w)
            for t in b.r.values():
                if t[0].num != skip:
                    self._wait(en, t)

    def _commit(self, tok, reads, writes):
        sem, val = tok
        for b in reads:
            b.r[sem.num] = tok
        for b in writes:
            b.w = tok
            b.r = {}

    def op(self, en, fn, reads=(), writes=(), sig=True):
        ex = [b for b in reads if b.excl]
        if ex:
            reads = [b for b in reads if not b.excl]
            writes = list(writes) + ex
        self._deps(en, reads, writes)
        ins = fn(self.eng[en])
        self.ninst += 1
        if not sig:
            self.pend_r[en].extend(reads)
            self.pend_w[en].extend(writes)
            return None
        if self.cnt[en] >= SEM_LIMIT:
            self.sem[en] = self._newsem("e_" + en)
            self.cnt[en] = 0
        self.cnt[en] += 1
        ins.then_inc(self.sem[en], 1)
        tok = (self.sem[en], self.cnt[en])
        self._commit(tok, list(reads) + self.pend_r[en], list(writes) + self.pend_w[en])
        self.pend_r[en] = []
        self.pend_w[en] = []
        return tok

    def dma(self, q, out, in_, reads=(), writes=(), owner=None, is_output=False):
        self._deps(q, reads, writes)
        ins = self.eng[q].dma_start(out=out, in_=in_)
        self.ninst += 1
        if owner.dsem is None or owner.dcnt >= SEM_LIMIT:
            owner.dsem = self._newsem("d_" + owner.name)
            owner.dcnt = 0
        owner.dcnt += 16
        ins.then_inc(owner.dsem, 16)
        tok = (owner.dsem, owner.dcnt)
        self.dtoks[owner.dsem.num] = tok
        self._commit(tok, reads, writes)
        if is_output:
            self.out_tokens[owner.dsem.num] = tok
        return tok

    def barrier(self):
        toks = [(self.sem[k], self.cnt[k]) for k in self.eng if self.cnt[k] > 0] + list(self.dtoks.values())
        for en in self.eng:
            for t in toks:
                if t[0] is self.sem[en]:
                    continue
                self._wait(en, t)

    def finish(self, en="sp"):
        for tok in self.out_tokens.values():
            self._wait(en, tok)


AX = mybir.AxisListType
WALL = 1152
GT = 768


class Ring:
    def __init__(self, aps, name):
        self.t = list(aps)
        self.b = [Buf(f"{name}{i}") for i in range(len(aps))]
        self.i = 0

    def next(self):
        t, b = self.t[self.i], self.b[self.i]
        self.i = (self.i + 1) % len(self.t)
        return t, b


class _Pool:
    def __init__(self, t, size):
        self.t, self.size, self.off = t, size, 0


class Arena:
    def __init__(self, pool, f32):
        self.p, self.f32 = pool, f32

    def reset(self):
        self.p.off = 0

    def get(self, n, b=None):
        p = self.p
        if self.f32:
            p.off += p.off % 2
            a = p.t[:, p.off:p.off + 2 * n].bitcast(F32)
            p.off += 2 * n
        else:
            a = p.t[:, p.off:p.off + n]
            p.off += n
        assert p.off <= p.size, (p.off, p.size)
        if b is not None:
            a = a.rearrange("p (a b) -> p a b", b=b)
        return a


def build(nc, NXT=64, NH=8, NS=NSC, dbg=False):
    c = Ctx(nc)
    dt = nc.dram_tensor
    NL = NXT // 4
    NTOK = NL * 128 + NS * DEC_T
    assert NTOK % 128 == 0
    ntp = NMETA + NXT * 128
    NBLK = max(1 + NXT, 17)
    I = "ExternalInput"
    xall = dt("xall", [NXT * 128, D], F32, kind=I).ap()
    meta = dt("meta", [NMETA, D], F32, kind=I).ap()
    xtok = dt("xtok", [NTOK, D], F32, kind=I).ap()
    g1 = dt("g1", [D], F32, kind=I).ap()
    g2 = dt("g2", [D], F32, kind=I).ap()
    gf = dt("gf", [D], F32, kind=I).ap()
    wh = dt("wh", [NH, D, WALL], F32, kind=I).ap()
    st = dt("st", [NS, NH, 128, 256], F32, kind=I).ap()
    cstf = dt("cstf", [NH, 128, 262], F32, kind=I).ap()
    cstb = dt("cstb", [128, 512], F32, kind=I).ap()
    ropec = dt("ropec", [ntp + DEC_T, 128], F32, kind=I).ap()
    ropes = dt("ropes", [ntp + DEC_T, 128], F32, kind=I).ap()
    ropesmc = dt("ropesmc", [DEC_T, 256], F32, kind=I).ap()
    ropesms = dt("ropesms", [DEC_T, 256], F32, kind=I).ap()
    ropeoc = dt("ropeoc", [NL * 128, 256], F32, kind=I).ap()
    ropeos = dt("ropeos", [NL * 128, 256], F32, kind=I).ap()
    msk = dt("msk", [128, 512], F32, kind=I).ap()
    sel = dt("sel", [128, 4], F32, kind=I).ap()
    ck = dt("ck", [NS, PAST, NH * 128], F32, kind=I).ap()
    cv = dt("cv", [NS, PAST, NH * 128], F32, kind=I).ap()
    wg = dt("wg", [D, 2 * D], F32, kind=I).ap()
    wsbo = dt("wsbo", [NH * 128, D], F32, kind=I).ap()
    wreto = dt("wreto", [NH * 256, D], F32, kind=I).ap()
    wout = dt("wout", [D, D], F32, kind=I).ap()
    wr = dt("wr", [D, 20], F32, kind=I).ap()
    br = dt("br", [20], F32, kind=I).ap()
    wgate = dt("wgate", [16, D, 512], F32, kind=I).ap()
    wup = dt("wup", [16, D, 512], F32, kind=I).ap()
    wdown = dt("wdown", [16, 512, D], F32, kind=I).ap()

    O = "ExternalOutput"
    kp = dt("kp", [ntp, NH * 128], F32, kind=O).ap()
    vp = dt("vp", [ntp, NH * 128], F32, kind=O).ap()
    sp_o = dt("sp_o", [NH, 128, 256], F32, kind=O).ap()
    ks = dt("ks", [NS, DEC_T, NH * 128], F32, kind=O).ap()
    vs = dt("vs", [NS, DEC_T, NH * 128], F32, kind=O).ap()
    ss_o = dt("ss_o", [NS, NH, 128, 256], F32, kind=O).ap()
    yo = dt("yo", [NTOK, D], F32, kind=O).ap()

    SK = O if dbg else "Internal"
    NTL = 1 + NXT + NS
    uts = dt("uts", [NTL, 128, 16 * 128], BF16, kind="Internal").ap()
    uto = dt("uto", [128, 16 * NTOK], BF16, kind="Internal").ap().rearrange("p (k t) -> p k t", t=NTOK)
    osbT = dt("osbT", [NH, 128, NTOK], BF16, kind=SK).ap()
    oretT = dt("oretT", [2 * NH, 128, NTOK], BF16, kind=SK).ap()
    hdbg = dt("hdbg", [NTOK, D], F32, kind=O).ap() if dbg else None
    b_uts = Buf("uts")
    b_osb = Buf("osbT")
    b_oret = Buf("oretT")

    NPOOL = 95000
    pool_ = _Pool(nc.alloc_sbuf_tensor("arena", [128, NPOOL], BF16), NPOOL)
    AFa, ABa = Arena(pool_, True), Arena(pool_, False)

    pT = nc.alloc_psum_tensor("pT", [128, 16, 128], BF16); bpT = Buf("pT", True)
    pA = nc.alloc_psum_tensor("pA", [128, 512], F32); bpA = Buf("pA", True)
    pB = nc.alloc_psum_tensor("pB", [128, 512], F32); bpB = Buf("pB", True)
    pC = nc.alloc_psum_tensor("pC", [128, 512], F32); bpC = Buf("pC", True)
    p6 = nc.alloc_psum_tensor("p6", [128, 512], F32); bp6 = Buf("p6", True)
    p7 = nc.alloc_psum_tensor("p7", [128, 512], F32); bp7 = Buf("p7", True)
    pR = nc.alloc_psum_tensor("pR", [128, 8, 128], BF16); bpR = Buf("pR", True)

    def ring(ar, n, cnt, name, b=None):
        return Ring([ar.get(n, b) for _ in range(cnt)], name)

    CB = ABa.get(512); bCB = Buf("CB")
    gbc = AFa.get(D); bg = Buf("gbc")
    c.dma("sp", gbc, g1.partition_broadcast(128), writes=[bg], owner=bg)
    c.dma("pool", CB, cstb, writes=[bCB], owner=bCB)
    MK = ABa.get(512, 128); bMK = Buf("MK")
    c.dma("pool", MK, msk.rearrange("p (r q) -> p r q", q=128), writes=[bMK], owner=bMK)
    SEL = AFa.get(4); bSEL = Buf("SEL")
    c.dma("sp", SEL, sel, writes=[bSEL], owner=bSEL)
    IDN = CB[:, 0:128]
    NEGM = CB[:, 128:256]
    TRIN = CB[:, 256:384]
    ONESN = CB[:, 384:512]
    NTI = {128: 0, 64: 1, 16: 2}

    xr = ring(AFa, D, 2, "xt")
    st_r = ring(AFa, 4, 2, "ss")
    ur = ring(ABa, D, 2, "u")
    uTr = ring(ABa, 2048, 2, "uT", 128)
    ccr = ring(AFa, 256, 2, "cc")
    ssr = ring(AFa, 256, 2, "sn")
    kvr = ring(AFa, 256, 3, "kvst")
    rfr = ring(AFa, 256, 2, "rf")
    tmr = ring(AFa, 256, 2, "tm")
    swr = ring(AFa, 256, 2, "sw")
    sgr = ring(AFa, 256, 2, "sg")
    vbr = ring(ABa, 256, 2, "vbf")
    kdr = ring(ABa, 128, 2, "kdec")
    k16r = ring(ABa, 128, 2, "k16")
    qkr = ring(ABa, 384, 2, "qk16")
    trr = ring(ABa, 256, 2, "trT", 128)
    scr = ring(ABa, 128, 2, "scm")
    qdr = ring(ABa, 128, 2, "qdec")
    ogr = ring(ABa, 256, 2, "og")
    ogTr = ring(ABa, 256, 2, "ogT", 128)
    Wh = ABa.get(16 * WALL, WALL); bWh = Buf("Wh")
    CF = AFa.get(262); bCF = Buf("CF")
    S = AFa.get(256); bS = Buf("S")
    Sb = ABa.get(256); bSb = Buf("Sb")
    Ssel = ABa.get(max(NL, 1) * 256, 256); bSsel = Buf("Ssel")
    KT = ABa.get(NBLK * 128)
    VA = ABa.get(NBLK * 128, 128)
    QT = ABa.get(max(NL, 1) * 128)
    bKT = [Buf(f"KT{i}") for i in range(NBLK)]
    bVA = [Buf(f"VA{i}") for i in range(NBLK)]
    bQT = [Buf(f"QT{i}") for i in range(max(NL, 1))]
    Er = ring(AFa, 512, 2, "E")
    SPr = ring(ABa, 512, 2, "SP")
    ATr = ring(ABa, 512, 2, "AT")
    Sacc = AFa.get(512); bSacc = Buf("Sacc")
    Saccb = ABa.get(512); bSaccb = Buf("Saccb")
    osr = ring(ABa, 512, 2, "oso")
    ckst = ABa.get(2048, 128); bckst = Buf("ckst")

    def norm_tile(xsrc, nt, dsts):
        xt, bx = xr.next()
        c.dma("sp", xt[:nt, :], xsrc, writes=[bx], owner=bx)
        stt, bst = st_r.next()
        u, bu = ur.next()
        c.op("pool", lambda e: e.memset(stt[:, :], 0.0), writes=[bst])
        c.op("act", lambda e: e.activation(out=u[:nt, :], in_=xt[:nt, :], func=AF.Square,
                                           accum_out=stt[:nt, 0:1]), reads=[bx], writes=[bu, bst])
        c.op("act", lambda e: e.activation(out=stt[:nt, 1:2], in_=stt[:nt, 0:1], func=AF.Ln,
                                           scale=1.0 / D, bias=EPS), reads=[bst], writes=[bst])
        c.op("act", lambda e: e.activation(out=stt[:nt, 1:2], in_=stt[:nt, 1:2], func=AF.Exp,
                                           scale=-0.5), reads=[bst], writes=[bst])
        c.op("dve", lambda e: e.scalar_tensor_tensor(out=u[:nt, :], in0=xt[:nt, :], scalar=stt[:nt, 1:2],
                                                     in1=gbc[:nt, :], op0=ALU.mult, op1=ALU.mult),
             reads=[bx, bst, bg], writes=[bu])
        for j in range(16):
            c.op("pe", lambda e, j=j: e.transpose(out=pT[:, j, :nt], in_=u[:nt, j * 128:(j + 1) * 128],
                                                  identity=IDN[:nt, :nt]),
                 reads=[bu, bCB], writes=[bpT], sig=(j == 15))
        uT, buT = uTr.next()
        c.op("act", lambda e: e.copy(out=uT[:, :, :nt], in_=pT[:, :, :nt]), reads=[bpT], writes=[buT])
        for dst in dsts:
            c.dma("pool", dst, uT[:, :, :nt], reads=[buT], owner=b_uts)

    def uts_ap(idx, nt):
        return uts[idx].rearrange("p (k t) -> p k t", t=128)[:, :, :nt]

    norm_tile(meta, NMETA, [uts_ap(0, NMETA)])
    for i in range(NXT):
        norm_tile(xall[i * 128:(i + 1) * 128, :], 128, [uts_ap(1 + i, 128)])
    for l in range(NL):
        norm_tile(xtok[l * 128:(l + 1) * 128, :], 128, [uto[:, :, l * 128:(l + 1) * 128]])
    for s in range(NS):
        t0 = NL * 128 + s * DEC_T
        norm_tile(xtok[t0:t0 + DEC_T, :], DEC_T, [uts_ap(1 + NXT + s, DEC_T), uto[:, :, t0:t0 + DEC_T]])
    b_uts.w = (b_uts.dsem, b_uts.dcnt)

    def load_head(h):
        for k4 in range(4):
            c.dma("pool", Wh[:, 4 * k4:4 * k4 + 4, :],
                  wh[h, 512 * k4:512 * (k4 + 1), :].rearrange("(k p) n -> p k n", p=128),
                  writes=[bWh], owner=bWh)
        c.dma("sp", CF, cstf[h], writes=[bCF], owner=bCF)

    pA_, bpA_, pB_, bpB_ = pA, bpA, pB, bpB
    sci = [0]

    def scan_tile(idx, nt, rope_row, kout, vout, blk, sel_l=None, sel_r=None):
        (pA, bpA, pB, bpB) = ((pA_, bpA_, pB_, bpB_), (pC, bpC, p6, bp6))[sci[0] % 2]
        sci[0] += 1
        uT, buT = uTr.next()
        c.dma("sp", uT[:, :, :nt], uts_ap(idx, nt), reads=[b_uts], writes=[buT], owner=buT)
        cc, bcc = ccr.next()
        sn, bsn = ssr.next()
        c.dma("sp", cc[:nt, 0:128], ropec[rope_row:rope_row + nt, :], writes=[bcc], owner=bcc)
        c.dma("sp", sn[:nt, 0:128], ropes[rope_row:rope_row + nt, :], writes=[bsn], owner=bsn)
        for (ps, bps, c0, cn) in ((pA, bpA, 0, 256), (pB, bpB, 512, 384)):
            for k in range(16):
                c.op("pe", lambda e, ps=ps, c0=c0, cn=cn, k=k: e.matmul(
                    ps[:nt, :cn], lhsT=uT[:, k, :nt], rhs=Wh[:, k, c0:c0 + cn],
                    start=(k == 0), stop=(k == 15)),
                    reads=[buT, bWh], writes=[bps], sig=(k == 15))
        kv, bkv = kvr.next()
        c.op("act", lambda e: e.copy(out=kv[:nt, 0:128], in_=pA[:nt, 0:128]), reads=[bpA], writes=[bkv])
        c.op("act", lambda e: e.copy(out=kv[:nt, 128:256], in_=pA[:nt, 128:256]), reads=[bpA], writes=[bkv])
        c.dma("pool", kout, kv[:nt, 0:128], reads=[bkv], owner=bkv, is_output=True)
        c.dma("pool", vout, kv[:nt, 128:256], reads=[bkv], owner=bkv, is_output=True)
        k16, bk16 = k16r.next()
        c.op("act", lambda e: e.copy(out=k16[:nt, :], in_=pA[:nt, 0:128]), reads=[bpA], writes=[bk16])
        c.op("dve", lambda e: e.tensor_copy(out=VA[:nt, blk, :], in_=pA[:nt, 128:256]), reads=[bpA], writes=[bVA[blk]])
        c.op("pe", lambda e: e.transpose(out=pR[:, 7, :nt], in_=k16[:nt, :], identity=IDN[:nt, :nt]),
             reads=[bk16, bCB], writes=[bpR])
        c.op("act", lambda e: e.mul(out=KT[:, blk * 128:blk * 128 + nt], in_=pR[:, 7, :nt], mul=128.0 ** -0.5),
             reads=[bpR], writes=[bKT[blk]])
        rf, brf = rfr.next()
        c.op("dve", lambda e: e.tensor_copy(out=rf[:nt, 0:128], in_=pB[:nt, 0:128]), reads=[bpB], writes=[brf])
        vb, bvb = vbr.next()
        c.op("act", lambda e: e.copy(out=vb[:nt, :], in_=pB[:nt, 128:384]), reads=[bpB], writes=[bvb])
        tm, btm = tmr.next()
        sw, bsw = swr.next()
        c.op("dve", lambda e: e.tensor_tensor(out=tm[:nt, 0:128], in0=rf[:nt, 0:128], in1=cc[:nt, 0:128], op=ALU.mult),
             reads=[brf, bcc], writes=[btm])
        c.op("pool", lambda e: e.tensor_tensor(out=sw[:nt, 0:64], in0=rf[:nt, 64:128], in1=sn[:nt, 0:64],
                                               op=ALU.mult), reads=[brf, bsn], writes=[bsw])
        c.op("pool", lambda e: e.tensor_tensor(out=sw[:nt, 64:128], in0=rf[:nt, 0:64], in1=sn[:nt, 64:128],
                                               op=ALU.mult), reads=[brf, bsn], writes=[bsw])
        c.op("dve", lambda e: e.tensor_tensor(out=tm[:nt, 0:128], in0=tm[:nt, 0:128], in1=sw[:nt, 0:128], op=ALU.add),
             reads=[btm, bsw], writes=[btm])
        kd, bkd = kdr.next()
        gk = CF[:nt, 256 + NTI[nt]:257 + NTI[nt]]
        c.op("dve", lambda e: e.tensor_scalar(out=kd[:nt, :], in0=tm[:nt, 0:128], scalar1=gk, scalar2=None,
                                              op0=ALU.mult), reads=[btm, bCF], writes=[bkd])
        if sel_l is not None:
            sc1 = SEL[:, sel_r:sel_r + 1]
            if sel_r == 0:
                c.op("dve", lambda e: e.tensor_scalar(out=Ssel[:, sel_l, :], in0=S, scalar1=sc1, scalar2=None,
                                                      op0=ALU.mult), reads=[bS, bSEL], writes=[bSsel])
            else:
                c.op("dve", lambda e: e.scalar_tensor_tensor(out=Ssel[:, sel_l, :], in0=S, scalar=sc1,
                                                             in1=Ssel[:, sel_l, :], op0=ALU.mult, op1=ALU.add),
                     reads=[bS, bSEL, bSsel], writes=[bSsel])
        c.op("pe", lambda e: e.matmul(p7[:, 0:256], lhsT=kd[:nt, :], rhs=vb[:nt, :], start=True, stop=True),
             reads=[bkd, bvb], writes=[bp7])
        gn = CF[:, 259 + NTI[nt]:260 + NTI[nt]]
        c.op("dve", lambda e: e.scalar_tensor_tensor(out=S, in0=S, scalar=gn, in1=p7[:, 0:256],
                                                     op0=ALU.mult, op1=ALU.add),
             reads=[bS, bCF, bp7], writes=[bS])

    def own_tile(h, usrc, nt, rc_ap, rs_ap, qdst, bqd, Sb_ap, bSb_, tok0):
        uT, buT = uTr.next()
        c.dma("sp", uT[:, :, :nt], usrc, reads=[b_uts], writes=[buT], owner=buT)
        cc, bcc = ccr.next()
        sn, bsn = ssr.next()
        c.dma("sp", cc[:nt, :], rc_ap, writes=[bcc], owner=bcc)
        c.dma("sp", sn[:nt, :], rs_ap, writes=[bsn], owner=bsn)
        for (ps, bps, c0, cn) in ((pA, bpA, 256, 384), (pB, bpB, 640, 512)):
            for k in range(16):
                c.op("pe", lambda e, ps=ps, c0=c0, cn=cn, k=k: e.matmul(
                    ps[:nt, :cn], lhsT=uT[:, k, :nt], rhs=Wh[:, k, c0:c0 + cn],
                    start=(k == 0), stop=(k == 15)),
                    reads=[buT, bWh], writes=[bps], sig=(k == 15))
        qk, bqk = qkr.next()
        c.op("act", lambda e: e.copy(out=qk[:nt, 0:128], in_=pA[:nt, 0:128]), reads=[bpA], writes=[bqk])
        rf, brf = rfr.next()
        c.op("dve", lambda e: e.tensor_copy(out=rf[:nt, :], in_=pA[:nt, 128:384]), reads=[bpA], writes=[brf])
        vb, bvb = vbr.next()
        c.op("act", lambda e: e.copy(out=vb[:nt, :], in_=pB[:nt, 0:256]), reads=[bpB], writes=[bvb])
        sg, bsg = sgr.next()
        c.op("act", lambda e: e.activation(out=sg[:nt, :], in_=pB[:nt, 256:512], func=AF.Exp, scale=-1.0),
             reads=[bpB], writes=[bsg])
        c.op("dve", lambda e: e.tensor_scalar(out=sg[:nt, :], in0=sg[:nt, :], scalar1=1.0, scalar2=None,
                                              op0=ALU.add), reads=[bsg], writes=[bsg])
        c.op("dve", lambda e: e.reciprocal(out=sg[:nt, :], in_=sg[:nt, :]), reads=[bsg], writes=[bsg])
        c.op("dve", lambda e: e.tensor_tensor(out=sg[:nt, :], in0=sg[:nt, :], in1=pB[:nt, 256:512], op=ALU.mult),
             reads=[bsg, bpB], writes=[bsg])
        tm, btm = tmr.next()
        sw, bsw = swr.next()
        c.op("dve", lambda e: e.tensor_tensor(out=tm[:nt, :], in0=rf[:nt, :], in1=cc[:nt, :], op=ALU.mult),
             reads=[brf, bcc], writes=[btm])
        rf4 = rf[:nt, :].rearrange("p (a h d) -> p a h d", a=2, h=2)
        sw4 = sw[:nt, :].rearrange("p (a h d) -> p a h d", a=2, h=2)
        sn4 = sn[:nt, :].rearrange("p (a h d) -> p a h d", a=2, h=2)
        c.op("pool", lambda e: e.tensor_tensor(out=sw4[:, :, 0, :], in0=rf4[:, :, 1, :], in1=sn4[:, :, 0, :],
                                               op=ALU.mult), reads=[brf, bsn], writes=[bsw])
        c.op("pool", lambda e: e.tensor_tensor(out=sw4[:, :, 1, :], in0=rf4[:, :, 0, :], in1=sn4[:, :, 1, :],
                                               op=ALU.mult), reads=[brf, bsn], writes=[bsw])
        c.op("dve", lambda e: e.tensor_tensor(out=qk[:nt, 128:384], in0=tm[:nt, :], in1=sw[:nt, :], op=ALU.add),
             reads=[btm, bsw], writes=[bqk])
        for j in range(3):
            c.op("pe", lambda e, j=j: e.transpose(out=pR[:, j, :nt], in_=qk[:nt, j * 128:(j + 1) * 128],
                                                  identity=IDN[:nt, :nt]),
                 reads=[bqk, bCB], writes=[bpR], sig=(j == 2))
        c.op("act", lambda e: e.copy(out=qdst, in_=pR[:, 0, :nt]), reads=[bpR], writes=[bqd])
        tr, btr = trr.next()
        c.op("dve", lambda e: e.tensor_copy(out=tr[:, :, :nt], in_=pR[:, 1:3, :nt]), reads=[bpR], writes=[btr])
        c.op("pe", lambda e: e.matmul(p6[:nt, 0:nt], lhsT=tr[:, 1, :nt], rhs=tr[:, 0, :nt], start=True, stop=True),
             reads=[btr], writes=[bp6])
        sc, bsc = scr.next()
        c.op("dve", lambda e: e.tensor_tensor(out=sc[:nt, :nt], in0=p6[:nt, 0:nt], in1=CF[:nt, 0:nt], op=ALU.mult),
             reads=[bp6, bCF], writes=[bsc])
        qd, bqdc = qdr.next()
        c.op("pool", lambda e: e.tensor_tensor(out=qd[:, :nt], in0=tr[:, 0, :nt], in1=CF[:, 128:128 + nt], op=ALU.mult),
             reads=[btr, bCF], writes=[bqdc])
        c.op("pe", lambda e: e.matmul(p7[:nt, 0:256], lhsT=sc[:nt, :nt], rhs=vb[:nt, :], start=True, stop=False),
             reads=[bsc, bvb], writes=[bp7], sig=False)
        c.op("pe", lambda e: e.matmul(p7[:nt, 0:256], lhsT=qd[:, :nt], rhs=Sb_ap, start=False, stop=True),
             reads=[bqdc, bSb_], writes=[bp7])
        stt, bst = st_r.next()
        og, bog = ogr.next()
        c.op("pool", lambda e: e.memset(stt[:, :], 0.0), writes=[bst])
        c.op("act", lambda e: e.activation(out=og[:nt, :], in_=p7[:nt, 0:256], func=AF.Square,
                                           accum_out=stt[:nt, 2:3]), reads=[bp7], writes=[bog, bst])
        c.op("act", lambda e: e.activation(out=stt[:nt, 3:4], in_=stt[:nt, 2:3], func=AF.Ln,
                                           scale=1.0 / 256, bias=EPS), reads=[bst], writes=[bst])
        c.op("act", lambda e: e.activation(out=stt[:nt, 3:4], in_=stt[:nt, 3:4], func=AF.Exp,
                                           scale=-0.5), reads=[bst], writes=[bst])
        c.op("dve", lambda e: e.scalar_tensor_tensor(out=og[:nt, :], in0=p7[:nt, 0:256], scalar=stt[:nt, 3:4],
                                                     in1=sg[:nt, :], op0=ALU.mult, op1=ALU.mult),
             reads=[bp7, bst, bsg], writes=[bog])
        for j in range(2):
            c.op("pe", lambda e, j=j: e.transpose(out=pR[:, 4 + j, :nt], in_=og[:nt, j * 128:(j + 1) * 128],
                                                  identity=IDN[:nt, :nt]),
                 reads=[bog, bCB], writes=[bpR], sig=(j == 1))
        ogT, bogT = ogTr.next()
        c.op("act", lambda e: e.copy(out=ogT[:, :, :nt], in_=pR[:, 4:6, :nt]), reads=[bpR], writes=[bogT])
        c.dma("pool", oretT[2 * h:2 * h + 2, :, tok0:tok0 + nt].rearrange("c p t -> p c t"), ogT[:, :, :nt],
              reads=[bogT], owner=b_oret)

    banksZ = [(pA, bpA), (pB, bpB)]
    banksA = [(pC, bpC), (p6, bp6)]
    banksA4 = [(pA, bpA), (pC, bpC), (pB, bpB), (p6, bp6)]
    zi = [0]

    def attn_run(qcols, rdq, keys, ncb, cw, dst):
        NA = ncb * cw
        c.op("pool", lambda e: e.memset(Sacc[:, :NA], 0.0), writes=[bSacc])
        c.op("pool", lambda e: e.memset(Saccb[:, :], 0.0), writes=[bSaccb])
        c.op("pe", lambda e: e.matmul(p7[:, :NA], lhsT=Saccb[:, 0:128], rhs=Saccb[:, :NA], start=True, stop=False),
             reads=[bSaccb], writes=[bp7])
        started = [True] * ncb
        for idx, (ktap, bkt, vaap, bva, nk, cb0, mask) in enumerate(keys):
            last = idx == len(keys) - 1
            c0 = cb0 * cw
            N = NA - c0
            (pa, bpa) = banksA4[zi[0] % 4]
            zi[0] += 1
            c.op("pe", lambda e: e.matmul(pa[:nk, :N], lhsT=ktap, rhs=qcols[:, c0:NA], start=True, stop=False),
                 reads=[bkt] + rdq, writes=[bpa], sig=(mask is None))
            if mask is not None:
                c.op("pe", lambda e: e.matmul(pa[:nk, 0:cw], lhsT=IDN[:nk, :nk], rhs=mask, start=False, stop=False),
                     reads=[bCB, bMK], writes=[bpa])
            E, bE = Er.next()
            c.op("act", lambda e: e.activation(out=E[:nk, :N], in_=pa[:nk, :N], func=AF.Exp),
                 reads=[bpa], writes=[bE])
            SPt, bSP = SPr.next()
            c.op("act", lambda e: e.activation(out=SPt[:nk, :N], in_=E[:nk, :N], func=AF.Ln, bias=1.0),
                 reads=[bE], writes=[bSP])
            c.op("pe", lambda e: e.matmul(pa[:nk, :N], lhsT=TRIN[:nk, :nk], rhs=SPt[:nk, :N],
                                          start=False, stop=(idx == 0)),
                 reads=[bCB, bSP], writes=[bpa], sig=(idx == 0))
            if idx > 0:
                c.op("pe", lambda e: e.matmul(pa[:nk, :N], lhsT=ONESN[:, :nk], rhs=Saccb[:, c0:NA],
                                              start=False, stop=True),
                     reads=[bCB, bSaccb], writes=[bpa])
            if not last:
                c.op("dve", lambda e: e.tensor_tensor(out=Sacc[:nk, c0:NA], in0=Sacc[:nk, c0:NA],
                                                      in1=SPt[:nk, :N], op=ALU.add),
                     reads=[bSacc, bSP], writes=[bSacc])
                c.op("dve", lambda e: e.tensor_copy(out=Saccb[:, c0:NA], in_=Sacc[:, c0:NA]),
                     reads=[bSacc], writes=[bSaccb])
            AT, bAT = ATr.next()
            c.op("act", lambda e: e.activation(out=AT[:nk, :N], in_=pa[:nk, :N], func=AF.Exp),
                 reads=[bpa], writes=[bAT])
            for cb in range(cb0, ncb):
                a0 = (cb - cb0) * cw
                c.op("pe", lambda e, cb=cb, a0=a0: e.matmul(
                    p7[:, cb * cw:(cb + 1) * cw], lhsT=vaap, rhs=AT[:nk, a0:a0 + cw],
                    start=(not started[cb]), stop=last),
                    reads=[bAT, bva], writes=[bp7], sig=(cb == ncb - 1))
                started[cb] = True
        oso, boso = osr.next()
        c.op("dve", lambda e: e.tensor_copy(out=oso[:, :NA], in_=p7[:, :NA]), reads=[bp7], writes=[boso])
        c.dma("pool", dst, oso[:, :NA], reads=[boso], owner=b_osb)

    for h in range(NH):
        load_head(h)
        hc = slice(h * 128, (h + 1) * 128)
        c.op("pool", lambda e: e.memset(S, 0.0), writes=[bS])
        scan_tile(0, NMETA, 0, kp[0:NMETA, hc], vp[0:NMETA, hc], 0)
        for i in range(NXT):
            r0 = NMETA + i * 128
            scan_tile(1 + i, 128, r0, kp[r0:r0 + 128, hc], vp[r0:r0 + 128, hc], 1 + i, sel_l=i // 4, sel_r=i % 4)
        c.dma("pool", sp_o[h], S, reads=[bS], owner=bS, is_output=True)
        for l in range(NL):
            own_tile(h, uto[:, :, l * 128:(l + 1) * 128], 128, ropeoc[l * 128:(l + 1) * 128, :],
                     ropeos[l * 128:(l + 1) * 128, :], QT[:, l * 128:(l + 1) * 128], bQT[l],
                     Ssel[:, l, :], bSsel, l * 128)
        for l0 in range(0, NL, 4):
            l1 = min(l0 + 4, NL)
            keys = []
            for kt in range(4 * (l1 - 1) + 3, -1, -1):
                lp, r = kt // 4, kt % 4
                blk = 1 + kt
                keys.append((KT[:, blk * 128:(blk + 1) * 128], bKT[blk], VA[:, blk, :], bVA[blk], 128,
                             max(lp - l0, 0), MK[:, r, :] if lp >= l0 else None))
            keys.append((KT[:, 0:NMETA], bKT[0], VA[:NMETA, 0, :], bVA[0], NMETA, 0, None))
            attn_run(QT[:, l0 * 128:l1 * 128], [bQT[i] for i in range(l0, l1)], keys, l1 - l0, 128,
                     osbT[h, :, l0 * 128:l1 * 128])
        for s in range(NS):
            tok0 = NL * 128 + s * DEC_T
            c.dma("pool", VA[:, 0:16, :], cv[s, :, hc].rearrange("(a p) d -> p a d", p=128),
                  writes=[bVA[i] for i in range(16)], owner=bVA[0])
            c.dma("pool", ckst, ck[s, :, hc].rearrange("(a p) d -> p a d", p=128), writes=[bckst], owner=bckst)
            for j in range(16):
                c.op("pe", lambda e, j=j: e.transpose(out=pT[:, j, :], in_=ckst[:, j, :], identity=IDN),
                     reads=[bckst, bCB], writes=[bpT], sig=(j == 15))
            c.op("act", lambda e: e.mul(out=KT[:, 0:2048], in_=pT[:].rearrange("p a d -> p (a d)"), mul=128.0 ** -0.5),
                 reads=[bpT], writes=[bKT[i] for i in range(16)])
            c.dma("sp", S, st[s, h], writes=[bS], owner=bS)
            c.op("pool", lambda e: e.tensor_copy(out=Sb, in_=S), reads=[bS], writes=[bSb])
            own_tile(h, uto[:, :, tok0:tok0 + DEC_T], DEC_T, ropesmc[:, :], ropesms[:, :],
                     QT[:, 0:DEC_T], bQT[0], Sb, bSb, tok0)
            scan_tile(1 + NXT + s, DEC_T, ntp, ks[s, :, hc], vs[s, :, hc], 16)
            c.dma("pool", ss_o[s, h], S, reads=[bS], owner=bS, is_output=True)
            keys = [(KT[:, 2048:2048 + DEC_T], bKT[16], VA[:DEC_T, 16, :], bVA[16], DEC_T, 0, NEGM[:DEC_T, :DEC_T])]
            for kb in range(15, -1, -1):
                keys.append((KT[:, kb * 128:(kb + 1) * 128], bKT[kb], VA[:, kb, :], bVA[kb], 128, 0, None))
            attn_run(QT[:, 0:DEC_T], [bQT[0]], keys, 1, DEC_T, osbT[h, :, tok0:tok0 + DEC_T])
    b_osb.w = (b_osb.dsem, b_osb.dcnt)
    b_oret.w = (b_oret.dsem, b_oret.dcnt)

    hs = dt("hs", [NTOK, D], F32, kind=SK).ap()
    b_hs = Buf("hs")
    c.barrier()
    AFa.reset(); ABa.reset()
    CB2 = ABa.get(512); IDN = CB2[:, 0:128]
    GC = 768
    xcr = ring(AFa, 512, 2, "xc")
    gsr = ring(AFa, 512, 4, "gs")
    hcr = ring(AFa, 512, 3, "hc")
    slots = ring(ABa, 8192, 3, "slot")
    actU = ABa.get(16 * GC, GC); bactU = Buf("actU")
    actS = ABa.get(8 * GC, GC); bactS = Buf("actS")
    actR = ABa.get(16 * GC, GC); bactR = Buf("actR")
    MT = ABa.get(16 * GC, GC); bMT = Buf("MT")

    for t0 in range(0, NTOK, GC):
        T = min(GC, NTOK - t0)
        ntl = T // 128
        c.dma("sp", actU[:, :, :T], uto[:, :, t0:t0 + T], reads=[b_uts], writes=[bactU], owner=bactU)
        c.dma("sp", actS[:, :NH, :T], osbT[:, :, t0:t0 + T].rearrange("h p t -> p h t"), reads=[b_osb],
              writes=[bactS], owner=bactS)
        c.dma("sp", actR[:, :2 * NH, :T], oretT[:, :, t0:t0 + T].rearrange("h p t -> p h t"), reads=[b_oret],
              writes=[bactR], owner=bactR)
        for fc in range(16):
            sl, bsl = slots.next()
            fcs = slice(fc * 128, (fc + 1) * 128)
            w1 = sl[:, 0:2048].rearrange("p (a b) -> p a b", b=128)
            w2 = sl[:, 2048:4096].rearrange("p (a b) -> p a b", b=128)
            w3 = sl[:, 4096:4096 + NH * 128].rearrange("p (a b) -> p a b", b=128)
            w4 = sl[:, 6144:6144 + 2 * NH * 128].rearrange("p (a b) -> p a b", b=128)
            c.dma("pool", w1, wg[:, fc * 128:(fc + 1) * 128].rearrange("(k p) n -> p k n", p=128), writes=[bsl], owner=bsl)
            c.dma("pool", w2, wg[:, D + fc * 128:D + (fc + 1) * 128].rearrange("(k p) n -> p k n", p=128), writes=[bsl], owner=bsl)
            c.dma("pool", w3, wsbo[:, fcs].rearrange("(k p) n -> p k n", p=128), writes=[bsl], owner=bsl)
            c.dma("pool", w4, wreto[:, fcs].rearrange("(k p) n -> p k n", p=128), writes=[bsl], owner=bsl)
            for (o, n) in [(o_, min(512, T - o_)) for o_ in range(0, T, 512)]:
                for (ps, bps, w, act, bact, nk_) in ((pA, bpA, w1, actU, bactU, 16), (pB, bpB, w2, actU, bactU, 16),
                                                     (pC, bpC, w3, actS, bactS, NH), (p6, bp6, w4, actR, bactR, 2 * NH)):
                    for k in range(nk_):
                        c.op("pe", lambda e, ps=ps, w=w, act=act, k=k, nk_=nk_: e.matmul(
                            ps[:, :n], lhsT=w[:, k, :], rhs=act[:, k, o:o + n], start=(k == 0), stop=(k == nk_ - 1)),
                            reads=[bsl, bact], writes=[bps], sig=(k == nk_ - 1))
                ga, bga = gsr.next()
                gb, bgb = gsr.next()
                c.op("act", lambda e: e.activation(out=ga[:, :n], in_=pA[:, :n], func=AF.Sigmoid), reads=[bpA], writes=[bga])
                c.op("act", lambda e: e.activation(out=gb[:, :n], in_=pB[:, :n], func=AF.Sigmoid), reads=[bpB], writes=[bgb])
                c.op("dve", lambda e: e.tensor_tensor(out=ga[:, :n], in0=ga[:, :n], in1=pC[:, :n], op=ALU.mult),
                     reads=[bga, bpC], writes=[bga])
                c.op("dve", lambda e: e.tensor_tensor(out=gb[:, :n], in0=gb[:, :n], in1=p6[:, :n], op=ALU.mult),
                     reads=[bgb, bp6], writes=[bgb])
                c.op("dve", lambda e, fc=fc: e.tensor_tensor(out=MT[:, fc, o:o + n], in0=ga[:, :n], in1=gb[:, :n], op=ALU.add),
                     reads=[bga, bgb], writes=[bMT])
        for oc in range(4):
            sl, bsl = slots.next()
            wo = sl[:, 0:8192].rearrange("p (a b) -> p a b", b=512)
            for k4 in range(4):
                c.dma("pool", wo[:, 4 * k4:4 * k4 + 4, :],
                      wout[512 * k4:512 * (k4 + 1), oc * 512:(oc + 1) * 512].rearrange("(k p) n -> p k n", p=128),
                      writes=[bsl], owner=bsl)
            for ti in range(ntl):
                xc, bxc = xcr.next()
                c.dma("sp", xc, xtok[t0 + ti * 128:t0 + (ti + 1) * 128, oc * 512:(oc + 1) * 512], writes=[bxc], owner=bxc)
                (ps, bps) = ((pA, bpA), (pB, bpB))[(oc * ntl + ti) % 2]
                for k in range(16):
                    c.op("pe", lambda e, k=k, ti=ti, ps=ps: e.matmul(ps[:, :], lhsT=MT[:, k, ti * 128:(ti + 1) * 128], rhs=wo[:, k, :],
                                                                     start=(k == 0), stop=(k == 15)),
                         reads=[bMT, bsl], writes=[bps], sig=(k == 15))
                hc_, bhc = hcr.next()
                c.op("dve", lambda e, ps=ps: e.tensor_tensor(out=hc_, in0=ps[:, :], in1=xc, op=ALU.add),
                     reads=[bps, bxc], writes=[bhc])
                c.dma("pool", hs[t0 + ti * 128:t0 + (ti + 1) * 128, oc * 512:(oc + 1) * 512], hc_, reads=[bhc], owner=b_hs,
                      is_output=dbg)
    b_hs.w = (b_hs.dsem, b_hs.dcnt)

    c.barrier()
    AFa.reset(); ABa.reset()
    CB2 = ABa.get(512); IDN = CB2[:, 0:128]
    NTG = GT // 128
    gv = AFa.get(D); bgv = Buf("gv")
    brc = AFa.get(20); bbr = Buf("brc")
    c.dma("sp", brc, br.partition_broadcast(128), writes=[bbr], owner=bbr)
    wrf = AFa.get(320, 20); bwrf = Buf("wrf")
    c.dma("sp", wrf, wr.rearrange("(k p) n -> p k n", p=128), writes=[bwrf], owner=bwrf)
    wrh = ABa.get(320, 20); bwrh = Buf("wrh")
    wrl = ABa.get(320, 20); bwrl = Buf("wrl")
    c.op("act", lambda e: e.copy(out=wrh, in_=wrf), reads=[bwrf], writes=[bwrh])
    c.op("dve", lambda e: e.tensor_tensor(out=wrl, in0=wrf, in1=wrh, op=ALU.subtract), reads=[bwrf, bwrh], writes=[bwrl])
    H = AFa.get(NTG * D, D); bH = [Buf(f"H{i}") for i in range(NTG)]
    Cmb = AFa.get(NTG * 16, 16); bCmb = Buf("Cmb")
    gsr = ring(AFa, 512, 2, "gs2")
    rt = ring(AFa, 64, 2, "rt")
    st2 = ring(AFa, 8, 2, "st2")
    u2f = AFa.get(D); bu2f = Buf("u2f")
    slots = ring(ABa, 8192, 3, "eslot")
    actU = ABa.get(16 * GT, GT); bactU = Buf("u2T")
    u2h = ABa.get(D); bu2h = Buf("u2h")
    u2l = ABa.get(D); bu2l = Buf("u2l")
    loT = ABa.get(D, 128); bloT = Buf("loT")
    hT = ABa.get(4 * GT, GT); bhT = Buf("hT")

    def subgroups(T):
        out, o = [], 0
        while o < T:
            n = min(512, T - o)
            out.append((o, n))
            o += n
        return out

    def rms_tile(ti, gain_buf):
        stt, bst = st2.next()
        c.op("pool", lambda e: e.memset(stt, 0.0), writes=[bst])
        c.op("act", lambda e: e.activation(out=u2f, in_=H[:, ti, :], func=AF.Square, accum_out=stt[:, 0:1]),
             reads=[bH[ti]], writes=[bu2f, bst])
        c.op("act", lambda e: e.activation(out=stt[:, 1:2], in_=stt[:, 0:1], func=AF.Ln, scale=1.0 / D, bias=EPS),
             reads=[bst], writes=[bst])
        c.op("act", lambda e: e.activation(out=stt[:, 1:2], in_=stt[:, 1:2], func=AF.Exp, scale=-0.5),
             reads=[bst], writes=[bst])
        c.op("dve", lambda e: e.scalar_tensor_tensor(out=u2f, in0=H[:, ti, :], scalar=stt[:, 1:2], in1=gv,
                                                     op0=ALU.mult, op1=ALU.mult),
             reads=[bH[ti], bst, bgv], writes=[bu2f])

    for t0 in range(0, NTOK, GT):
        T = min(GT, NTOK - t0)
        ntl = T // 128
        for ti in range(ntl):
            c.dma("sp", H[:, ti, :], hs[t0 + ti * 128:t0 + (ti + 1) * 128, :], reads=[b_hs], writes=[bH[ti]], owner=bH[ti])
        c.dma("sp", gv, g2.partition_broadcast(128), writes=[bgv], owner=bgv)
        for ti in range(ntl):
            rms_tile(ti, gv)
            c.op("act", lambda e: e.copy(out=u2h, in_=u2f), reads=[bu2f], writes=[bu2h])
            c.op("dve", lambda e: e.tensor_tensor(out=u2l, in0=u2f, in1=u2h, op=ALU.subtract),
                 reads=[bu2f, bu2h], writes=[bu2l])
            for j in range(16):
                c.op("pe", lambda e, j=j: e.transpose(out=pT[:, j, :], in_=u2h[:, j * 128:(j + 1) * 128], identity=IDN),
                     reads=[bu2h], writes=[bpT], sig=(j == 15))
            c.op("act", lambda e, ti=ti: e.copy(out=actU[:, :, ti * 128:(ti + 1) * 128], in_=pT[:, :, :]),
                 reads=[bpT], writes=[bactU])
            for j in range(16):
                c.op("pe", lambda e, j=j: e.transpose(out=pT[:, j, :], in_=u2l[:, j * 128:(j + 1) * 128], identity=IDN),
                     reads=[bu2l], writes=[bpT], sig=(j == 15))
            c.op("act", lambda e: e.copy(out=loT, in_=pT[:, :, :]), reads=[bpT], writes=[bloT])
            mm = []
            for k in range(16):
                mm.append((actU[:, k, ti * 128:(ti + 1) * 128], wrh[:, k, :]))
            for k in range(16):
                mm.append((loT[:, k, :], wrh[:, k, :]))
            for k in range(16):
                mm.append((actU[:, k, ti * 128:(ti + 1) * 128], wrl[:, k, :]))
            for i_, (l_, r_) in enumerate(mm):
                c.op("pe", lambda e, l_=l_, r_=r_, i_=i_: e.matmul(pB[:, 0:20], lhsT=l_, rhs=r_, start=(i_ == 0), stop=(i_ == 47)),
                     reads=[bactU, bloT, bwrh, bwrl], writes=[bpB], sig=(i_ == 47))
            R_, bR = rt.next()
            L = R_[:, 0:20]
            c.op("dve", lambda e: e.tensor_tensor(out=L, in0=pB[:, 0:20], in1=brc, op=ALU.add), reads=[bpB, bbr], writes=[bR])
            gmax, gsum, m1, m2 = R_[:, 20:21], R_[:, 21:22], R_[:, 22:23], R_[:, 23:24]
            oh, pen = R_[:, 24:28], R_[:, 28:32]
            elm, mk1 = R_[:, 32:48], R_[:, 48:64]
            R2, bR2 = rt.next()
            elm2, mk2, ge = R2[:, 0:16], R2[:, 16:32], R2[:, 32:36]
            w1_, w2_, e2 = R2[:, 36:37], R2[:, 37:38], R2[:, 38:39]
            rb = [bR, bR2]
            V = lambda fn: c.op("dve", fn, reads=rb, writes=rb)
            V(lambda e: e.tensor_reduce(out=gmax, in_=L[:, 0:4], op=ALU.max, axis=AX.X))
            V(lambda e: e.tensor_scalar(out=oh, in0=L[:, 0:4], scalar1=gmax, scalar2=None, op0=ALU.is_ge))
            V(lambda e: e.tensor_scalar(out=ge, in0=L[:, 0:4], scalar1=gmax, scalar2=None, op0=ALU.subtract))
            c.op("act", lambda e: e.activation(out=ge, in_=ge, func=AF.Exp), reads=rb, writes=rb)
            V(lambda e: e.tensor_reduce(out=gsum, in_=ge, op=ALU.add, axis=AX.X))
            V(lambda e: e.reciprocal(out=gsum, in_=gsum))
            V(lambda e: e.tensor_scalar(out=pen, in0=oh, scalar1=-1.0, scalar2=1e9, op0=ALU.add, op1=ALU.mult))
            for gi in range(4):
                V(lambda e, gi=gi: e.tensor_scalar(out=elm[:, gi * 4:gi * 4 + 4], in0=L[:, 4 + gi * 4:8 + gi * 4],
                                                   scalar1=pen[:, gi:gi + 1], scalar2=None, op0=ALU.add))
            V(lambda e: e.tensor_reduce(out=m1, in_=elm, op=ALU.max, axis=AX.X))
            V(lambda e: e.tensor_scalar(out=mk1, in0=elm, scalar1=m1, scalar2=None, op0=ALU.is_ge))
            V(lambda e: e.scalar_tensor_tensor(out=elm2, in0=mk1, scalar=-1e9, in1=elm, op0=ALU.mult, op1=ALU.add))
            V(lambda e: e.tensor_reduce(out=m2, in_=elm2, op=ALU.max, axis=AX.X))
            V(lambda e: e.tensor_scalar(out=mk2, in0=elm2, scalar1=m2, scalar2=None, op0=ALU.is_ge))
            V(lambda e: e.tensor_tensor(out=e2, in0=m2, in1=m1, op=ALU.subtract))
            c.op("act", lambda e: e.activation(out=e2, in_=e2, func=AF.Exp), reads=rb, writes=rb)
            V(lambda e: e.tensor_scalar(out=w1_, in0=e2, scalar1=1.0, scalar2=None, op0=ALU.add))
            V(lambda e: e.reciprocal(out=w1_, in_=w1_))
            V(lambda e: e.tensor_tensor(out=w1_, in0=w1_, in1=gsum, op=ALU.mult))
            V(lambda e: e.tensor_tensor(out=w2_, in0=w1_, in1=e2, op=ALU.mult))
            V(lambda e: e.tensor_scalar(out=mk1, in0=mk1, scalar1=w1_, scalar2=None, op0=ALU.mult))
            c.op("dve", lambda e, ti=ti: e.scalar_tensor_tensor(out=Cmb[:, ti, :], in0=mk2, scalar=w2_, in1=mk1,
                                                                op0=ALU.mult, op1=ALU.add), reads=rb, writes=rb + [bCmb])
        for ex in range(16):
            wge_, bwg = slots.next()
            wue_, bwu = slots.next()
            wde_, bwd = slots.next()
            wge = wge_.rearrange("p (a b) -> p a b", b=512)
            wue = wue_.rearrange("p (a b) -> p a b", b=512)
            wde = wde_.rearrange("p (a b) -> p a b", b=D)
            for k4 in range(4):
                c.dma("pool", wge[:, 4 * k4:4 * k4 + 4, :], wgate[ex, 512 * k4:512 * (k4 + 1), :].rearrange("(k p) n -> p k n", p=128),
                      writes=[bwg], owner=bwg)
            for k4 in range(4):
                c.dma("pool", wue[:, 4 * k4:4 * k4 + 4, :], wup[ex, 512 * k4:512 * (k4 + 1), :].rearrange("(k p) n -> p k n", p=128),
                      writes=[bwu], owner=bwu)
            c.dma("pool", wde, wdown[ex].rearrange("(k p) n -> p k n", p=128), writes=[bwd], owner=bwd)
            for (o, n) in subgroups(T):
                for fx in range(4):
                    for (ps, bps, w, bw) in ((pA, bpA, wge, bwg), (pB, bpB, wue, bwu)):
                        for k in range(16):
                            c.op("pe", lambda e, ps=ps, w=w, k=k, fx=fx: e.matmul(
                                ps[:, :n], lhsT=w[:, k, fx * 128:(fx + 1) * 128], rhs=actU[:, k, o:o + n],
                                start=(k == 0), stop=(k == 15)),
                                reads=[bw, bactU], writes=[bps], sig=(k == 15))
                    ga, bga = gsr.next()
                    c.op("act", lambda e: e.activation(out=ga[:, :n], in_=pA[:, :n], func=AF.Sigmoid), reads=[bpA], writes=[bga])
                    c.op("dve", lambda e: e.tensor_tensor(out=ga[:, :n], in0=ga[:, :n], in1=pA[:, :n], op=ALU.mult),
                         reads=[bga, bpA], writes=[bga])
                    c.op("dve", lambda e, fx=fx: e.tensor_tensor(out=hT[:, fx, o:o + n], in0=ga[:, :n], in1=pB[:, :n], op=ALU.mult),
                         reads=[bga, bpB], writes=[bhT])
            for ti in range(ntl):
                for oc in range(4):
                    (ps, bps) = ((pC, bpC), (p6, bp6))[(ti * 4 + oc) % 2]
                    for fx in range(4):
                        c.op("pe", lambda e, ps=ps, fx=fx, ti=ti, oc=oc: e.matmul(
                            ps[:, :], lhsT=hT[:, fx, ti * 128:(ti + 1) * 128], rhs=wde[:, fx, oc * 512:(oc + 1) * 512],
                            start=(fx == 0), stop=(fx == 3)),
                            reads=[bhT, bwd], writes=[bps], sig=(fx == 3))
                    c.op("dve", lambda e, ps=ps, ti=ti, oc=oc, ex=ex: e.scalar_tensor_tensor(
                        out=H[:, ti, oc * 512:(oc + 1) * 512], in0=ps[:, :], scalar=Cmb[:, ti, ex:ex + 1],
                        in1=H[:, ti, oc * 512:(oc + 1) * 512], op0=ALU.mult, op1=ALU.add),
                        reads=[bps, bCmb, bH[ti]], writes=[bH[ti]])
        c.dma("sp", gv, gf.partition_broadcast(128), writes=[bgv], owner=bgv)
        for ti in range(ntl):
            rms_tile(ti, gv)
            c.dma("sp", yo[t0 + ti * 128:t0 + (ti + 1) * 128, :], u2f, reads=[bu2f], owner=bu2f, is_output=True)
    c.finish()
    return c


def head_consts(h):
    lg = np.log1p(-np.float32(2.0) ** np.float32(-5.0 - h)).astype(np.float32)
    t = np.arange(128, dtype=np.float32)
    rel = t[None, :] - t[:, None]
    dmt = np.where(rel >= 0, np.exp(np.maximum(rel, 0) * lg), 0.0).astype(np.float32)
    gq = np.broadcast_to(np.exp((t + 1.0) * lg)[None, :], (128, 128)).astype(np.float32)
    cf = np.zeros((128, 262), np.float32)
    cf[:, 0:128] = dmt
    cf[:, 128:256] = gq
    for i, nt in enumerate((128, 64, 16)):
        v = np.zeros(128, np.float32)
        v[:nt] = np.exp((nt - 1.0 - t[:nt]) * lg)
        cf[:, 256 + i] = v
        cf[:, 259 + i] = np.exp(np.float32(nt) * lg)
    return cf


def bf_consts():
    cb = np.zeros((128, 512), np.float32)
    i = np.arange(128)
    cb[:, 0:128] = np.eye(128)
    cb[:, 128:256] = np.where(i[:, None] >= i[None, :], NEG, 0.0)
    cb[:, 256:384] = np.where(i[:, None] >= i[None, :], -1.0, 0.0)
    cb[:, 384:512] = -1.0
    return cb


def rope_tables(ntp):
    pos = np.concatenate([np.arange(ntp, dtype=np.float32) - NMETA, PAST + np.arange(DEC_T, dtype=np.float32)])
    inv = (1.0 / (np.float32(10000.0) ** (np.arange(0, 128, 2, dtype=np.float32) / np.float32(128)))).astype(np.float32)
    ang = (pos[:, None] * inv[None, :]).astype(np.float32)
    cs, sn = np.cos(ang).astype(np.float32), np.sin(ang).astype(np.float32)
    s = np.float32(128.0 ** -0.5)
    cc = np.concatenate([cs, cs, cs * s, cs * s], axis=1).astype(np.float32)
    ss = np.concatenate([-sn, sn, -sn * s, sn * s], axis=1).astype(np.float32)
    return np.ascontiguousarray(cc), np.ascontiguousarray(ss)


def wh_cols(h):
    def r(o, w):
        return np.arange(o + h * w, o + (h + 1) * w)
    return np.concatenate([r(1024, 128), r(2048, 128), r(0, 128), r(3072, 128), r(4096, 128),
                           r(5120, 256), r(7168, 256)])


def make_maps(inp, NXT=64, NH=8, NS=NSC, cores=range(8)):
    ntp = NMETA + NXT * 128
    NL = NXT // 4
    cc, ss = rope_tables(ntp)
    cb = bf_consts()
    cf = np.stack([head_consts(h) for h in range(NH)], 0)
    w_in = inp["w_in"][0]
    wh = np.stack([w_in[:, wh_cols(h)] for h in range(NH)], 0)
    c_ = np.ascontiguousarray
    shared = {
        "meta": c_(inp["meta"]), "g1": c_(inp["norm1_g"][0]), "g2": c_(inp["norm2_g"][0]), "gf": c_(inp["normf_g"]),
        "wh": wh, "cstf": cf, "cstb": cb, "ropec": c_(cc[:, 128:256]), "ropes": c_(ss[:, 128:256]),
        "ropesmc": c_(cc[ntp:ntp + DEC_T]), "ropesms": c_(ss[ntp:ntp + DEC_T]),
        "wg": c_(w_in[:, 9216:13312]), "wsbo": c_(inp["w_sb_o"][0][:NH * 128]), "wreto": c_(inp["w_ret_o"][0][:NH * 256]),
        "wout": c_(inp["w_out"][0]),
        "wr": c_(np.concatenate([inp["w_grp"][0], inp["w_exp"][0]], axis=1)),
        "br": c_(np.concatenate([inp["b_grp"][0], inp["b_exp"][0]], axis=0)),
        "wgate": c_(inp["w_gate"][0]), "wup": c_(inp["w_up"][0]), "wdown": c_(inp["w_down"][0]),
    }
    maps = []
    ii = np.arange(128)
    for cid in cores:
        b, j = cid // 4, cid % 4
        tiles = [4 * l + j for l in range(NL)]
        xp = inp["x_prompt"][b]
        xtok = np.concatenate([xp[t * 128:(t + 1) * 128] for t in tiles]
                              + [inp["x_sample"][NSC * cid + s] for s in range(NS)], axis=0)
        rows = np.concatenate([NMETA + t * 128 + ii for t in tiles]) if NL else np.zeros((0,), np.int64)
        mk = np.zeros((128, 4, 128), np.float32)
        for r in range(4):
            if r == j:
                mk[:, r, :] = np.where(ii[:, None] >= ii[None, :], NEG, 0.0)
            elif r > j:
                mk[:, r, :] = NEG
        sel = np.zeros((128, 4), np.float32)
        sel[:, j] = 1.0
        m = dict(shared)
        m.update({
            "xall": c_(xp[:NXT * 128]), "xtok": c_(xtok),
            "st": c_(inp["state_ret"][0, NSC * cid:NSC * cid + NS, :NH]),
            "ropeoc": c_(cc[rows]), "ropeos": c_(ss[rows]),
            "msk": c_(mk.reshape(128, 512)), "sel": sel,
            "ck": c_(inp["cache_sb_k"][0, NSC * cid:NSC * cid + NS, :, :NH].reshape(NS, PAST, NH * 128)),
            "cv": c_(inp["cache_sb_v"][0, NSC * cid:NSC * cid + NS, :, :NH].reshape(NS, PAST, NH * 128)),
        })
        maps.append(m)
    return maps


def kernel(**inputs):
    inp = {k: np.asarray(v) for k, v in inputs.items()}
    nc = bass.Bass("TRN2", target_bir_lowering=False)
    build(nc)
    maps = make_maps(inp)
    res = run_bass_kernel_spmd(nc, maps, core_ids=list(range(8)))
    R = res.results
    NL = 16
    y_p = np.zeros((2, SEQ, D), np.float32)
    y_s = np.zeros((DEC_B, DEC_T, D), np.float32)
    for cid in range(8):
        b, j = cid // 4, cid % 4
        yo = np.asarray(R[cid]["yo"])
        for l in range(NL):
            t = 4 * l + j
            y_p[b, t * 128:(t + 1) * 128] = yo[l * 128:(l + 1) * 128]
        for s in range(NSC):
            y_s[NSC * cid + s] = yo[NL * 128 + s * DEC_T:NL * 128 + (s + 1) * DEC_T]
    kp = np.stack([np.asarray(R[4 * b]["kp"]).reshape(TP, 8, 128) for b in range(2)], 0)[None]
    vp = np.stack([np.asarray(R[4 * b]["vp"]).reshape(TP, 8, 128) for b in range(2)], 0)[None]
    sp = np.stack([np.asarray(R[4 * b]["sp_o"]) for b in range(2)], 0)[None]
    ks = np.concatenate([np.asarray(R[c]["ks"]).reshape(NSC, DEC_T, 8, 128) for c in range(8)], 0)[None]
    vs = np.concatenate([np.asarray(R[c]["vs"]).reshape(NSC, DEC_T, 8, 128) for c in range(8)], 0)[None]
    ss = np.concatenate([np.asarray(R[c]["ss_o"]) for c in range(8)], 0)[None]
    f = lambda a: np.ascontiguousarray(a, dtype=np.float32)
    return (y_p, y_s, f(kp), f(vp), f(sp), f(ks), f(vs), f(ss))
```

```python
import numpy as np
import concourse.bass as bass
import concourse.mybir as mybir
from concourse.bass_utils import run_bass_kernel_spmd

F32 = mybir.dt.float32
BF16 = mybir.dt.bfloat16
AF = mybir.ActivationFunctionType
ALU = mybir.AluOpType

D = 2048
SEQ = 8192
NMETA = 16
TP = SEQ + NMETA
DEC_B = 32
DEC_T = 64
PAST = 2048
NSC = 4
EPS = 1e-6
NEG = -30000.0
SEM_LIMIT = 30000
WKV = 640


class Buf:
    __slots__ = ("name", "w", "r", "dsem", "dcnt", "excl")

    def __init__(self, name, excl=False):
        self.name = name
        self.excl = excl
        self.w = None
        self.r = {}
        self.dsem = None
        self.dcnt = 0


class Ctx:
    def __init__(self, nc):
        self.nc = nc
        self.eng = {"pe": nc.tensor, "act": nc.scalar, "dve": nc.vector,
                    "pool": nc.gpsimd, "sp": nc.sync}
        self.sem = {}
        self.cnt = {}
        self.waited = {}
        self.pend_r = {}
        self.pend_w = {}
        self.nsem = 0
        for k in self.eng:
            self.sem[k] = self._newsem("e_" + k)
            self.cnt[k] = 0
            self.waited[k] = {}
            self.pend_r[k] = []
            self.pend_w[k] = []
        self.out_tokens = {}
        self.dtoks = {}
        self.ninst = 0

    def _newsem(self, name):
        self.nsem += 1
        return self.nc.alloc_semaphore(f"{name}_{self.nsem}")

    def _wait(self, en, tok):
        if tok is None:
            return
        sem, val = tok
        w = self.waited[en]
        if w.get(sem.num, 0) >= val:
            return
        self.eng[en].wait_ge(sem, val)
        w[sem.num] = val

    def _deps(self, en, reads, writes):
        skip = self.sem[en].num if en == "pe" else None
        for b in reads:
            if b.w is not None and b.w[0].num != skip:
                self._wait(en, b.w)
        for b in writes:
            if b.w is not None and b.w[0].num != skip:
                self._wait(en, b.w)
            for t in b.r.values():
                if t[0].num != skip:
                    self._wait(en, t)

    def _commit(self, tok, reads, writes):
        sem, val = tok
        for b in reads:
            b.r[sem.num] = tok
        for b in writes:
            b.w = tok
            b.r = {}

    def op(self, en, fn, reads=(), writes=(), sig=True):
        ex = [b for b in reads if b.excl]
        if ex:
            reads = [b for b in reads if not b.excl]
            writes = list(writes) + ex
        self._deps(en, reads, writes)
        ins = fn(self.eng[en])
        self.ninst += 1
        if not sig:
            self.pend_r[en].extend(reads)
            self.pend_w[en].extend(writes)
            return None
        if self.cnt[en] >= SEM_LIMIT:
            self.sem[en] = self._newsem("e_" + en)
            self.cnt[en] = 0
        self.cnt[en] += 1
        ins.then_inc(self.sem[en], 1)
        tok = (self.sem[en], self.cnt[en])
        self._commit(tok, list(reads) + self.pend_r[en], list(writes) + self.pend_w[en])
        self.pend_r[en] = []
        self.pend_w[en] = []
        return tok

    def dma(self, q, out, in_, reads=(), writes=(), owner=None, is_output=False):
        self._deps(q, reads, writes)
        ins = self.eng[q].dma_start(out=out, in_=in_)
        self.ninst += 1
        if owner.dsem is None or owner.dcnt >= SEM_LIMIT:
            owner.dsem = self._newsem("d_" + owner.name)
            owner.dcnt = 0
        owner.dcnt += 16
        ins.then_inc(owner.dsem, 16)
        tok = (owner.dsem, owner.dcnt)
        self.dtoks[owner.dsem.num] = tok
        self._commit(tok, reads, writes)
        if is_output:
            self.out_tokens[owner.dsem.num] = tok
        return tok

    def barrier(self):
        toks = [(self.sem[k], self.cnt[k]) for k in self.eng if self.cnt[k] > 0] + list(self.dtoks.values())
        for en in self.eng:
            for t in toks:
                if t[0] is self.sem[en]:
                    continue
                self._wait(en, t)

    def finish(self, en="sp"):
        for tok in self.out_tokens.values():
            self._wait(en, tok)


AX = mybir.AxisListType
WALL = 1152
GT = 768


class Ring:
    def __init__(self, aps, name):
        self.t = list(aps)
        self.b = [Buf(f"{name}{i}") for i in range(len(aps))]
        self.i = 0

    def next(self):
        t, b = self.t[self.i], self.b[self.i]
        self.i = (self.i + 1) % len(self.t)
        return t, b


class _Pool:
    def __init__(self, t, size):
        self.t, self.size, self.off = t, size, 0


class Arena:
    def __init__(self, pool, f32):
        self.p, self.f32 = pool, f32

    def reset(self):
        self.p.off = 0

    def get(self, n, b=None):
        p = self.p
        if self.f32:
            p.off += p.off % 2
            a = p.t[:, p.off:p.off + 2 * n].bitcast(F32)
            p.off += 2 * n
        else:
            a = p.t[:, p.off:p.off + n]
            p.off += n
        assert p.off <= p.size, (p.off, p.size)
        if b is not None:
            a = a.rearrange("p (a b) -> p a b", b=b)
        return a


def build(nc, NXT=64, NH=8, NS=NSC, dbg=False):
    c = Ctx(nc)
    dt = nc.dram_tensor
    NL = NXT // 4
    NTOK = NL * 128 + NS * DEC_T
    assert NTOK % 128 == 0
    ntp = NMETA + NXT * 128
    NBLK = max(1 + NXT, 17)
    I = "ExternalInput"
    xall = dt("xall", [NXT * 128, D], F32, kind=I).ap()
    meta = dt("meta", [NMETA, D], F32, kind=I).ap()
    xtok = dt("xtok", [NTOK, D], F32, kind=I).ap()
    g1 = dt("g1", [D], F32, kind=I).ap()
    g2 = dt("g2", [D], F32, kind=I).ap()
    gf = dt("gf", [D], F32, kind=I).ap()
    wh = dt("wh", [NH, D, WALL], F32, kind=I).ap()
    st = dt("st", [NS, NH, 128, 256], F32, kind=I).ap()
    cstf = dt("cstf", [NH, 128, 262], F32, kind=I).ap()
    cstb = dt("cstb", [128, 512], F32, kind=I).ap()
    ropec = dt("ropec", [ntp + DEC_T, 128], F32, kind=I).ap()
    ropes = dt("ropes", [ntp + DEC_T, 128], F32, kind=I).ap()
    ropesmc = dt("ropesmc", [DEC_T, 256], F32, kind=I).ap()
    ropesms = dt("ropesms", [DEC_T, 256], F32, kind=I).ap()
    ropeoc = dt("ropeoc", [NL * 128, 256], F32, kind=I).ap()
    ropeos = dt("ropeos", [NL * 128, 256], F32, kind=I).ap()
    msk = dt("msk", [128, 512], F32, kind=I).ap()
    sel = dt("sel", [128, 4], F32, kind=I).ap()
    ck = dt("ck", [NS, PAST, NH * 128], F32, kind=I).ap()
    cv = dt("cv", [NS, PAST, NH * 128], F32, kind=I).ap()
    wg = dt("wg", [D, 2 * D], F32, kind=I).ap()
    wsbo = dt("wsbo", [NH * 128, D], F32, kind=I).ap()
    wreto = dt("wreto", [NH * 256, D], F32, kind=I).ap()
    wout = dt("wout", [D, D], F32, kind=I).ap()
    wr = dt("wr", [D, 20], F32, kind=I).ap()
    br = dt("br", [20], F32, kind=I).ap()
    wgate = dt("wgate", [16, D, 512], F32, kind=I).ap()
    wup = dt("wup", [16, D, 512], F32, kind=I).ap()
    wdown = dt("wdown", [16, 512, D], F32, kind=I).ap()

    O = "ExternalOutput"
    kp = dt("kp", [ntp, NH * 128], F32, kind=O).ap()
    vp = dt("vp", [ntp, NH * 128], F32, kind=O).ap()
    sp_o = dt("sp_o", [NH, 128, 256], F32, kind=O).ap()
    ks = dt("ks", [NS, DEC_T, NH * 128], F32, kind=O).ap()
    vs = dt("vs", [NS, DEC_T, NH * 128], F32, kind=O).ap()
    ss_o = dt("ss_o", [NS, NH, 128, 256], F32, kind=O).ap()
    yo = dt("yo", [NTOK, D], F32, kind=O).ap()

    SK = O if dbg else "Internal"
    NTL = 1 + NXT + NS
    uts = dt("uts", [NTL, 128, 16 * 128], BF16, kind="Internal").ap()
    uto = dt("uto", [128, 16 * NTOK], BF16, kind="Internal").ap().rearrange("p (k t) -> p k t", t=NTOK)
    osbT = dt("osbT", [NH, 128, NTOK], BF16, kind=SK).ap()
    oretT = dt("oretT", [2 * NH, 128, NTOK], BF16, kind=SK).ap()
    hdbg = dt("hdbg", [NTOK, D], F32, kind=O).ap() if dbg else None
    b_uts = Buf("uts")
    b_osb = Buf("osbT")
    b_oret = Buf("oretT")

    NPOOL = 95000
    pool_ = _Pool(nc.alloc_sbuf_tensor("arena", [128, NPOOL], BF16), NPOOL)
    AFa, ABa = Arena(pool_, True), Arena(pool_, False)

    pT = nc.alloc_psum_tensor("pT", [128, 16, 128], BF16); bpT = Buf("pT", True)
    pA = nc.alloc_psum_tensor("pA", [128, 512], F32); bpA = Buf("pA", True)
    pB = nc.alloc_psum_tensor("pB", [128, 512], F32); bpB = Buf("pB", True)
    pC = nc.alloc_psum_tensor("pC", [128, 512], F32); bpC = Buf("pC", True)
    p6 = nc.alloc_psum_tensor("p6", [128, 512], F32); bp6 = Buf("p6", True)
    p7 = nc.alloc_psum_tensor("p7", [128, 512], F32); bp7 = Buf("p7", True)
    pR = nc.alloc_psum_tensor("pR", [128, 8, 128], BF16); bpR = Buf("pR", True)

    def ring(ar, n, cnt, name, b=None):
        return Ring([ar.get(n, b) for _ in range(cnt)], name)

    CB = ABa.get(512); bCB = Buf("CB")
    gbc = AFa.get(D); bg = Buf("gbc")
    c.dma("sp", gbc, g1.partition_broadcast(128), writes=[bg], owner=bg)
    c.dma("pool", CB, cstb, writes=[bCB], owner=bCB)
    MK = ABa.get(512, 128); bMK = Buf("MK")
    c.dma("pool", MK, msk.rearrange("p (r q) -> p r q", q=128), writes=[bMK], owner=bMK)
    SEL = AFa.get(4); bSEL = Buf("SEL")
    c.dma("sp", SEL, sel, writes=[bSEL], owner=bSEL)
    IDN = CB[:, 0:128]
    NEGM = CB[:, 128:256]
    TRIN = CB[:, 256:384]
    ONESN = CB[:, 384:512]
    NTI = {128: 0, 64: 1, 16: 2}

    xr = ring(AFa, D, 2, "xt")
    st_r = ring(AFa, 4, 2, "ss")
    ur = ring(ABa, D, 2, "u")
    uTr = ring(ABa, 2048, 2, "uT", 128)
    ccr = ring(AFa, 256, 2, "cc")
    ssr = ring(AFa, 256, 2, "sn")
    kvr = ring(AFa, 256, 3, "kvst")
    rfr = ring(AFa, 256, 2, "rf")
    tmr = ring(AFa, 256, 2, "tm")
    swr = ring(AFa, 256, 2, "sw")
    sgr = ring(AFa, 256, 2, "sg")
    vbr = ring(ABa, 256, 2, "vbf")
    kdr = ring(ABa, 128, 2, "kdec")
    k16r = ring(ABa, 128, 2, "k16")
    qkr = ring(ABa, 384, 2, "qk16")
    trr = ring(ABa, 256, 2, "trT", 128)
    scr = ring(ABa, 128, 2, "scm")
    qdr = ring(ABa, 128, 2, "qdec")
    ogr = ring(ABa, 256, 2, "og")
    ogTr = ring(ABa, 256, 2, "ogT", 128)
    Wh = ABa.get(16 * WALL, WALL); bWh = Buf("Wh")
    CF = AFa.get(262); bCF = Buf("CF")
    S = AFa.get(256); bS = Buf("S")
    Sb = ABa.get(256); bSb = Buf("Sb")
    Ssel = ABa.get(max(NL, 1) * 256, 256); bSsel = Buf("Ssel")
    KT = ABa.get(NBLK * 128)
    VA = ABa.get(NBLK * 128, 128)
    QT = ABa.get(max(NL, 1) * 128)
    bKT = [Buf(f"KT{i}") for i in range(NBLK)]
    bVA = [Buf(f"VA{i}") for i in range(NBLK)]
    bQT = [Buf(f"QT{i}") for i in range(max(NL, 1))]
    Er = ring(AFa, 512, 2, "E")
    SPr = ring(ABa, 512, 2, "SP")
    ATr = ring(ABa, 512, 2, "AT")
    Sacc = AFa.get(512); bSacc = Buf("Sacc")
    Saccb = ABa.get(512); bSaccb = Buf("Saccb")
    osr = ring(ABa, 512, 2, "oso")
    ckst = ABa.get(2048, 128); bckst = Buf("ckst")

    def norm_tile(xsrc, nt, dsts):
        xt, bx = xr.next()
        c.dma("sp", xt[:nt, :], xsrc, writes=[bx], owner=bx)
        stt, bst = st_r.next()
        u, bu = ur.next()
        c.op("pool", lambda e: e.memset(stt[:, :], 0.0), writes=[bst])
        c.op("act", lambda e: e.activation(out=u[:nt, :], in_=xt[:nt, :], func=AF.Square,
                                           accum_out=stt[:nt, 0:1]), reads=[bx], writes=[bu, bst])
        c.op("act", lambda e: e.activation(out=stt[:nt, 1:2], in_=stt[:nt, 0:1], func=AF.Ln,
                                           scale=1.0 / D, bias=EPS), reads=[bst], writes=[bst])
        c.op("act", lambda e: e.activation(out=stt[:nt, 1:2], in_=stt[:nt, 1:2], func=AF.Exp,
                                           scale=-0.5), reads=[bst], writes=[bst])
        c.op("dve", lambda e: e.scalar_tensor_tensor(out=u[:nt, :], in0=xt[:nt, :], scalar=stt[:nt, 1:2],
                                                     in1=gbc[:nt, :], op0=ALU.mult, op1=ALU.mult),
             reads=[bx, bst, bg], writes=[bu])
        for j in range(16):
            c.op("pe", lambda e, j=j: e.transpose(out=pT[:, j, :nt], in_=u[:nt, j * 128:(j + 1) * 128],
                                                  identity=IDN[:nt, :nt]),
                 reads=[bu, bCB], writes=[bpT], sig=(j == 15))
        uT, buT = uTr.next()
        c.op("act", lambda e: e.copy(out=uT[:, :, :nt], in_=pT[:, :, :nt]), reads=[bpT], writes=[buT])
        for dst in dsts:
            c.dma("pool", dst, uT[:, :, :nt], reads=[buT], owner=b_uts)

    def uts_ap(idx, nt):
        return uts[idx].rearrange("p (k t) -> p k t", t=128)[:, :, :nt]

    norm_tile(meta, NMETA, [uts_ap(0, NMETA)])
    for i in range(NXT):
        norm_tile(xall[i * 128:(i + 1) * 128, :], 128, [uts_ap(1 + i, 128)])
    for l in range(NL):
        norm_tile(xtok[l * 128:(l + 1) * 128, :], 128, [uto[:, :, l * 128:(l + 1) * 128]])
    for s in range(NS):
        t0 = NL * 128 + s * DEC_T
        norm_tile(xtok[t0:t0 + DEC_T, :], DEC_T, [uts_ap(1 + NXT + s, DEC_T), uto[:, :, t0:t0 + DEC_T]])
    b_uts.w = (b_uts.dsem, b_uts.dcnt)

    def load_head(h):
        for k4 in range(4):
            c.dma("pool", Wh[:, 4 * k4:4 * k4 + 4, :],
                  wh[h, 512 * k4:512 * (k4 + 1), :].rearrange("(k p) n -> p k n", p=128),
                  writes=[bWh], owner=bWh)
        c.dma("sp", CF, cstf[h], writes=[bCF], owner=bCF)

    pA_, bpA_, pB_, bpB_ = pA, bpA, pB, bpB
    sci = [0]

    def scan_tile(idx, nt, rope_row, kout, vout, blk, sel_l=None, sel_r=None):
        (pA, bpA, pB, bpB) = ((pA_, bpA_, pB_, bpB_), (pC, bpC, p6, bp6))[sci[0] % 2]
        sci[0] += 1
        uT, buT = uTr.next()
        c.dma("sp", uT[:, :, :nt], uts_ap(idx, nt), reads=[b_uts], writes=[buT], owner=buT)
        cc, bcc = ccr.next()
        sn, bsn = ssr.next()
        c.dma("sp", cc[:nt, 0:128], ropec[rope_row:rope_row + nt, :], writes=[bcc], owner=bcc)
        c.dma("sp", sn[:nt, 0:128], ropes[rope_row:rope_row + nt, :], writes=[bsn], owner=bsn)
        for (ps, bps, c0, cn) in ((pA, bpA, 0, 256), (pB, bpB, 512, 384)):
            for k in range(16):
                c.op("pe", lambda e, ps=ps, c0=c0, cn=cn, k=k: e.matmul(
                    ps[:nt, :cn], lhsT=uT[:, k, :nt], rhs=Wh[:, k, c0:c0 + cn],
                    start=(k == 0), stop=(k == 15)),
                    reads=[buT, bWh], writes=[bps], sig=(k == 15))
        kv, bkv = kvr.next()
        c.op("act", lambda e: e.copy(out=kv[:nt, 0:128], in_=pA[:nt, 0:128]), reads=[bpA], writes=[bkv])
        c.op("act", lambda e: e.copy(out=kv[:nt, 128:256], in_=pA[:nt, 128:256]), reads=[bpA], writes=[bkv])
        c.dma("pool", kout, kv[:nt, 0:128], reads=[bkv], owner=bkv, is_output=True)
        c.dma("pool", vout, kv[:nt, 128:256], reads=[bkv], owner=bkv, is_output=True)
        k16, bk16 = k16r.next()
        c.op("act", lambda e: e.copy(out=k16[:nt, :], in_=pA[:nt, 0:128]), reads=[bpA], writes=[bk16])
        c.op("dve", lambda e: e.tensor_copy(out=VA[:nt, blk, :], in_=pA[:nt, 128:256]), reads=[bpA], writes=[bVA[blk]])
        c.op("pe", lambda e: e.transpose(out=pR[:, 7, :nt], in_=k16[:nt, :], identity=IDN[:nt, :nt]),
             reads=[bk16, bCB], writes=[bpR])
        c.op("act", lambda e: e.mul(out=KT[:, blk * 128:blk * 128 + nt], in_=pR[:, 7, :nt], mul=128.0 ** -0.5),
             reads=[bpR], writes=[bKT[blk]])
        rf, brf = rfr.next()
        c.op("dve", lambda e: e.tensor_copy(out=rf[:nt, 0:128], in_=pB[:nt, 0:128]), reads=[bpB], writes=[brf])
        vb, bvb = vbr.next()
        c.op("act", lambda e: e.copy(out=vb[:nt, :], in_=pB[:nt, 128:384]), reads=[bpB], writes=[bvb])
        tm, btm = tmr.next()
        sw, bsw = swr.next()
        c.op("dve", lambda e: e.tensor_tensor(out=tm[:nt, 0:128], in0=rf[:nt, 0:128], in1=cc[:nt, 0:128], op=ALU.mult),
             reads=[brf, bcc], writes=[btm])
        c.op("pool", lambda e: e.tensor_tensor(out=sw[:nt, 0:64], in0=rf[:nt, 64:128], in1=sn[:nt, 0:64],
                                               op=ALU.mult), reads=[brf, bsn], writes=[bsw])
        c.op("pool", lambda e: e.tensor_tensor(out=sw[:nt, 64:128], in0=rf[:nt, 0:64], in1=sn[:nt, 64:128],
                                               op=ALU.mult), reads=[brf, bsn], writes=[bsw])
        c.op("dve", lambda e: e.tensor_tensor(out=tm[:nt, 0:128], in0=tm[:nt, 0:128], in1=sw[:nt, 0:128], op=ALU.add),
             reads=[btm, bsw], writes=[btm])
        kd, bkd = kdr.next()
        gk = CF[:nt, 256 + NTI[nt]:257 + NTI[nt]]
        c.op("dve", lambda e: e.tensor_scalar(out=kd[:nt, :], in0=tm[:nt, 0:128], scalar1=gk, scalar2=None,
                                              op0=ALU.mult), reads=[btm, bCF], writes=[bkd])
        if sel_l is not None:
            sc1 = SEL[:, sel_r:sel_r + 1]
            if sel_r == 0:
                c.op("dve", lambda e: e.tensor_scalar(out=Ssel[:, sel_l, :], in0=S, scalar1=sc1, scalar2=None,
                                                      op0=ALU.mult), reads=[bS, bSEL], writes=[bSsel])
            else:
                c.op("dve", lambda e: e.scalar_tensor_tensor(out=Ssel[:, sel_l, :], in0=S, scalar=sc1,
                                                             in1=Ssel[:, sel_l, :], op0=ALU.mult, op1=ALU.add),
                     reads=[bS, bSEL, bSsel], writes=[bSsel])
        c.op("pe", lambda e: e.matmul(p7[:, 0:256], lhsT=kd[:nt, :], rhs=vb[:nt, :], start=True, stop=True),
             reads=[bkd, bvb], writes=[bp7])
        gn = CF[:, 259 + NTI[nt]:260 + NTI[nt]]
        c.op("dve", lambda e: e.scalar_tensor_tensor(out=S, in0=S, scalar=gn, in1=p7[:, 0:256],
                                                     op0=ALU.mult, op1=ALU.add),
             reads=[bS, bCF, bp7], writes=[bS])

    def own_tile(h, usrc, nt, rc_ap, rs_ap, qdst, bqd, Sb_ap, bSb_, tok0):
        uT, buT = uTr.next()
        c.dma("sp", uT[:, :, :nt], usrc, reads=[b_uts], writes=[buT], owner=buT)
        cc, bcc = ccr.next()
        sn, bsn = ssr.next()
        c.dma("sp", cc[:nt, :], rc_ap, writes=[bcc], owner=bcc)
        c.dma("sp", sn[:nt, :], rs_ap, writes=[bsn], owner=bsn)
        for (ps, bps, c0, cn) in ((pA, bpA, 256, 384), (pB, bpB, 640, 512)):
            for k in range(16):
                c.op("pe", lambda e, ps=ps, c0=c0, cn=cn, k=k: e.matmul(
                    ps[:nt, :cn], lhsT=uT[:, k, :nt], rhs=Wh[:, k, c0:c0 + cn],
                    start=(k == 0), stop=(k == 15)),
                    reads=[buT, bWh], writes=[bps], sig=(k == 15))
        qk, bqk = qkr.next()
        c.op("act", lambda e: e.copy(out=qk[:nt, 0:128], in_=pA[:nt, 0:128]), reads=[bpA], writes=[bqk])
        rf, brf = rfr.next()
        c.op("dve", lambda e: e.tensor_copy(out=rf[:nt, :], in_=pA[:nt, 128:384]), reads=[bpA], writes=[brf])
        vb, bvb = vbr.next()
        c.op("act", lambda e: e.copy(out=vb[:nt, :], in_=pB[:nt, 0:256]), reads=[bpB], writes=[bvb])
        sg, bsg = sgr.next()
        c.op("act", lambda e: e.activation(out=sg[:nt, :], in_=pB[:nt, 256:512], func=AF.Exp, scale=-1.0),
             reads=[bpB], writes=[bsg])
        c.op("dve", lambda e: e.tensor_scalar(out=sg[:nt, :], in0=sg[:nt, :], scalar1=1.0, scalar2=None,
                                              op0=ALU.add), reads=[bsg], writes=[bsg])
        c.op("dve", lambda e: e.reciprocal(out=sg[:nt, :], in_=sg[:nt, :]), reads=[bsg], writes=[bsg])
        c.op("dve", lambda e: e.tensor_tensor(out=sg[:nt, :], in0=sg[:nt, :], in1=pB[:nt, 256:512], op=ALU.mult),
             reads=[bsg, bpB], writes=[bsg])
        tm, btm = tmr.next()
        sw, bsw = swr.next()
        c.op("dve", lambda e: e.tensor_tensor(out=tm[:nt, :], in0=rf[:nt, :], in1=cc[:nt, :], op=ALU.mult),
             reads=[brf, bcc], writes=[btm])
        rf4 = rf[:nt, :].rearrange("p (a h d) -> p a h d", a=2, h=2)
        sw4 = sw[:nt, :].rearrange("p (a h d) -> p a h d", a=2, h=2)
        sn4 = sn[:nt, :].rearrange("p (a h d) -> p a h d", a=2, h=2)
        c.op("pool", lambda e: e.tensor_tensor(out=sw4[:, :, 0, :], in0=rf4[:, :, 1, :], in1=sn4[:, :, 0, :],
                                               op=ALU.mult), reads=[brf, bsn], writes=[bsw])
        c.op("pool", lambda e: e.tensor_tensor(out=sw4[:, :, 1, :], in0=rf4[:, :, 0, :], in1=sn4[:, :, 1, :],
                                               op=ALU.mult), reads=[brf, bsn], writes=[bsw])
        c.op("dve", lambda e: e.tensor_tensor(out=qk[:nt, 128:384], in0=tm[:nt, :], in1=sw[:nt, :], op=ALU.add),
             reads=[btm, bsw], writes=[bqk])
        for j in range(3):
            c.op("pe", lambda e, j=j: e.transpose(out=pR[:, j, :nt], in_=qk[:nt, j * 128:(j + 1) * 128],
                                                  identity=IDN[:nt, :nt]),
                 reads=[bqk, bCB], writes=[bpR], sig=(j == 2))
        c.op("act", lambda e: e.copy(out=qdst, in_=pR[:, 0, :nt]), reads=[bpR], writes=[bqd])
        tr, btr = trr.next()
        c.op("dve", lambda e: e.tensor_copy(out=tr[:, :, :nt], in_=pR[:, 1:3, :nt]), reads=[bpR], writes=[btr])
        c.op("pe", lambda e: e.matmul(p6[:nt, 0:nt], lhsT=tr[:, 1, :nt], rhs=tr[:, 0, :nt], start=True, stop=True),
             reads=[btr], writes=[bp6])
        sc, bsc = scr.next()
        c.op("dve", lambda e: e.tensor_tensor(out=sc[:nt, :nt], in0=p6[:nt, 0:nt], in1=CF[:nt, 0:nt], op=ALU.mult),
             reads=[bp6, bCF], writes=[bsc])
        qd, bqdc = qdr.next()
        c.op("pool", lambda e: e.tensor_tensor(out=qd[:, :nt], in0=tr[:, 0, :nt], in1=CF[:, 128:128 + nt], op=ALU.mult),
             reads=[btr, bCF], writes=[bqdc])
        c.op("pe", lambda e: e.matmul(p7[:nt, 0:256], lhsT=sc[:nt, :nt], rhs=vb[:nt, :], start=True, stop=False),
             reads=[bsc, bvb], writes=[bp7], sig=False)
        c.op("pe", lambda e: e.matmul(p7[:nt, 0:256], lhsT=qd[:, :nt], rhs=Sb_ap, start=False, stop=True),
             reads=[bqdc, bSb_], writes=[bp7])
        stt, bst = st_r.next()
        og, bog = ogr.next()
        c.op("pool", lambda e: e.memset(stt[:, :], 0.0), writes=[bst])
        c.op("act", lambda e: e.activation(out=og[:nt, :], in_=p7[:nt, 0:256], func=AF.Square,
                                           accum_out=stt[:nt, 2:3]), reads=[bp7], writes=[bog, bst])
        c.op("act", lambda e: e.activation(out=stt[:nt, 3:4], in_=stt[:nt, 2:3], func=AF.Ln,
                                           scale=1.0 / 256, bias=EPS), reads=[bst], writes=[bst])
        c.op("act", lambda e: e.activation(out=stt[:nt, 3:4], in_=stt[:nt, 3:4], func=AF.Exp,
                                           scale=-0.5), reads=[bst], writes=[bst])
        c.op("dve", lambda e: e.scalar_tensor_tensor(out=og[:nt, :], in0=p7[:nt, 0:256], scalar=stt[:nt, 3:4],
                                                     in1=sg[:nt, :], op0=ALU.mult, op1=ALU.mult),
             reads=[bp7, bst, bsg], writes=[bog])
        for j in range(2):
            c.op("pe", lambda e, j=j: e.transpose(out=pR[:, 4 + j, :nt], in_=og[:nt, j * 128:(j + 1) * 128],
                                                  identity=IDN[:nt, :nt]),
                 reads=[bog, bCB], writes=[bpR], sig=(j == 1))
        ogT, bogT = ogTr.next()
        c.op("act", lambda e: e.copy(out=ogT[:, :, :nt], in_=pR[:, 4:6, :nt]), reads=[bpR], writes=[bogT])
        c.dma("pool", oretT[2 * h:2 * h + 2, :, tok0:tok0 + nt].rearrange("c p t -> p c t"), ogT[:, :, :nt],
              reads=[bogT], owner=b_oret)

    banksZ = [(pA, bpA), (pB, bpB)]
    banksA = [(pC, bpC), (p6, bp6)]
    banksA4 = [(pA, bpA), (pC, bpC), (pB, bpB), (p6, bp6)]
    zi = [0]

    def attn_run(qcols, rdq, keys, ncb, cw, dst):
        NA = ncb * cw
        c.op("pool", lambda e: e.memset(Sacc[:, :NA], 0.0), writes=[bSacc])
        c.op("pool", lambda e: e.memset(Saccb[:, :], 0.0), writes=[bSaccb])
        c.op("pe", lambda e: e.matmul(p7[:, :NA], lhsT=Saccb[:, 0:128], rhs=Saccb[:, :NA], start=True, stop=False),
             reads=[bSaccb], writes=[bp7])
        started = [True] * ncb
        for idx, (ktap, bkt, vaap, bva, nk, cb0, mask) in enumerate(keys):
            last = idx == len(keys) - 1
            c0 = cb0 * cw
            N = NA - c0
            (pa, bpa) = banksA4[zi[0] % 4]
            zi[0] += 1
            c.op("pe", lambda e: e.matmul(pa[:nk, :N], lhsT=ktap, rhs=qcols[:, c0:NA], start=True, stop=False),
                 reads=[bkt] + rdq, writes=[bpa], sig=(mask is None))
            if mask is not None:
                c.op("pe", lambda e: e.matmul(pa[:nk, 0:cw], lhsT=IDN[:nk, :nk], rhs=mask, start=False, stop=False),
                     reads=[bCB, bMK], writes=[bpa])
            E, bE = Er.next()
            c.op("act", lambda e: e.activation(out=E[:nk, :N], in_=pa[:nk, :N], func=AF.Exp),
                 reads=[bpa], writes=[bE])
            SPt, bSP = SPr.next()
            c.op("act", lambda e: e.activation(out=SPt[:nk, :N], in_=E[:nk, :N], func=AF.Ln, bias=1.0),
                 reads=[bE], writes=[bSP])
            c.op("pe", lambda e: e.matmul(pa[:nk, :N], lhsT=TRIN[:nk, :nk], rhs=SPt[:nk, :N],
                                          start=False, stop=(idx == 0)),
                 reads=[bCB, bSP], writes=[bpa], sig=(idx == 0))
            if idx > 0:
                c.op("pe", lambda e: e.matmul(pa[:nk, :N], lhsT=ONESN[:, :nk], rhs=Saccb[:, c0:NA],
                                              start=False, stop=True),
                     reads=[bCB, bSaccb], writes=[bpa])
            if not last:
                c.op("dve", lambda e: e.tensor_tensor(out=Sacc[:nk, c0:NA], in0=Sacc[:nk, c0:NA],
                                                      in1=SPt[:nk, :N], op=ALU.add),
                     reads=[bSacc, bSP], writes=[bSacc])
                c.op("dve", lambda e: e.tensor_copy(out=Saccb[:, c0:NA], in_=Sacc[:, c0:NA]),
                     reads=[bSacc], writes=[bSaccb])
            AT, bAT = ATr.next()
            c.op("act", lambda e: e.activation(out=AT[:nk, :N], in_=pa[:nk, :N], func=AF.Exp),
                 reads=[bpa], writes=[bAT])
            for cb in range(cb0, ncb):
                a0 = (cb - cb0) * cw
                c.op("pe", lambda e, cb=cb, a0=a0: e.matmul(
                    p7[:, cb * cw:(cb + 1) * cw], lhsT=vaap, rhs=AT[:nk, a0:a0 + cw],
                    start=(not started[cb]), stop=last),
                    reads=[bAT, bva], writes=[bp7], sig=(cb == ncb - 1))
                started[cb] = True
        oso, boso = osr.next()
        c.op("dve", lambda e: e.tensor_copy(out=oso[:, :NA], in_=p7[:, :NA]), reads=[bp7], writes=[boso])
        c.dma("pool", dst, oso[:, :NA], reads=[boso], owner=b_osb)

    for h in range(NH):
        load_head(h)
        hc = slice(h * 128, (h + 1) * 128)
        c.op("pool", lambda e: e.memset(S, 0.0), writes=[bS])
        scan_tile(0, NMETA, 0, kp[0:NMETA, hc], vp[0:NMETA, hc], 0)
        for i in range(NXT):
            r0 = NMETA + i * 128
            scan_tile(1 + i, 128, r0, kp[r0:r0 + 128, hc], vp[r0:r0 + 128, hc], 1 + i, sel_l=i // 4, sel_r=i % 4)
        c.dma("pool", sp_o[h], S, reads=[bS], owner=bS, is_output=True)
        for l in range(NL):
            own_tile(h, uto[:, :, l * 128:(l + 1) * 128], 128, ropeoc[l * 128:(l + 1) * 128, :],
                     ropeos[l * 128:(l + 1) * 128, :], QT[:, l * 128:(l + 1) * 128], bQT[l],
                     Ssel[:, l, :], bSsel, l * 128)
        for l0 in range(0, NL, 4):
            l1 = min(l0 + 4, NL)
            keys = []
            for kt in range(4 * (l1 - 1) + 3, -1, -1):
                lp, r = kt // 4, kt % 4
                blk = 1 + kt
                keys.append((KT[:, blk * 128:(blk + 1) * 128], bKT[blk], VA[:, blk, :], bVA[blk], 128,
                             max(lp - l0, 0), MK[:, r, :] if lp >= l0 else None))
            keys.append((KT[:, 0:NMETA], bKT[0], VA[:NMETA, 0, :], bVA[0], NMETA, 0, None))
            attn_run(QT[:, l0 * 128:l1 * 128], [bQT[i] for i in range(l0, l1)], keys, l1 - l0, 128,
                     osbT[h, :, l0 * 128:l1 * 128])
        for s in range(NS):
            tok0 = NL * 128 + s * DEC_T
            c.dma("pool", VA[:, 0:16, :], cv[s, :, hc].rearrange("(a p) d -> p a d", p=128),
                  writes=[bVA[i] for i in range(16)], owner=bVA[0])
            c.dma("pool", ckst, ck[s, :, hc].rearrange("(a p) d -> p a d", p=128), writes=[bckst], owner=bckst)
            for j in range(16):
                c.op("pe", lambda e, j=j: e.transpose(out=pT[:, j, :], in_=ckst[:, j, :], identity=IDN),
                     reads=[bckst, bCB], writes=[bpT], sig=(j == 15))
            c.op("act", lambda e: e.mul(out=KT[:, 0:2048], in_=pT[:].rearrange("p a d -> p (a d)"), mul=128.0 ** -0.5),
                 reads=[bpT], writes=[bKT[i] for i in range(16)])
            c.dma("sp", S, st[s, h], writes=[bS], owner=bS)
            c.op("pool", lambda e: e.tensor_copy(out=Sb, in_=S), reads=[bS], writes=[bSb])
            own_tile(h, uto[:, :, tok0:tok0 + DEC_T], DEC_T, ropesmc[:, :], ropesms[:, :],
                     QT[:, 0:DEC_T], bQT[0], Sb, bSb, tok0)
            scan_tile(1 + NXT + s, DEC_T, ntp, ks[s, :, hc], vs[s, :, hc], 16)
            c.dma("pool", ss_o[s, h], S, reads=[bS], owner=bS, is_output=True)
            keys = [(KT[:, 2048:2048 + DEC_T], bKT[16], VA[:DEC_T, 16, :], bVA[16], DEC_T, 0, NEGM[:DEC_T, :DEC_T])]
            for kb in range(15, -1, -1):
                keys.append((KT[:, kb * 128:(kb + 1) * 128], bKT[kb], VA[:, kb, :], bVA[kb], 128, 0, None))
            attn_run(QT[:, 0:DEC_T], [bQT[0]], keys, 1, DEC_T, osbT[h, :, tok0:tok0 + DEC_T])
    b_osb.w = (b_osb.dsem, b_osb.dcnt)
    b_oret.w = (b_oret.dsem, b_oret.dcnt)

    hs = dt("hs", [NTOK, D], F32, kind=SK).ap()
    b_hs = Buf("hs")
    c.barrier()
    AFa.reset(); ABa.reset()
    CB2 = ABa.get(512); IDN = CB2[:, 0:128]
    GC = 768
    xcr = ring(AFa, 512, 2, "xc")
    gsr = ring(AFa, 512, 4, "gs")
    hcr = ring(AFa, 512, 3, "hc")
    slots = ring(ABa, 8192, 3, "slot")
    actU = ABa.get(16 * GC, GC); bactU = Buf("actU")
    actS = ABa.get(8 * GC, GC); bactS = Buf("actS")
    actR = ABa.get(16 * GC, GC); bactR = Buf("actR")
    MT = ABa.get(16 * GC, GC); bMT = Buf("MT")

    for t0 in range(0, NTOK, GC):
        T = min(GC, NTOK - t0)
        ntl = T // 128
        c.dma("sp", actU[:, :, :T], uto[:, :, t0:t0 + T], reads=[b_uts], writes=[bactU], owner=bactU)
        c.dma("sp", actS[:, :NH, :T], osbT[:, :, t0:t0 + T].rearrange("h p t -> p h t"), reads=[b_osb],
              writes=[bactS], owner=bactS)
        c.dma("sp", actR[:, :2 * NH, :T], oretT[:, :, t0:t0 + T].rearrange("h p t -> p h t"), reads=[b_oret],
              writes=[bactR], owner=bactR)
        for fc in range(16):
            sl, bsl = slots.next()
            fcs = slice(fc * 128, (fc + 1) * 128)
            w1 = sl[:, 0:2048].rearrange("p (a b) -> p a b", b=128)
            w2 = sl[:, 2048:4096].rearrange("p (a b) -> p a b", b=128)
            w3 = sl[:, 4096:4096 + NH * 128].rearrange("p (a b) -> p a b", b=128)
            w4 = sl[:, 6144:6144 + 2 * NH * 128].rearrange("p (a b) -> p a b", b=128)
            c.dma("pool", w1, wg[:, fc * 128:(fc + 1) * 128].rearrange("(k p) n -> p k n", p=128), writes=[bsl], owner=bsl)
            c.dma("pool", w2, wg[:, D + fc * 128:D + (fc + 1) * 128].rearrange("(k p) n -> p k n", p=128), writes=[bsl], owner=bsl)
            c.dma("pool", w3, wsbo[:, fcs].rearrange("(k p) n -> p k n", p=128), writes=[bsl], owner=bsl)
            c.dma("pool", w4, wreto[:, fcs].rearrange("(k p) n -> p k n", p=128), writes=[bsl], owner=bsl)
            for (o, n) in [(o_, min(512, T - o_)) for o_ in range(0, T, 512)]:
                for (ps, bps, w, act, bact, nk_) in ((pA, bpA, w1, actU, bactU, 16), (pB, bpB, w2, actU, bactU, 16),
                                                     (pC, bpC, w3, actS, bactS, NH), (p6, bp6, w4, actR, bactR, 2 * NH)):
                    for k in range(nk_):
                        c.op("pe", lambda e, ps=ps, w=w, act=act, k=k, nk_=nk_: e.matmul(
                            ps[:, :n], lhsT=w[:, k, :], rhs=act[:, k, o:o + n], start=(k == 0), stop=(k == nk_ - 1)),
                            reads=[bsl, bact], writes=[bps], sig=(k == nk_ - 1))
                ga, bga = gsr.next()
                gb, bgb = gsr.next()
                c.op("act", lambda e: e.activation(out=ga[:, :n], in_=pA[:, :n], func=AF.Sigmoid), reads=[bpA], writes=[bga])
                c.op("act", lambda e: e.activation(out=gb[:, :n], in_=pB[:, :n], func=AF.Sigmoid), reads=[bpB], writes=[bgb])
                c.op("dve", lambda e: e.tensor_tensor(out=ga[:, :n], in0=ga[:, :n], in1=pC[:, :n], op=ALU.mult),
                     reads=[bga, bpC], writes=[bga])
                c.op("dve", lambda e: e.tensor_tensor(out=gb[:, :n], in0=gb[:, :n], in1=p6[:, :n], op=ALU.mult),
                     reads=[bgb, bp6], writes=[bgb])
                c.op("dve", lambda e, fc=fc: e.tensor_tensor(out=MT[:, fc, o:o + n], in0=ga[:, :n], in1=gb[:, :n], op=ALU.add),
                     reads=[bga, bgb], writes=[bMT])
        for oc in range(4):
            sl, bsl = slots.next()
            wo = sl[:, 0:8192].rearrange("p (a b) -> p a b", b=512)
            for k4 in range(4):
                c.dma("pool", wo[:, 4 * k4:4 * k4 + 4, :],
                      wout[512 * k4:512 * (k4 + 1), oc * 512:(oc + 1) * 512].rearrange("(k p) n -> p k n", p=128),
                      writes=[bsl], owner=bsl)
            for ti in range(ntl):
                xc, bxc = xcr.next()
                c.dma("sp", xc, xtok[t0 + ti * 128:t0 + (ti + 1) * 128, oc * 512:(oc + 1) * 512], writes=[bxc], owner=bxc)
                (ps, bps) = ((pA, bpA), (pB, bpB))[(oc * ntl + ti) % 2]
                for k in range(16):
                    c.op("pe", lambda e, k=k, ti=ti, ps=ps: e.matmul(ps[:, :], lhsT=MT[:, k, ti * 128:(ti + 1) * 128], rhs=wo[:, k, :],
                                                                     start=(k == 0), stop=(k == 15)),
                         reads=[bMT, bsl], writes=[bps], sig=(k == 15))
                hc_, bhc = hcr.next()
                c.op("dve", lambda e, ps=ps: e.tensor_tensor(out=hc_, in0=ps[:, :], in1=xc, op=ALU.add),
                     reads=[bps, bxc], writes=[bhc])
                c.dma("pool", hs[t0 + ti * 128:t0 + (ti + 1) * 128, oc * 512:(oc + 1) * 512], hc_, reads=[bhc], owner=b_hs,
                      is_output=dbg)
    b_hs.w = (b_hs.dsem, b_hs.dcnt)

    c.barrier()
    AFa.reset(); ABa.reset()
    CB2 = ABa.get(512); IDN = CB2[:, 0:128]
    NTG = GT // 128
    gv = AFa.get(D); bgv = Buf("gv")
    brc = AFa.get(20); bbr = Buf("brc")
    c.dma("sp", brc, br.partition_broadcast(128), writes=[bbr], owner=bbr)
    wrf = AFa.get(320, 20); bwrf = Buf("wrf")
    c.dma("sp", wrf, wr.rearrange("(k p) n -> p k n", p=128), writes=[bwrf], owner=bwrf)
    wrh = ABa.get(320, 20); bwrh = Buf("wrh")
    wrl = ABa.get(320, 20); bwrl = Buf("wrl")
    c.op("act", lambda e: e.copy(out=wrh, in_=wrf), reads=[bwrf], writes=[bwrh])
    c.op("dve", lambda e: e.tensor_tensor(out=wrl, in0=wrf, in1=wrh, op=ALU.subtract), reads=[bwrf, bwrh], writes=[bwrl])
    H = AFa.get(NTG * D, D); bH = [Buf(f"H{i}") for i in range(NTG)]
    Cmb = AFa.get(NTG * 16, 16); bCmb = Buf("Cmb")
    gsr = ring(AFa, 512, 2, "gs2")
    rt = ring(AFa, 64, 2, "rt")
    st2 = ring(AFa, 8, 2, "st2")
    u2f = AFa.get(D); bu2f = Buf("u2f")
    slots = ring(ABa, 8192, 4, "eslot")
    wdeS = ABa.get(8192); bwdS = Buf("wdeS")
    actU = ABa.get(16 * GT, GT); bactU = Buf("u2T")
    u2h = wdeS[:, 0:D]; bu2h = Buf("u2h")
    u2l = wdeS[:, D:2 * D]; bu2l = Buf("u2l")
    loT = wdeS[:, 2 * D:3 * D].rearrange("p (a b) -> p a b", b=128); bloT = Buf("loT")
    hT = ABa.get(4 * GT, GT); bhT = Buf("hT")

    def subgroups(T):
        out, o = [], 0
        while o < T:
            n = min(512, T - o)
            out.append((o, n))
            o += n
        return out

    def rms_tile(ti, gain_buf):
        stt, bst = st2.next()
        c.op("pool", lambda e: e.memset(stt, 0.0), writes=[bst])
        c.op("act", lambda e: e.activation(out=u2f, in_=H[:, ti, :], func=AF.Square, accum_out=stt[:, 0:1]),
             reads=[bH[ti]], writes=[bu2f, bst])
        c.op("act", lambda e: e.activation(out=stt[:, 1:2], in_=stt[:, 0:1], func=AF.Ln, scale=1.0 / D, bias=EPS),
             reads=[bst], writes=[bst])
        c.op("act", lambda e: e.activation(out=stt[:, 1:2], in_=stt[:, 1:2], func=AF.Exp, scale=-0.5),
             reads=[bst], writes=[bst])
        c.op("dve", lambda e: e.scalar_tensor_tensor(out=u2f, in0=H[:, ti, :], scalar=stt[:, 1:2], in1=gv,
                                                     op0=ALU.mult, op1=ALU.mult),
             reads=[bH[ti], bst, bgv], writes=[bu2f])

    for t0 in range(0, NTOK, GT):
        T = min(GT, NTOK - t0)
        ntl = T // 128
        for ti in range(ntl):
            c.dma("sp", H[:, ti, :], hs[t0 + ti * 128:t0 + (ti + 1) * 128, :], reads=[b_hs], writes=[bH[ti]], owner=bH[ti])
        c.dma("sp", gv, g2.partition_broadcast(128), writes=[bgv], owner=bgv)
        for ti in range(ntl):
            rms_tile(ti, gv)
            c.op("act", lambda e: e.copy(out=u2h, in_=u2f), reads=[bu2f], writes=[bu2h])
            c.op("dve", lambda e: e.tensor_tensor(out=u2l, in0=u2f, in1=u2h, op=ALU.subtract),
                 reads=[bu2f, bu2h], writes=[bu2l])
            for j in range(16):
                c.op("pe", lambda e, j=j: e.transpose(out=pT[:, j, :], in_=u2h[:, j * 128:(j + 1) * 128], identity=IDN),
                     reads=[bu2h], writes=[bpT], sig=(j == 15))
            c.op("act", lambda e, ti=ti: e.copy(out=actU[:, :, ti * 128:(ti + 1) * 128], in_=pT[:, :, :]),
                 reads=[bpT], writes=[bactU])
            for j in range(16):
                c.op("pe", lambda e, j=j: e.transpose(out=pT[:, j, :], in_=u2l[:, j * 128:(j + 1) * 128], identity=IDN),
                     reads=[bu2l], writes=[bpT], sig=(j == 15))
            c.op("act", lambda e: e.copy(out=loT, in_=pT[:, :, :]), reads=[bpT], writes=[bloT])
            mm = []
            for k in range(16):
                mm.append((actU[:, k, ti * 128:(ti + 1) * 128], wrh[:, k, :]))
            for k in range(16):
                mm.append((loT[:, k, :], wrh[:, k, :]))
            for k in range(16):
                mm.append((actU[:, k, ti * 128:(ti + 1) * 128], wrl[:, k, :]))
            for i_, (l_, r_) in enumerate(mm):
                c.op("pe", lambda e, l_=l_, r_=r_, i_=i_: e.matmul(pB[:, 0:20], lhsT=l_, rhs=r_, start=(i_ == 0), stop=(i_ == 47)),
                     reads=[bactU, bloT, bwrh, bwrl], writes=[bpB], sig=(i_ == 47))
            R_, bR = rt.next()
            L = R_[:, 0:20]
            c.op("dve", lambda e: e.tensor_tensor(out=L, in0=pB[:, 0:20], in1=brc, op=ALU.add), reads=[bpB, bbr], writes=[bR])
            gmax, gsum, m1, m2 = R_[:, 20:21], R_[:, 21:22], R_[:, 22:23], R_[:, 23:24]
            oh, pen = R_[:, 24:28], R_[:, 28:32]
            elm, mk1 = R_[:, 32:48], R_[:, 48:64]
            R2, bR2 = rt.next()
            elm2, mk2, ge = R2[:, 0:16], R2[:, 16:32], R2[:, 32:36]
            w1_, w2_, e2 = R2[:, 36:37], R2[:, 37:38], R2[:, 38:39]
            rb = [bR, bR2]
            V = lambda fn: c.op("dve", fn, reads=rb, writes=rb)
            V(lambda e: e.tensor_reduce(out=gmax, in_=L[:, 0:4], op=ALU.max, axis=AX.X))
            V(lambda e: e.tensor_scalar(out=oh, in0=L[:, 0:4], scalar1=gmax, scalar2=None, op0=ALU.is_ge))
            V(lambda e: e.tensor_scalar(out=ge, in0=L[:, 0:4], scalar1=gmax, scalar2=None, op0=ALU.subtract))
            c.op("act", lambda e: e.activation(out=ge, in_=ge, func=AF.Exp), reads=rb, writes=rb)
            V(lambda e: e.tensor_reduce(out=gsum, in_=ge, op=ALU.add, axis=AX.X))
            V(lambda e: e.reciprocal(out=gsum, in_=gsum))
            V(lambda e: e.tensor_scalar(out=pen, in0=oh, scalar1=-1.0, scalar2=1e9, op0=ALU.add, op1=ALU.mult))
            for gi in range(4):
                V(lambda e, gi=gi: e.tensor_scalar(out=elm[:, gi * 4:gi * 4 + 4], in0=L[:, 4 + gi * 4:8 + gi * 4],
                                                   scalar1=pen[:, gi:gi + 1], scalar2=None, op0=ALU.add))
            V(lambda e: e.tensor_reduce(out=m1, in_=elm, op=ALU.max, axis=AX.X))
            V(lambda e: e.tensor_scalar(out=mk1, in0=elm, scalar1=m1, scalar2=None, op0=ALU.is_ge))
            V(lambda e: e.scalar_tensor_tensor(out=elm2, in0=mk1, scalar=-1e9, in1=elm, op0=ALU.mult, op1=ALU.add))
            V(lambda e: e.tensor_reduce(out=m2, in_=elm2, op=ALU.max, axis=AX.X))
            V(lambda e: e.tensor_scalar(out=mk2, in0=elm2, scalar1=m2, scalar2=None, op0=ALU.is_ge))
            V(lambda e: e.tensor_tensor(out=e2, in0=m2, in1=m1, op=ALU.subtract))
            c.op("act", lambda e: e.activation(out=e2, in_=e2, func=AF.Exp), reads=rb, writes=rb)
            V(lambda e: e.tensor_scalar(out=w1_, in0=e2, scalar1=1.0, scalar2=None, op0=ALU.add))
            V(lambda e: e.reciprocal(out=w1_, in_=w1_))
            V(lambda e: e.tensor_tensor(out=w1_, in0=w1_, in1=gsum, op=ALU.mult))
            V(lambda e: e.tensor_tensor(out=w2_, in0=w1_, in1=e2, op=ALU.mult))
            V(lambda e: e.tensor_scalar(out=mk1, in0=mk1, scalar1=w1_, scalar2=None, op0=ALU.mult))
            c.op("dve", lambda e, ti=ti: e.scalar_tensor_tensor(out=Cmb[:, ti, :], in0=mk2, scalar=w2_, in1=mk1,
                                                                op0=ALU.mult, op1=ALU.add), reads=rb, writes=rb + [bCmb])
        c.barrier()
        for ex in range(16):
            wge_, bwg = slots.next()
            wue_, bwu = slots.next()
            wde_, bwd = wdeS, bwdS
            wge = wge_.rearrange("p (a b) -> p a b", b=512)
            wue = wue_.rearrange("p (a b) -> p a b", b=512)
            wde = wde_.rearrange("p (a b) -> p a b", b=D)
            for k4 in range(4):
                c.dma("pool", wge[:, 4 * k4:4 * k4 + 4, :], wgate[ex, 512 * k4:512 * (k4 + 1), :].rearrange("(k p) n -> p k n", p=128),
                      writes=[bwg], owner=bwg)
            for k4 in range(4):
                c.dma("pool", wue[:, 4 * k4:4 * k4 + 4, :], wup[ex, 512 * k4:512 * (k4 + 1), :].rearrange("(k p) n -> p k n", p=128),
                      writes=[bwu], owner=bwu)
            c.dma("pool", wde, wdown[ex].rearrange("(k p) n -> p k n", p=128), writes=[bwd], owner=bwd)
            for (o, n) in subgroups(T):
                for fx in range(4):
                    for (ps, bps, w, bw) in ((pA, bpA, wge, bwg), (pB, bpB, wue, bwu)):
                        for k in range(16):
                            c.op("pe", lambda e, ps=ps, w=w, k=k, fx=fx: e.matmul(
                                ps[:, :n], lhsT=w[:, k, fx * 128:(fx + 1) * 128], rhs=actU[:, k, o:o + n],
                                start=(k == 0), stop=(k == 15)),
                                reads=[bw, bactU], writes=[bps], sig=(k == 15))
                    ga, bga = gsr.next()
                    c.op("act", lambda e: e.activation(out=ga[:, :n], in_=pA[:, :n], func=AF.Sigmoid), reads=[bpA], writes=[bga])
                    c.op("dve", lambda e: e.tensor_tensor(out=ga[:, :n], in0=ga[:, :n], in1=pA[:, :n], op=ALU.mult),
                         reads=[bga, bpA], writes=[bga])
                    c.op("dve", lambda e, fx=fx: e.tensor_tensor(out=hT[:, fx, o:o + n], in0=ga[:, :n], in1=pB[:, :n], op=ALU.mult),
                         reads=[bga, bpB], writes=[bhT])
            for ti in range(ntl):
                for oc in range(4):
                    (ps, bps) = ((pC, bpC), (p6, bp6))[(ti * 4 + oc) % 2]
                    for fx in range(4):
                        c.op("pe", lambda e, ps=ps, fx=fx, ti=ti, oc=oc: e.matmul(
                            ps[:, :], lhsT=hT[:, fx, ti * 128:(ti + 1) * 128], rhs=wde[:, fx, oc * 512:(oc + 1) * 512],
                            start=(fx == 0), stop=(fx == 3)),
                            reads=[bhT, bwd], writes=[bps], sig=(fx == 3))
                    c.op("dve", lambda e, ps=ps, ti=ti, oc=oc, ex=ex: e.scalar_tensor_tensor(
                        out=H[:, ti, oc * 512:(oc + 1) * 512], in0=ps[:, :], scalar=Cmb[:, ti, ex:ex + 1],
                        in1=H[:, ti, oc * 512:(oc + 1) * 512], op0=ALU.mult, op1=ALU.add),
                        reads=[bps, bCmb, bH[ti]], writes=[bH[ti]])
        c.barrier()
        c.dma("sp", gv, gf.partition_broadcast(128), writes=[bgv], owner=bgv)
        for ti in range(ntl):
            rms_tile(ti, gv)
            c.dma("sp", yo[t0 + ti * 128:t0 + (ti + 1) * 128, :], u2f, reads=[bu2f], owner=bu2f, is_output=True)
    c.finish()
    return c


def head_consts(h):
    lg = np.log1p(-np.float32(2.0) ** np.float32(-5.0 - h)).astype(np.float32)
    t = np.arange(128, dtype=np.float32)
    rel = t[None, :] - t[:, None]
    dmt = np.where(rel >= 0, np.exp(np.maximum(rel, 0) * lg), 0.0).astype(np.float32)
    gq = np.broadcast_to(np.exp((t + 1.0) * lg)[None, :], (128, 128)).astype(np.float32)
    cf = np.zeros((128, 262), np.float32)
    cf[:, 0:128] = dmt
    cf[:, 128:256] = gq
    for i, nt in enumerate((128, 64, 16)):
        v = np.zeros(128, np.float32)
        v[:nt] = np.exp((nt - 1.0 - t[:nt]) * lg)
        cf[:, 256 + i] = v
        cf[:, 259 + i] = np.exp(np.float32(nt) * lg)
    return cf


def bf_consts():
    cb = np.zeros((128, 512), np.float32)
    i = np.arange(128)
    cb[:, 0:128] = np.eye(128)
    cb[:, 128:256] = np.where(i[:, None] >= i[None, :], NEG, 0.0)
    cb[:, 256:384] = np.where(i[:, None] >= i[None, :], -1.0, 0.0)
    cb[:, 384:512] = -1.0
    return cb


def rope_tables(ntp):
    pos = np.concatenate([np.arange(ntp, dtype=np.float32) - NMETA, PAST + np.arange(DEC_T, dtype=np.float32)])
    inv = (1.0 / (np.float32(10000.0) ** (np.arange(0, 128, 2, dtype=np.float32) / np.float32(128)))).astype(np.float32)
    ang = (pos[:, None] * inv[None, :]).astype(np.float32)
    cs, sn = np.cos(ang).astype(np.float32), np.sin(ang).astype(np.float32)
    s = np.float32(128.0 ** -0.5)
    cc = np.concatenate([cs, cs, cs * s, cs * s], axis=1).astype(np.float32)
    ss = np.concatenate([-sn, sn, -sn * s, sn * s], axis=1).astype(np.float32)
    return np.ascontiguousarray(cc), np.ascontiguousarray(ss)


def wh_cols(h):
    def r(o, w):
        return np.arange(o + h * w, o + (h + 1) * w)
    return np.concatenate([r(1024, 128), r(2048, 128), r(0, 128), r(3072, 128), r(4096, 128),
                           r(5120, 256), r(7168, 256)])


def make_maps(inp, NXT=64, NH=8, NS=NSC, cores=range(8)):
    ntp = NMETA + NXT * 128
    NL = NXT // 4
    cc, ss = rope_tables(ntp)
    cb = bf_consts()
    cf = np.stack([head_consts(h) for h in range(NH)], 0)
    w_in = inp["w_in"][0]
    wh = np.stack([w_in[:, wh_cols(h)] for h in range(NH)], 0)
    c_ = np.ascontiguousarray
    shared = {
        "meta": c_(inp["meta"]), "g1": c_(inp["norm1_g"][0]), "g2": c_(inp["norm2_g"][0]), "gf": c_(inp["normf_g"]),
        "wh": wh, "cstf": cf, "cstb": cb, "ropec": c_(cc[:, 128:256]), "ropes": c_(ss[:, 128:256]),
        "ropesmc": c_(cc[ntp:ntp + DEC_T]), "ropesms": c_(ss[ntp:ntp + DEC_T]),
        "wg": c_(w_in[:, 9216:13312]), "wsbo": c_(inp["w_sb_o"][0][:NH * 128]), "wreto": c_(inp["w_ret_o"][0][:NH * 256]),
        "wout": c_(inp["w_out"][0]),
        "wr": c_(np.concatenate([inp["w_grp"][0], inp["w_exp"][0]], axis=1)),
        "br": c_(np.concatenate([inp["b_grp"][0], inp["b_exp"][0]], axis=0)),
        "wgate": c_(inp["w_gate"][0]), "wup": c_(inp["w_up"][0]), "wdown": c_(inp["w_down"][0]),
    }
    maps = []
    ii = np.arange(128)
    for cid in cores:
        b, j = cid // 4, cid % 4
        tiles = [4 * l + j for l in range(NL)]
        xp = inp["x_prompt"][b]
        xtok = np.concatenate([xp[t * 128:(t + 1) * 128] for t in tiles]
                              + [inp["x_sample"][NSC * cid + s] for s in range(NS)], axis=0)
        rows = np.concatenate([NMETA + t * 128 + ii for t in tiles]) if NL else np.zeros((0,), np.int64)
        mk = np.zeros((128, 4, 128), np.float32)
        for r in range(4):
            if r == j:
                mk[:, r, :] = np.where(ii[:, None] >= ii[None, :], NEG, 0.0)
            elif r > j:
                mk[:, r, :] = NEG
        sel = np.zeros((128, 4), np.float32)
        sel[:, j] = 1.0
        m = dict(shared)
        m.update({
            "xall": c_(xp[:NXT * 128]), "xtok": c_(xtok),
            "st": c_(inp["state_ret"][0, NSC * cid:NSC * cid + NS, :NH]),
            "ropeoc": c_(cc[rows]), "ropeos": c_(ss[rows]),
            "msk": c_(mk.reshape(128, 512)), "sel": sel,
            "ck": c_(inp["cache_sb_k"][0, NSC * cid:NSC * cid + NS, :, :NH].reshape(NS, PAST, NH * 128)),
            "cv": c_(inp["cache_sb_v"][0, NSC * cid:NSC * cid + NS, :, :NH].reshape(NS, PAST, NH * 128)),
        })
        maps.append(m)
    return maps


def kernel(**inputs):
    inp = {k: np.asarray(v) for k, v in inputs.items()}
    nc = bass.Bass("TRN2", target_bir_lowering=False)
    build(nc)
    maps = make_maps(inp)
    res = run_bass_kernel_spmd(nc, maps, core_ids=list(range(8)))
    R = res.results
    NL = 16
    y_p = np.zeros((2, SEQ, D), np.float32)
    y_s = np.zeros((DEC_B, DEC_T, D), np.float32)
    for cid in range(8):
        b, j = cid // 4, cid % 4
        yo = np.asarray(R[cid]["yo"])
        for l in range(NL):
            t = 4 * l + j
            y_p[b, t * 128:(t + 1) * 128] = yo[l * 128:(l + 1) * 128]
        for s in range(NSC):
            y_s[NSC * cid + s] = yo[NL * 128 + s * DEC_T:NL * 128 + (s + 1) * DEC_T]
    kp = np.stack([np.asarray(R[4 * b]["kp"]).reshape(TP, 8, 128) for b in range(2)], 0)[None]
    vp = np.stack([np.asarray(R[4 * b]["vp"]).reshape(TP, 8, 128) for b in range(2)], 0)[None]
    sp = np.stack([np.asarray(R[4 * b]["sp_o"]) for b in range(2)], 0)[None]
    ks = np.concatenate([np.asarray(R[c]["ks"]).reshape(NSC, DEC_T, 8, 128) for c in range(8)], 0)[None]
    vs = np.concatenate([np.asarray(R[c]["vs"]).reshape(NSC, DEC_T, 8, 128) for c in range(8)], 0)[None]
    ss = np.concatenate([np.asarray(R[c]["ss_o"]) for c in range(8)], 0)[None]
    f = lambda a: np.ascontiguousarray(a, dtype=np.float32)
    return (y_p, y_s, f(kp), f(vp), f(sp), f(ks), f(vs), f(ss))
```

```python
import numpy as np
import concourse.bass as bass
import concourse.mybir as mybir
from concourse.bass_utils import run_bass_kernel_spmd

F32 = mybir.dt.float32
BF16 = mybir.dt.bfloat16
AF = mybir.ActivationFunctionType
ALU = mybir.AluOpType

D = 2048
SEQ = 8192
NMETA = 16
TP = SEQ + NMETA
DEC_B = 32
DEC_T = 64
PAST = 2048
NSC = 4
EPS = 1e-6
NEG = -30000.0
SEM_LIMIT = 30000
WKV = 640


class Buf:
    __slots__ = ("name", "w", "r", "dsem", "dcnt", "excl")

    def __init__(self, name, excl=False):
        self.name = name
        self.excl = excl
        self.w = None
        self.r = {}
        self.dsem = None
        self.dcnt = 0


class Ctx:
    def __init__(self, nc):
        self.nc = nc
        self.eng = {"pe": nc.tensor, "act": nc.scalar, "dve": nc.vector,
                    "pool": nc.gpsimd, "sp": nc.sync}
        self.sem = {}
        self.cnt = {}
        self.waited = {}
        self.pend_r = {}
        self.pend_w = {}
        self.nsem = 0
        for k in self.eng:
            self.sem[k] = self._newsem("e_" + k)
            self.cnt[k] = 0
            self.waited[k] = {}
            self.pend_r[k] = []
            self.pend_w[k] = []
        self.out_tokens = {}
        self.dtoks = {}
        self.ninst = 0

    def _newsem(self, name):
        self.nsem += 1
        return self.nc.alloc_semaphore(f"{name}_{self.nsem}")

    def _wait(self, en, tok):
        if tok is None:
            return
        sem, val = tok
        w = self.waited[en]
        if w.get(sem.num, 0) >= val:
            return
        self.eng[en].wait_ge(sem, val)
        w[sem.num] = val

    def _deps(self, en, reads, writes):
        skip = self.sem[en].num if en == "pe" else None
        for b in reads:
            if b.w is not None and b.w[0].num != skip:
                self._wait(en, b.w)
        for b in writes:
            if b.w is not None and b.w[0].num != skip:
                self._wait(en, b.w)
            for t in b.r.values():
                if t[0].num != skip:
                    self._wait(en, t)

    def _commit(self, tok, reads, writes):
        sem, val = tok
        for b in reads:
            b.r[sem.num] = tok
        for b in writes:
            b.w = tok
            b.r = {}

    def op(self, en, fn, reads=(), writes=(), sig=True):
        ex = [b for b in reads if b.excl]
        if ex:
            reads = [b for b in reads if not b.excl]
            writes = list(writes) + ex
        self._deps(en, reads, writes)
        ins = fn(self.eng[en])
        self.ninst += 1
        if not sig:
            self.pend_r[en].extend(reads)
            self.pend_w[en].extend(writes)
            return None
        if self.cnt[en] >= SEM_LIMIT:
            self.sem[en] = self._newsem("e_" + en)
            self.cnt[en] = 0
        self.cnt[en] += 1
        ins.then_inc(self.sem[en], 1)
        tok = (self.sem[en], self.cnt[en])
        self._commit(tok, list(reads) + self.pend_r[en], list(writes) + self.pend_w[en])
        self.pend_r[en] = []
        self.pend_w[en] = []
        return tok

    def dma(self, q, out, in_, reads=(), writes=(), owner=None, is_output=False):
        self._deps(q, reads, writes)
        ins = self.eng[q].dma_start(out=out, in_=in_)
        self.ninst += 1
        if owner.dsem is None or owner.dcnt >= SEM_LIMIT:
            owner.dsem = self._newsem("d_" + owner.name)
            owner.dcnt = 0
        owner.dcnt += 16
        ins.then_inc(owner.dsem, 16)
        tok = (owner.dsem, owner.dcnt)
        self.dtoks[owner.dsem.num] = tok
        self._commit(tok, reads, writes)
        if is_output:
            self.out_tokens[owner.dsem.num] = tok
        return tok

    def barrier(self):
        toks = [(self.sem[k], self.cnt[k]) for k in self.eng if self.cnt[k] > 0] + list(self.dtoks.values())
        for en in self.eng:
            for t in toks:
                if t[0] is self.sem[en]:
                    continue
                self._wait(en, t)

    def finish(self, en="sp"):
        for tok in self.out_tokens.values():
            self._wait(en, tok)


AX = mybir.AxisListType
WALL = 1152
GT = 768


class Ring:
    def __init__(self, aps, name):
        self.t = list(aps)
        self.b = [Buf(f"{name}{i}") for i in range(len(aps))]
        self.i = 0

    def next(self):
        t, b = self.t[self.i], self.b[self.i]
        self.i = (self.i + 1) % len(self.t)
        return t, b


class _Pool:
    def __init__(self, t, size):
        self.t, self.size, self.off = t, size, 0


class Arena:
    def __init__(self, pool, f32):
        self.p, self.f32 = pool, f32

    def reset(self):
        self.p.off = 0

    def get(self, n, b=None):
        p = self.p
        if self.f32:
            p.off += p.off % 2
            a = p.t[:, p.off:p.off + 2 * n].bitcast(F32)
            p.off += 2 * n
        else:
            a = p.t[:, p.off:p.off + n]
            p.off += n
        assert p.off <= p.size, (p.off, p.size)
        if b is not None:
            a = a.rearrange("p (a b) -> p a b", b=b)
        return a


def build(nc, NXT=64, NH=8, NS=NSC, dbg=False):
    c = Ctx(nc)
    dt = nc.dram_tensor
    NL = NXT // 4
    NTOK = NL * 128 + NS * DEC_T
    assert NTOK % 128 == 0
    ntp = NMETA + NXT * 128
    NBLK = max(1 + NXT, 17)
    I = "ExternalInput"
    xall = dt("xall", [NXT * 128, D], F32, kind=I).ap()
    meta = dt("meta", [NMETA, D], F32, kind=I).ap()
    xtok = dt("xtok", [NTOK, D], F32, kind=I).ap()
    g1 = dt("g1", [D], F32, kind=I).ap()
    g2 = dt("g2", [D], F32, kind=I).ap()
    gf = dt("gf", [D], F32, kind=I).ap()
    wh = dt("wh", [NH, D, WALL], F32, kind=I).ap()
    st = dt("st", [NS, NH, 128, 256], F32, kind=I).ap()
    cstf = dt("cstf", [NH, 128, 262], F32, kind=I).ap()
    cstb = dt("cstb", [128, 512], F32, kind=I).ap()
    ropec = dt("ropec", [ntp + DEC_T, 128], F32, kind=I).ap()
    ropes = dt("ropes", [ntp + DEC_T, 128], F32, kind=I).ap()
    ropesmc = dt("ropesmc", [DEC_T, 256], F32, kind=I).ap()
    ropesms = dt("ropesms", [DEC_T, 256], F32, kind=I).ap()
    ropeoc = dt("ropeoc", [NL * 128, 256], F32, kind=I).ap()
    ropeos = dt("ropeos", [NL * 128, 256], F32, kind=I).ap()
    msk = dt("msk", [128, 512], F32, kind=I).ap()
    sel = dt("sel", [128, 4], F32, kind=I).ap()
    ck = dt("ck", [NS, PAST, NH * 128], F32, kind=I).ap()
    cv = dt("cv", [NS, PAST, NH * 128], F32, kind=I).ap()
    wg = dt("wg", [D, 2 * D], F32, kind=I).ap()
    wsbo = dt("wsbo", [NH * 128, D], F32, kind=I).ap()
    wreto = dt("wreto", [NH * 256, D], F32, kind=I).ap()
    wout = dt("wout", [D, D], F32, kind=I).ap()
    wr = dt("wr", [D, 20], F32, kind=I).ap()
    br = dt("br", [20], F32, kind=I).ap()
    wgate = dt("wgate", [16, D, 512], F32, kind=I).ap()
    wup = dt("wup", [16, D, 512], F32, kind=I).ap()
    wdown = dt("wdown", [16, 512, D], F32, kind=I).ap()

    O = "ExternalOutput"
    kp = dt("kp", [ntp, NH * 128], F32, kind=O).ap()
    vp = dt("vp", [ntp, NH * 128], F32, kind=O).ap()
    sp_o = dt("sp_o", [NH, 128, 256], F32, kind=O).ap()
    ks = dt("ks", [NS, DEC_T, NH * 128], F32, kind=O).ap()
    vs = dt("vs", [NS, DEC_T, NH * 128], F32, kind=O).ap()
    ss_o = dt("ss_o", [NS, NH, 128, 256], F32, kind=O).ap()
    yo = dt("yo", [NTOK, D], F32, kind=O).ap()

    SK = O if dbg else "Internal"
    NTL = 1 + NXT + NS
    uts = dt("uts", [NTL, 128, 16 * 128], BF16, kind="Internal").ap()
    uto = dt("uto", [128, 16 * NTOK], BF16, kind="Internal").ap().rearrange("p (k t) -> p k t", t=NTOK)
    osbT = dt("osbT", [NH, 128, NTOK], BF16, kind=SK).ap()
    oretT = dt("oretT", [2 * NH, 128, NTOK], BF16, kind=SK).ap()
    hdbg = dt("hdbg", [NTOK, D], F32, kind=O).ap() if dbg else None
    b_uts = Buf("uts")
    b_osb = Buf("osbT")
    b_oret = Buf("oretT")

    NPOOL = 95000
    pool_ = _Pool(nc.alloc_sbuf_tensor("arena", [128, NPOOL], BF16), NPOOL)
    AFa, ABa = Arena(pool_, True), Arena(pool_, False)

    pT = nc.alloc_psum_tensor("pT", [128, 16, 128], BF16); bpT = Buf("pT", True)
    pA = nc.alloc_psum_tensor("pA", [128, 512], F32); bpA = Buf("pA", True)
    pB = nc.alloc_psum_tensor("pB", [128, 512], F32); bpB = Buf("pB", True)
    pC = nc.alloc_psum_tensor("pC", [128, 512], F32); bpC = Buf("pC", True)
    p6 = nc.alloc_psum_tensor("p6", [128, 512], F32); bp6 = Buf("p6", True)
    p7 = nc.alloc_psum_tensor("p7", [128, 512], F32); bp7 = Buf("p7", True)
    pR = nc.alloc_psum_tensor("pR", [128, 8, 128], BF16); bpR = Buf("pR", True)

    def ring(ar, n, cnt, name, b=None):
        return Ring([ar.get(n, b) for _ in range(cnt)], name)

    CB = ABa.get(512); bCB = Buf("CB")
    gbc = AFa.get(D); bg = Buf("gbc")
    c.dma("sp", gbc, g1.partition_broadcast(128), writes=[bg], owner=bg)
    c.dma("pool", CB, cstb, writes=[bCB], owner=bCB)
    MK = ABa.get(512, 128); bMK = Buf("MK")
    c.dma("pool", MK, msk.rearrange("p (r q) -> p r q", q=128), writes=[bMK], owner=bMK)
    SEL = AFa.get(4); bSEL = Buf("SEL")
    c.dma("sp", SEL, sel, writes=[bSEL], owner=bSEL)
    IDN = CB[:, 0:128]
    NEGM = CB[:, 128:256]
    TRIN = CB[:, 256:384]
    ONESN = CB[:, 384:512]
    NTI = {128: 0, 64: 1, 16: 2}

    xr = ring(AFa, D, 2, "xt")
    st_r = ring(AFa, 4, 2, "ss")
    ur = ring(ABa, D, 2, "u")
    uTr = ring(ABa, 2048, 3, "uT", 128)
    ccr = ring(AFa, 256, 2, "cc")
    ssr = ring(AFa, 256, 2, "sn")
    kvr = ring(AFa, 256, 3, "kvst")
    rfr = ring(AFa, 256, 2, "rf")
    tmr = ring(AFa, 256, 2, "tm")
    swr = ring(AFa, 256, 2, "sw")
    sgr = ring(AFa, 256, 2, "sg")
    vbr = ring(ABa, 256, 2, "vbf")
    kdr = ring(ABa, 128, 2, "kdec")
    k16r = ring(ABa, 128, 2, "k16")
    qkr = ring(ABa, 384, 2, "qk16")
    trr = ring(ABa, 256, 2, "trT", 128)
    scr = ring(ABa, 128, 2, "scm")
    qdr = ring(ABa, 128, 2, "qdec")
    ogr = ring(ABa, 256, 2, "og")
    ogTr = ring(ABa, 256, 2, "ogT", 128)
    Wh = ABa.get(16 * WALL, WALL); bWh = Buf("Wh")
    CF = AFa.get(262); bCF = Buf("CF")
    S = AFa.get(256); bS = Buf("S")
    Sb = ABa.get(256); bSb = Buf("Sb")
    Ssel = ABa.get(max(NL, 1) * 256, 256); bSsel = Buf("Ssel")
    KT = ABa.get(NBLK * 128)
    VA = ABa.get(NBLK * 128, 128)
    QT = ABa.get(max(NL, 1) * 128)
    bKT = [Buf(f"KT{i}") for i in range(NBLK)]
    bVA = [Buf(f"VA{i}") for i in range(NBLK)]
    bQT = [Buf(f"QT{i}") for i in range(max(NL, 1))]
    Er = ring(AFa, 512, 2, "E")
    SPr = ring(ABa, 512, 2, "SP")
    ATr = ring(ABa, 512, 2, "AT")
    Sacc = AFa.get(512); bSacc = Buf("Sacc")
    Saccb = ABa.get(512); bSaccb = Buf("Saccb")
    osr = ring(ABa, 512, 2, "oso")
    ckst = ABa.get(2048, 128); bckst = Buf("ckst")

    def norm_tile(xsrc, nt, dsts):
        xt, bx = xr.next()
        c.dma("sp", xt[:nt, :], xsrc, writes=[bx], owner=bx)
        stt, bst = st_r.next()
        u, bu = ur.next()
        c.op("pool", lambda e: e.memset(stt[:, :], 0.0), writes=[bst])
        c.op("act", lambda e: e.activation(out=u[:nt, :], in_=xt[:nt, :], func=AF.Square,
                                           accum_out=stt[:nt, 0:1]), reads=[bx], writes=[bu, bst])
        c.op("act", lambda e: e.activation(out=stt[:nt, 1:2], in_=stt[:nt, 0:1], func=AF.Ln,
                                           scale=1.0 / D, bias=EPS), reads=[bst], writes=[bst])
        c.op("act", lambda e: e.activation(out=stt[:nt, 1:2], in_=stt[:nt, 1:2], func=AF.Exp,
                                           scale=-0.5), reads=[bst], writes=[bst])
        c.op("dve", lambda e: e.scalar_tensor_tensor(out=u[:nt, :], in0=xt[:nt, :], scalar=stt[:nt, 1:2],
                                                     in1=gbc[:nt, :], op0=ALU.mult, op1=ALU.mult),
             reads=[bx, bst, bg], writes=[bu])
        for j in range(16):
            c.op("pe", lambda e, j=j: e.transpose(out=pT[:, j, :nt], in_=u[:nt, j * 128:(j + 1) * 128],
                                                  identity=IDN[:nt, :nt]),
                 reads=[bu, bCB], writes=[bpT], sig=(j == 15))
        uT, buT = uTr.next()
        c.op("act", lambda e: e.copy(out=uT[:, :, :nt], in_=pT[:, :, :nt]), reads=[bpT], writes=[buT])
        for dst in dsts:
            c.dma("pool", dst, uT[:, :, :nt], reads=[buT], owner=b_uts)

    def uts_ap(idx, nt):
        return uts[idx].rearrange("p (k t) -> p k t", t=128)[:, :, :nt]

    norm_tile(meta, NMETA, [uts_ap(0, NMETA)])
    for i in range(NXT):
        norm_tile(xall[i * 128:(i + 1) * 128, :], 128, [uts_ap(1 + i, 128)])
    for l in range(NL):
        norm_tile(xtok[l * 128:(l + 1) * 128, :], 128, [uto[:, :, l * 128:(l + 1) * 128]])
    for s in range(NS):
        t0 = NL * 128 + s * DEC_T
        norm_tile(xtok[t0:t0 + DEC_T, :], DEC_T, [uts_ap(1 + NXT + s, DEC_T), uto[:, :, t0:t0 + DEC_T]])
    b_uts.w = (b_uts.dsem, b_uts.dcnt)

    def load_head(h):
        for k4 in range(4):
            c.dma("pool", Wh[:, 4 * k4:4 * k4 + 4, :],
                  wh[h, 512 * k4:512 * (k4 + 1), :].rearrange("(k p) n -> p k n", p=128),
                  writes=[bWh], owner=bWh)
        c.dma("sp", CF, cstf[h], writes=[bCF], owner=bCF)

    pA_, bpA_, pB_, bpB_ = pA, bpA, pB, bpB
    sci = [0]

    def scan_tile(idx, nt, rope_row, kout, vout, blk, sel_l=None, sel_r=None):
        (pA, bpA, pB, bpB) = ((pA_, bpA_, pB_, bpB_), (pC, bpC, p6, bp6))[sci[0] % 2]
        sci[0] += 1
        uT, buT = uTr.next()
        c.dma("sp", uT[:, :, :nt], uts_ap(idx, nt), reads=[b_uts], writes=[buT], owner=buT)
        cc, bcc = ccr.next()
        sn, bsn = ssr.next()
        c.dma("sp", cc[:nt, 0:128], ropec[rope_row:rope_row + nt, :], writes=[bcc], owner=bcc)
        c.dma("sp", sn[:nt, 0:128], ropes[rope_row:rope_row + nt, :], writes=[bsn], owner=bsn)
        for (ps, bps, c0, cn) in ((pA, bpA, 0, 256), (pB, bpB, 512, 384)):
            for k in range(16):
                c.op("pe", lambda e, ps=ps, c0=c0, cn=cn, k=k: e.matmul(
                    ps[:nt, :cn], lhsT=uT[:, k, :nt], rhs=Wh[:, k, c0:c0 + cn],
                    start=(k == 0), stop=(k == 15)),
                    reads=[buT, bWh], writes=[bps], sig=(k == 15))
        kv, bkv = kvr.next()
        c.op("act", lambda e: e.copy(out=kv[:nt, 0:128], in_=pA[:nt, 0:128]), reads=[bpA], writes=[bkv])
        c.op("act", lambda e: e.copy(out=kv[:nt, 128:256], in_=pA[:nt, 128:256]), reads=[bpA], writes=[bkv])
        c.dma("pool", kout, kv[:nt, 0:128], reads=[bkv], owner=bkv, is_output=True)
        c.dma("pool", vout, kv[:nt, 128:256], reads=[bkv], owner=bkv, is_output=True)
        k16, bk16 = k16r.next()
        c.op("act", lambda e: e.copy(out=k16[:nt, :], in_=pA[:nt, 0:128]), reads=[bpA], writes=[bk16])
        c.op("dve", lambda e: e.tensor_copy(out=VA[:nt, blk, :], in_=pA[:nt, 128:256]), reads=[bpA], writes=[bVA[blk]])
        c.op("pe", lambda e: e.transpose(out=pR[:, 7, :nt], in_=k16[:nt, :], identity=IDN[:nt, :nt]),
             reads=[bk16, bCB], writes=[bpR])
        c.op("act", lambda e: e.mul(out=KT[:, blk * 128:blk * 128 + nt], in_=pR[:, 7, :nt], mul=128.0 ** -0.5),
             reads=[bpR], writes=[bKT[blk]])
        rf, brf = rfr.next()
        c.op("dve", lambda e: e.tensor_copy(out=rf[:nt, 0:128], in_=pB[:nt, 0:128]), reads=[bpB], writes=[brf])
        vb, bvb = vbr.next()
        c.op("act", lambda e: e.copy(out=vb[:nt, :], in_=pB[:nt, 128:384]), reads=[bpB], writes=[bvb])
        tm, btm = tmr.next()
        sw, bsw = swr.next()
        c.op("dve", lambda e: e.tensor_tensor(out=tm[:nt, 0:128], in0=rf[:nt, 0:128], in1=cc[:nt, 0:128], op=ALU.mult),
             reads=[brf, bcc], writes=[btm])
        c.op("pool", lambda e: e.tensor_tensor(out=sw[:nt, 0:64], in0=rf[:nt, 64:128], in1=sn[:nt, 0:64],
                                               op=ALU.mult), reads=[brf, bsn], writes=[bsw])
        c.op("pool", lambda e: e.tensor_tensor(out=sw[:nt, 64:128], in0=rf[:nt, 0:64], in1=sn[:nt, 64:128],
                                               op=ALU.mult), reads=[brf, bsn], writes=[bsw])
        c.op("dve", lambda e: e.tensor_tensor(out=tm[:nt, 0:128], in0=tm[:nt, 0:128], in1=sw[:nt, 0:128], op=ALU.add),
             reads=[btm, bsw], writes=[btm])
        kd, bkd = kdr.next()
        gk = CF[:nt, 256 + NTI[nt]:257 + NTI[nt]]
        c.op("dve", lambda e: e.tensor_scalar(out=kd[:nt, :], in0=tm[:nt, 0:128], scalar1=gk, scalar2=None,
                                              op0=ALU.mult), reads=[btm, bCF], writes=[bkd])
        if sel_l is not None:
            sc1 = SEL[:, sel_r:sel_r + 1]
            if sel_r == 0:
                c.op("dve", lambda e: e.tensor_scalar(out=Ssel[:, sel_l, :], in0=S, scalar1=sc1, scalar2=None,
                                                      op0=ALU.mult), reads=[bS, bSEL], writes=[bSsel])
            else:
                c.op("dve", lambda e: e.scalar_tensor_tensor(out=Ssel[:, sel_l, :], in0=S, scalar=sc1,
                                                             in1=Ssel[:, sel_l, :], op0=ALU.mult, op1=ALU.add),
                     reads=[bS, bSEL, bSsel], writes=[bSsel])
        c.op("pe", lambda e: e.matmul(p7[:, 0:256], lhsT=kd[:nt, :], rhs=vb[:nt, :], start=True, stop=True),
             reads=[bkd, bvb], writes=[bp7])
        gn = CF[:, 259 + NTI[nt]:260 + NTI[nt]]
        c.op("dve", lambda e: e.scalar_tensor_tensor(out=S, in0=S, scalar=gn, in1=p7[:, 0:256],
                                                     op0=ALU.mult, op1=ALU.add),
             reads=[bS, bCF, bp7], writes=[bS])

    def own_tile(h, usrc, nt, rc_ap, rs_ap, qdst, bqd, Sb_ap, bSb_, tok0):
        uT, buT = uTr.next()
        c.dma("sp", uT[:, :, :nt], usrc, reads=[b_uts], writes=[buT], owner=buT)
        cc, bcc = ccr.next()
        sn, bsn = ssr.next()
        c.dma("sp", cc[:nt, :], rc_ap, writes=[bcc], owner=bcc)
        c.dma("sp", sn[:nt, :], rs_ap, writes=[bsn], owner=bsn)
        for (ps, bps, c0, cn) in ((pA, bpA, 256, 384), (pB, bpB, 640, 512)):
            for k in range(16):
                c.op("pe", lambda e, ps=ps, c0=c0, cn=cn, k=k: e.matmul(
                    ps[:nt, :cn], lhsT=uT[:, k, :nt], rhs=Wh[:, k, c0:c0 + cn],
                    start=(k == 0), stop=(k == 15)),
                    reads=[buT, bWh], writes=[bps], sig=(k == 15))
        qk, bqk = qkr.next()
        c.op("act", lambda e: e.copy(out=qk[:nt, 0:128], in_=pA[:nt, 0:128]), reads=[bpA], writes=[bqk])
        rf, brf = rfr.next()
        c.op("dve", lambda e: e.tensor_copy(out=rf[:nt, :], in_=pA[:nt, 128:384]), reads=[bpA], writes=[brf])
        vb, bvb = vbr.next()
        c.op("act", lambda e: e.copy(out=vb[:nt, :], in_=pB[:nt, 0:256]), reads=[bpB], writes=[bvb])
        sg, bsg = sgr.next()
        c.op("act", lambda e: e.activation(out=sg[:nt, :], in_=pB[:nt, 256:512], func=AF.Exp, scale=-1.0),
             reads=[bpB], writes=[bsg])
        c.op("dve", lambda e: e.tensor_scalar(out=sg[:nt, :], in0=sg[:nt, :], scalar1=1.0, scalar2=None,
                                              op0=ALU.add), reads=[bsg], writes=[bsg])
        c.op("dve", lambda e: e.reciprocal(out=sg[:nt, :], in_=sg[:nt, :]), reads=[bsg], writes=[bsg])
        c.op("dve", lambda e: e.tensor_tensor(out=sg[:nt, :], in0=sg[:nt, :], in1=pB[:nt, 256:512], op=ALU.mult),
             reads=[bsg, bpB], writes=[bsg])
        tm, btm = tmr.next()
        sw, bsw = swr.next()
        c.op("dve", lambda e: e.tensor_tensor(out=tm[:nt, :], in0=rf[:nt, :], in1=cc[:nt, :], op=ALU.mult),
             reads=[brf, bcc], writes=[btm])
        rf4 = rf[:nt, :].rearrange("p (a h d) -> p a h d", a=2, h=2)
        sw4 = sw[:nt, :].rearrange("p (a h d) -> p a h d", a=2, h=2)
        sn4 = sn[:nt, :].rearrange("p (a h d) -> p a h d", a=2, h=2)
        c.op("pool", lambda e: e.tensor_tensor(out=sw4[:, :, 0, :], in0=rf4[:, :, 1, :], in1=sn4[:, :, 0, :],
                                               op=ALU.mult), reads=[brf, bsn], writes=[bsw])
        c.op("pool", lambda e: e.tensor_tensor(out=sw4[:, :, 1, :], in0=rf4[:, :, 0, :], in1=sn4[:, :, 1, :],
                                               op=ALU.mult), reads=[brf, bsn], writes=[bsw])
        c.op("dve", lambda e: e.tensor_tensor(out=qk[:nt, 128:384], in0=tm[:nt, :], in1=sw[:nt, :], op=ALU.add),
             reads=[btm, bsw], writes=[bqk])
        for j in range(3):
            c.op("pe", lambda e, j=j: e.transpose(out=pR[:, j, :nt], in_=qk[:nt, j * 128:(j + 1) * 128],
                                                  identity=IDN[:nt, :nt]),
                 reads=[bqk, bCB], writes=[bpR], sig=(j == 2))
        c.op("act", lambda e: e.copy(out=qdst, in_=pR[:, 0, :nt]), reads=[bpR], writes=[bqd])
        tr, btr = trr.next()
        c.op("dve", lambda e: e.tensor_copy(out=tr[:, :, :nt], in_=pR[:, 1:3, :nt]), reads=[bpR], writes=[btr])
        c.op("pe", lambda e: e.matmul(p6[:nt, 0:nt], lhsT=tr[:, 1, :nt], rhs=tr[:, 0, :nt], start=True, stop=True),
             reads=[btr], writes=[bp6])
        sc, bsc = scr.next()
        c.op("dve", lambda e: e.tensor_tensor(out=sc[:nt, :nt], in0=p6[:nt, 0:nt], in1=CF[:nt, 0:nt], op=ALU.mult),
             reads=[bp6, bCF], writes=[bsc])
        qd, bqdc = qdr.next()
        c.op("pool", lambda e: e.tensor_tensor(out=qd[:, :nt], in0=tr[:, 0, :nt], in1=CF[:, 128:128 + nt], op=ALU.mult),
             reads=[btr, bCF], writes=[bqdc])
        c.op("pe", lambda e: e.matmul(p7[:nt, 0:256], lhsT=sc[:nt, :nt], rhs=vb[:nt, :], start=True, stop=False),
             reads=[bsc, bvb], writes=[bp7], sig=False)
        c.op("pe", lambda e: e.matmul(p7[:nt, 0:256], lhsT=qd[:, :nt], rhs=Sb_ap, start=False, stop=True),
             reads=[bqdc, bSb_], writes=[bp7])
        stt, bst = st_r.next()
        og, bog = ogr.next()
        c.op("pool", lambda e: e.memset(stt[:, :], 0.0), writes=[bst])
        c.op("act", lambda e: e.activation(out=og[:nt, :], in_=p7[:nt, 0:256], func=AF.Square,
                                           accum_out=stt[:nt, 2:3]), reads=[bp7], writes=[bog, bst])
        c.op("act", lambda e: e.activation(out=stt[:nt, 3:4], in_=stt[:nt, 2:3], func=AF.Ln,
                                           scale=1.0 / 256, bias=EPS), reads=[bst], writes=[bst])
        c.op("act", lambda e: e.activation(out=stt[:nt, 3:4], in_=stt[:nt, 3:4], func=AF.Exp,
                                           scale=-0.5), reads=[bst], writes=[bst])
        c.op("dve", lambda e: e.scalar_tensor_tensor(out=og[:nt, :], in0=p7[:nt, 0:256], scalar=stt[:nt, 3:4],
                                                     in1=sg[:nt, :], op0=ALU.mult, op1=ALU.mult),
             reads=[bp7, bst, bsg], writes=[bog])
        for j in range(2):
            c.op("pe", lambda e, j=j: e.transpose(out=pR[:, 4 + j, :nt], in_=og[:nt, j * 128:(j + 1) * 128],
                                                  identity=IDN[:nt, :nt]),
                 reads=[bog, bCB], writes=[bpR], sig=(j == 1))
        ogT, bogT = ogTr.next()
        c.op("act", lambda e: e.copy(out=ogT[:, :, :nt], in_=pR[:, 4:6, :nt]), reads=[bpR], writes=[bogT])
        c.dma("pool", oretT[2 * h:2 * h + 2, :, tok0:tok0 + nt].rearrange("c p t -> p c t"), ogT[:, :, :nt],
              reads=[bogT], owner=b_oret)

    banksZ = [(pA, bpA), (pB, bpB)]
    banksA = [(pC, bpC), (p6, bp6)]
    banksA4 = [(pA, bpA), (pC, bpC), (pB, bpB), (p6, bp6)]
    zi = [0]

    def attn_run(qcols, rdq, keys, ncb, cw, dst):
        NA = ncb * cw
        c.op("pool", lambda e: e.memset(Sacc[:, :NA], 0.0), writes=[bSacc])
        c.op("pool", lambda e: e.memset(Saccb[:, :], 0.0), writes=[bSaccb])
        c.op("pe", lambda e: e.matmul(p7[:, :NA], lhsT=Saccb[:, 0:128], rhs=Saccb[:, :NA], start=True, stop=False),
             reads=[bSaccb], writes=[bp7])
        started = [True] * ncb
        for idx, (ktap, bkt, vaap, bva, nk, cb0, mask) in enumerate(keys):
            last = idx == len(keys) - 1
            c0 = cb0 * cw
            N = NA - c0
            (pa, bpa) = banksA4[zi[0] % 4]
            zi[0] += 1
            c.op("pe", lambda e: e.matmul(pa[:nk, :N], lhsT=ktap, rhs=qcols[:, c0:NA], start=True, stop=False),
                 reads=[bkt] + rdq, writes=[bpa], sig=(mask is None))
            if mask is not None:
                c.op("pe", lambda e: e.matmul(pa[:nk, 0:cw], lhsT=IDN[:nk, :nk], rhs=mask, start=False, stop=False),
                     reads=[bCB, bMK], writes=[bpa])
            E, bE = Er.next()
            c.op("act", lambda e: e.activation(out=E[:nk, :N], in_=pa[:nk, :N], func=AF.Exp),
                 reads=[bpa], writes=[bE])
            SPt, bSP = SPr.next()
            c.op("act", lambda e: e.activation(out=SPt[:nk, :N], in_=E[:nk, :N], func=AF.Ln, bias=1.0),
                 reads=[bE], writes=[bSP])
            c.op("pe", lambda e: e.matmul(pa[:nk, :N], lhsT=TRIN[:nk, :nk], rhs=SPt[:nk, :N],
                                          start=False, stop=(idx == 0)),
                 reads=[bCB, bSP], writes=[bpa], sig=(idx == 0))
            if idx > 0:
                c.op("pe", lambda e: e.matmul(pa[:nk, :N], lhsT=ONESN[:, :nk], rhs=Saccb[:, c0:NA],
                                              start=False, stop=True),
                     reads=[bCB, bSaccb], writes=[bpa])
            if not last:
                c.op("dve", lambda e: e.tensor_tensor(out=Sacc[:nk, c0:NA], in0=Sacc[:nk, c0:NA],
                                                      in1=SPt[:nk, :N], op=ALU.add),
                     reads=[bSacc, bSP], writes=[bSacc])
                c.op("dve", lambda e: e.tensor_copy(out=Saccb[:, c0:NA], in_=Sacc[:, c0:NA]),
                     reads=[bSacc], writes=[bSaccb])
            AT, bAT = ATr.next()
            c.op("act", lambda e: e.activation(out=AT[:nk, :N], in_=pa[:nk, :N], func=AF.Exp),
                 reads=[bpa], writes=[bAT])
            for cb in range(cb0, ncb):
                a0 = (cb - cb0) * cw
                c.op("pe", lambda e, cb=cb, a0=a0: e.matmul(
                    p7[:, cb * cw:(cb + 1) * cw], lhsT=vaap, rhs=AT[:nk, a0:a0 + cw],
                    start=(not started[cb]), stop=last),
                    reads=[bAT, bva], writes=[bp7], sig=(cb == ncb - 1))
                started[cb] = True
        oso, boso = osr.next()
        c.op("dve", lambda e: e.tensor_copy(out=oso[:, :NA], in_=p7[:, :NA]), reads=[bp7], writes=[boso])
        c.dma("pool", dst, oso[:, :NA], reads=[boso], owner=b_osb)

    for h in range(NH):
        load_head(h)
        hc = slice(h * 128, (h + 1) * 128)
        c.op("pool", lambda e: e.memset(S, 0.0), writes=[bS])
        scan_tile(0, NMETA, 0, kp[0:NMETA, hc], vp[0:NMETA, hc], 0)
        for i in range(NXT):
            r0 = NMETA + i * 128
            scan_tile(1 + i, 128, r0, kp[r0:r0 + 128, hc], vp[r0:r0 + 128, hc], 1 + i, sel_l=i // 4, sel_r=i % 4)
        c.dma("pool", sp_o[h], S, reads=[bS], owner=bS, is_output=True)
        for l in range(NL):
            own_tile(h, uto[:, :, l * 128:(l + 1) * 128], 128, ropeoc[l * 128:(l + 1) * 128, :],
                     ropeos[l * 128:(l + 1) * 128, :], QT[:, l * 128:(l + 1) * 128], bQT[l],
                     Ssel[:, l, :], bSsel, l * 128)
        for l0 in range(0, NL, 4):
            l1 = min(l0 + 4, NL)
            keys = []
            for kt in range(4 * (l1 - 1) + 3, -1, -1):
                lp, r = kt // 4, kt % 4
                blk = 1 + kt
                keys.append((KT[:, blk * 128:(blk + 1) * 128], bKT[blk], VA[:, blk, :], bVA[blk], 128,
                             max(lp - l0, 0), MK[:, r, :] if lp >= l0 else None))
            keys.append((KT[:, 0:NMETA], bKT[0], VA[:NMETA, 0, :], bVA[0], NMETA, 0, None))
            attn_run(QT[:, l0 * 128:l1 * 128], [bQT[i] for i in range(l0, l1)], keys, l1 - l0, 128,
                     osbT[h, :, l0 * 128:l1 * 128])
        for s in range(NS):
            tok0 = NL * 128 + s * DEC_T
            c.dma("pool", VA[:, 0:16, :], cv[s, :, hc].rearrange("(a p) d -> p a d", p=128),
                  writes=[bVA[i] for i in range(16)], owner=bVA[0])
            c.dma("pool", ckst, ck[s, :, hc].rearrange("(a p) d -> p a d", p=128), writes=[bckst], owner=bckst)
            for j in range(16):
                c.op("pe", lambda e, j=j: e.transpose(out=pT[:, j, :], in_=ckst[:, j, :], identity=IDN),
                     reads=[bckst, bCB], writes=[bpT], sig=(j == 15))
            c.op("act", lambda e: e.mul(out=KT[:, 0:2048], in_=pT[:].rearrange("p a d -> p (a d)"), mul=128.0 ** -0.5),
                 reads=[bpT], writes=[bKT[i] for i in range(16)])
            c.dma("sp", S, st[s, h], writes=[bS], owner=bS)
            c.op("pool", lambda e: e.tensor_copy(out=Sb, in_=S), reads=[bS], writes=[bSb])
            own_tile(h, uto[:, :, tok0:tok0 + DEC_T], DEC_T, ropesmc[:, :], ropesms[:, :],
                     QT[:, 0:DEC_T], bQT[0], Sb, bSb, tok0)
            scan_tile(1 + NXT + s, DEC_T, ntp, ks[s, :, hc], vs[s, :, hc], 16)
            c.dma("pool", ss_o[s, h], S, reads=[bS], owner=bS, is_output=True)
            keys = [(KT[:, 2048:2048 + DEC_T], bKT[16], VA[:DEC_T, 16, :], bVA[16], DEC_T, 0, NEGM[:DEC_T, :DEC_T])]
            for kb in range(15, -1, -1):
                keys.append((KT[:, kb * 128:(kb + 1) * 128], bKT[kb], VA[:, kb, :], bVA[kb], 128, 0, None))
            attn_run(QT[:, 0:DEC_T], [bQT[0]], keys, 1, DEC_T, osbT[h, :, tok0:tok0 + DEC_T])
    b_osb.w = (b_osb.dsem, b_osb.dcnt)
    b_oret.w = (b_oret.dsem, b_oret.dcnt)

    hs = dt("hs", [NTOK, D], F32, kind=SK).ap()
    b_hs = Buf("hs")
    c.barrier()
    AFa.reset(); ABa.reset()
    CB2 = ABa.get(512); IDN = CB2[:, 0:128]
    GC = 768
    xcr = ring(AFa, 512, 2, "xc")
    gsr = ring(AFa, 512, 4, "gs")
    hcr = ring(AFa, 512, 3, "hc")
    slots = ring(ABa, 8192, 5, "slot")
    actU = ABa.get(16 * GC, GC); bactU = Buf("actU")
    actS = ABa.get(8 * GC, GC); bactS = Buf("actS")
    actR = ABa.get(16 * GC, GC); bactR = Buf("actR")
    MT = ABa.get(16 * GC, GC); bMT = Buf("MT")

    for t0 in range(0, NTOK, GC):
        T = min(GC, NTOK - t0)
        ntl = T // 128
        c.dma("sp", actU[:, :, :T], uto[:, :, t0:t0 + T], reads=[b_uts], writes=[bactU], owner=bactU)
        c.dma("sp", actS[:, :NH, :T], osbT[:, :, t0:t0 + T].rearrange("h p t -> p h t"), reads=[b_osb],
              writes=[bactS], owner=bactS)
        c.dma("sp", actR[:, :2 * NH, :T], oretT[:, :, t0:t0 + T].rearrange("h p t -> p h t"), reads=[b_oret],
              writes=[bactR], owner=bactR)
        for fc in range(16):
            sl, bsl = slots.next()
            fcs = slice(fc * 128, (fc + 1) * 128)
            w1 = sl[:, 0:2048].rearrange("p (a b) -> p a b", b=128)
            w2 = sl[:, 2048:4096].rearrange("p (a b) -> p a b", b=128)
            w3 = sl[:, 4096:4096 + NH * 128].rearrange("p (a b) -> p a b", b=128)
            w4 = sl[:, 6144:6144 + 2 * NH * 128].rearrange("p (a b) -> p a b", b=128)
            c.dma("pool", w1, wg[:, fc * 128:(fc + 1) * 128].rearrange("(k p) n -> p k n", p=128), writes=[bsl], owner=bsl)
            c.dma("pool", w2, wg[:, D + fc * 128:D + (fc + 1) * 128].rearrange("(k p) n -> p k n", p=128), writes=[bsl], owner=bsl)
            c.dma("pool", w3, wsbo[:, fcs].rearrange("(k p) n -> p k n", p=128), writes=[bsl], owner=bsl)
            c.dma("pool", w4, wreto[:, fcs].rearrange("(k p) n -> p k n", p=128), writes=[bsl], owner=bsl)
            for (o, n) in [(o_, min(512, T - o_)) for o_ in range(0, T, 512)]:
                for (ps, bps, w, act, bact, nk_) in ((pA, bpA, w1, actU, bactU, 16), (pB, bpB, w2, actU, bactU, 16),
                                                     (pC, bpC, w3, actS, bactS, NH), (p6, bp6, w4, actR, bactR, 2 * NH)):
                    for k in range(nk_):
                        c.op("pe", lambda e, ps=ps, w=w, act=act, k=k, nk_=nk_: e.matmul(
                            ps[:, :n], lhsT=w[:, k, :], rhs=act[:, k, o:o + n], start=(k == 0), stop=(k == nk_ - 1)),
                            reads=[bsl, bact], writes=[bps], sig=(k == nk_ - 1))
                ga, bga = gsr.next()
                gb, bgb = gsr.next()
                c.op("act", lambda e: e.activation(out=ga[:, :n], in_=pA[:, :n], func=AF.Sigmoid), reads=[bpA], writes=[bga])
                c.op("act", lambda e: e.activation(out=gb[:, :n], in_=pB[:, :n], func=AF.Sigmoid), reads=[bpB], writes=[bgb])
                c.op("dve", lambda e: e.tensor_tensor(out=ga[:, :n], in0=ga[:, :n], in1=pC[:, :n], op=ALU.mult),
                     reads=[bga, bpC], writes=[bga])
                c.op("dve", lambda e: e.tensor_tensor(out=gb[:, :n], in0=gb[:, :n], in1=p6[:, :n], op=ALU.mult),
                     reads=[bgb, bp6], writes=[bgb])
                c.op("dve", lambda e, fc=fc: e.tensor_tensor(out=MT[:, fc, o:o + n], in0=ga[:, :n], in1=gb[:, :n], op=ALU.add),
                     reads=[bga, bgb], writes=[bMT])
        for oc in range(4):
            sl, bsl = slots.next()
            wo = sl[:, 0:8192].rearrange("p (a b) -> p a b", b=512)
            for k4 in range(4):
                c.dma("pool", wo[:, 4 * k4:4 * k4 + 4, :],
                      wout[512 * k4:512 * (k4 + 1), oc * 512:(oc + 1) * 512].rearrange("(k p) n -> p k n", p=128),
                      writes=[bsl], owner=bsl)
            for ti in range(ntl):
                xc, bxc = xcr.next()
                c.dma("sp", xc, xtok[t0 + ti * 128:t0 + (ti + 1) * 128, oc * 512:(oc + 1) * 512], writes=[bxc], owner=bxc)
                (ps, bps) = ((pA, bpA), (pB, bpB))[(oc * ntl + ti) % 2]
                for k in range(16):
                    c.op("pe", lambda e, k=k, ti=ti, ps=ps: e.matmul(ps[:, :], lhsT=MT[:, k, ti * 128:(ti + 1) * 128], rhs=wo[:, k, :],
                                                                     start=(k == 0), stop=(k == 15)),
                         reads=[bMT, bsl], writes=[bps], sig=(k == 15))
                hc_, bhc = hcr.next()
                c.op("dve", lambda e, ps=ps: e.tensor_tensor(out=hc_, in0=ps[:, :], in1=xc, op=ALU.add),
                     reads=[bps, bxc], writes=[bhc])
                c.dma("pool", hs[t0 + ti * 128:t0 + (ti + 1) * 128, oc * 512:(oc + 1) * 512], hc_, reads=[bhc], owner=b_hs,
                      is_output=dbg)
    b_hs.w = (b_hs.dsem, b_hs.dcnt)

    c.barrier()
    AFa.reset(); ABa.reset()
    CB2 = ABa.get(512); IDN = CB2[:, 0:128]
    NTG = GT // 128
    gv = AFa.get(D); bgv = Buf("gv")
    brc = AFa.get(20); bbr = Buf("brc")
    c.dma("sp", brc, br.partition_broadcast(128), writes=[bbr], owner=bbr)
    wrf = AFa.get(320, 20); bwrf = Buf("wrf")
    c.dma("sp", wrf, wr.rearrange("(k p) n -> p k n", p=128), writes=[bwrf], owner=bwrf)
    wrh = ABa.get(320, 20); bwrh = Buf("wrh")
    wrl = ABa.get(320, 20); bwrl = Buf("wrl")
    c.op("act", lambda e: e.copy(out=wrh, in_=wrf), reads=[bwrf], writes=[bwrh])
    c.op("dve", lambda e: e.tensor_tensor(out=wrl, in0=wrf, in1=wrh, op=ALU.subtract), reads=[bwrf, bwrh], writes=[bwrl])
    H = AFa.get(NTG * D, D); bH = [Buf(f"H{i}") for i in range(NTG)]
    Cmb = AFa.get(NTG * 16, 16); bCmb = Buf("Cmb")
    gsr = ring(AFa, 512, 2, "gs2")
    rt = ring(AFa, 64, 2, "rt")
    st2 = ring(AFa, 8, 2, "st2")
    u2f = AFa.get(D); bu2f = Buf("u2f")
    slots = ring(ABa, 8192, 4, "eslot")
    wdeS = ABa.get(8192); bwdS = Buf("wdeS")
    actU = ABa.get(16 * GT, GT); bactU = Buf("u2T")
    u2h = wdeS[:, 0:D]; bu2h = Buf("u2h")
    u2l = wdeS[:, D:2 * D]; bu2l = Buf("u2l")
    loT = wdeS[:, 2 * D:3 * D].rearrange("p (a b) -> p a b", b=128); bloT = Buf("loT")
    hT = ABa.get(4 * GT, GT); bhT = Buf("hT")

    def subgroups(T):
        out, o = [], 0
        while o < T:
            n = min(512, T - o)
            out.append((o, n))
            o += n
        return out

    def rms_tile(ti, gain_buf):
        stt, bst = st2.next()
        c.op("pool", lambda e: e.memset(stt, 0.0), writes=[bst])
        c.op("act", lambda e: e.activation(out=u2f, in_=H[:, ti, :], func=AF.Square, accum_out=stt[:, 0:1]),
             reads=[bH[ti]], writes=[bu2f, bst])
        c.op("act", lambda e: e.activation(out=stt[:, 1:2], in_=stt[:, 0:1], func=AF.Ln, scale=1.0 / D, bias=EPS),
             reads=[bst], writes=[bst])
        c.op("act", lambda e: e.activation(out=stt[:, 1:2], in_=stt[:, 1:2], func=AF.Exp, scale=-0.5),
             reads=[bst], writes=[bst])
        c.op("dve", lambda e: e.scalar_tensor_tensor(out=u2f, in0=H[:, ti, :], scalar=stt[:, 1:2], in1=gv,
                                                     op0=ALU.mult, op1=ALU.mult),
             reads=[bH[ti], bst, bgv], writes=[bu2f])

    for t0 in range(0, NTOK, GT):
        T = min(GT, NTOK - t0)
        ntl = T // 128
        for ti in range(ntl):
            c.dma("sp", H[:, ti, :], hs[t0 + ti * 128:t0 + (ti + 1) * 128, :], reads=[b_hs], writes=[bH[ti]], owner=bH[ti])
        c.dma("sp", gv, g2.partition_broadcast(128), writes=[bgv], owner=bgv)
        for ti in range(ntl):
            rms_tile(ti, gv)
            c.op("act", lambda e: e.copy(out=u2h, in_=u2f), reads=[bu2f], writes=[bu2h])
            c.op("dve", lambda e: e.tensor_tensor(out=u2l, in0=u2f, in1=u2h, op=ALU.subtract),
                 reads=[bu2f, bu2h], writes=[bu2l])
            for j in range(16):
                c.op("pe", lambda e, j=j: e.transpose(out=pT[:, j, :], in_=u2h[:, j * 128:(j + 1) * 128], identity=IDN),
                     reads=[bu2h], writes=[bpT], sig=(j == 15))
            c.op("act", lambda e, ti=ti: e.copy(out=actU[:, :, ti * 128:(ti + 1) * 128], in_=pT[:, :, :]),
                 reads=[bpT], writes=[bactU])
            for j in range(16):
                c.op("pe", lambda e, j=j: e.transpose(out=pT[:, j, :], in_=u2l[:, j * 128:(j + 1) * 128], identity=IDN),
                     reads=[bu2l], writes=[bpT], sig=(j == 15))
            c.op("act", lambda e: e.copy(out=loT, in_=pT[:, :, :]), reads=[bpT], writes=[bloT])
            mm = []
            for k in range(16):
                mm.append((actU[:, k, ti * 128:(ti + 1) * 128], wrh[:, k, :]))
            for k in range(16):
                mm.append((loT[:, k, :], wrh[:, k, :]))
            for k in range(16):
                mm.append((actU[:, k, ti * 128:(ti + 1) * 128], wrl[:, k, :]))
            for i_, (l_, r_) in enumerate(mm):
                c.op("pe", lambda e, l_=l_, r_=r_, i_=i_: e.matmul(pB[:, 0:20], lhsT=l_, rhs=r_, start=(i_ == 0), stop=(i_ == 47)),
                     reads=[bactU, bloT, bwrh, bwrl], writes=[bpB], sig=(i_ == 47))
            R_, bR = rt.next()
            L = R_[:, 0:20]
            c.op("dve", lambda e: e.tensor_tensor(out=L, in0=pB[:, 0:20], in1=brc, op=ALU.add), reads=[bpB, bbr], writes=[bR])
            gmax, gsum, m1, m2 = R_[:, 20:21], R_[:, 21:22], R_[:, 22:23], R_[:, 23:24]
            oh, pen = R_[:, 24:28], R_[:, 28:32]
            elm, mk1 = R_[:, 32:48], R_[:, 48:64]
            R2, bR2 = rt.next()
            elm2, mk2, ge = R2[:, 0:16], R2[:, 16:32], R2[:, 32:36]
            w1_, w2_, e2 = R2[:, 36:37], R2[:, 37:38], R2[:, 38:39]
            rb = [bR, bR2]
            V = lambda fn: c.op("dve", fn, reads=rb, writes=rb)
            V(lambda e: e.tensor_reduce(out=gmax, in_=L[:, 0:4], op=ALU.max, axis=AX.X))
            V(lambda e: e.tensor_scalar(out=oh, in0=L[:, 0:4], scalar1=gmax, scalar2=None, op0=ALU.is_ge))
            V(lambda e: e.tensor_scalar(out=ge, in0=L[:, 0:4], scalar1=gmax, scalar2=None, op0=ALU.subtract))
            c.op("act", lambda e: e.activation(out=ge, in_=ge, func=AF.Exp), reads=rb, writes=rb)
            V(lambda e: e.tensor_reduce(out=gsum, in_=ge, op=ALU.add, axis=AX.X))
            V(lambda e: e.reciprocal(out=gsum, in_=gsum))
            V(lambda e: e.tensor_scalar(out=pen, in0=oh, scalar1=-1.0, scalar2=1e9, op0=ALU.add, op1=ALU.mult))
            for gi in range(4):
                V(lambda e, gi=gi: e.tensor_scalar(out=elm[:, gi * 4:gi * 4 + 4], in0=L[:, 4 + gi * 4:8 + gi * 4],
                                                   scalar1=pen[:, gi:gi + 1], scalar2=None, op0=ALU.add))
            V(lambda e: e.tensor_reduce(out=m1, in_=elm, op=ALU.max, axis=AX.X))
            V(lambda e: e.tensor_scalar(out=mk1, in0=elm, scalar1=m1, scalar2=None, op0=ALU.is_ge))
            V(lambda e: e.scalar_tensor_tensor(out=elm2, in0=mk1, scalar=-1e9, in1=elm, op0=ALU.mult, op1=ALU.add))
            V(lambda e: e.tensor_reduce(out=m2, in_=elm2, op=ALU.max, axis=AX.X))
            V(lambda e: e.tensor_scalar(out=mk2, in0=elm2, scalar1=m2, scalar2=None, op0=ALU.is_ge))
            V(lambda e: e.tensor_tensor(out=e2, in0=m2, in1=m1, op=ALU.subtract))
            c.op("act", lambda e: e.activation(out=e2, in_=e2, func=AF.Exp), reads=rb, writes=rb)
            V(lambda e: e.tensor_scalar(out=w1_, in0=e2, scalar1=1.0, scalar2=None, op0=ALU.add))
            V(lambda e: e.reciprocal(out=w1_, in_=w1_))
            V(lambda e: e.tensor_tensor(out=w1_, in0=w1_, in1=gsum, op=ALU.mult))
            V(lambda e: e.tensor_tensor(out=w2_, in0=w1_, in1=e2, op=ALU.mult))
            V(lambda e: e.tensor_scalar(out=mk1, in0=mk1, scalar1=w1_, scalar2=None, op0=ALU.mult))
            c.op("dve", lambda e, ti=ti: e.scalar_tensor_tensor(out=Cmb[:, ti, :], in0=mk2, scalar=w2_, in1=mk1,
                                                                op0=ALU.mult, op1=ALU.add), reads=rb, writes=rb + [bCmb])
        c.barrier()
        for ex in range(16):
            wge_, bwg = slots.next()
            wue_, bwu = slots.next()
            wde_, bwd = wdeS, bwdS
            wge = wge_.rearrange("p (a b) -> p a b", b=512)
            wue = wue_.rearrange("p (a b) -> p a b", b=512)
            wde = wde_.rearrange("p (a b) -> p a b", b=D)
            for k4 in range(4):
                c.dma("pool", wge[:, 4 * k4:4 * k4 + 4, :], wgate[ex, 512 * k4:512 * (k4 + 1), :].rearrange("(k p) n -> p k n", p=128),
                      writes=[bwg], owner=bwg)
            for k4 in range(4):
                c.dma("pool", wue[:, 4 * k4:4 * k4 + 4, :], wup[ex, 512 * k4:512 * (k4 + 1), :].rearrange("(k p) n -> p k n", p=128),
                      writes=[bwu], owner=bwu)
            c.dma("pool", wde, wdown[ex].rearrange("(k p) n -> p k n", p=128), writes=[bwd], owner=bwd)
            for (o, n) in subgroups(T):
                for fx in range(4):
                    for (ps, bps, w, bw) in ((pA, bpA, wge, bwg), (pB, bpB, wue, bwu)):
                        for k in range(16):
                            c.op("pe", lambda e, ps=ps, w=w, k=k, fx=fx: e.matmul(
                                ps[:, :n], lhsT=w[:, k, fx * 128:(fx + 1) * 128], rhs=actU[:, k, o:o + n],
                                start=(k == 0), stop=(k == 15)),
                                reads=[bw, bactU], writes=[bps], sig=(k == 15))
                    ga, bga = gsr.next()
                    c.op("act", lambda e: e.activation(out=ga[:, :n], in_=pA[:, :n], func=AF.Sigmoid), reads=[bpA], writes=[bga])
                    c.op("dve", lambda e: e.tensor_tensor(out=ga[:, :n], in0=ga[:, :n], in1=pA[:, :n], op=ALU.mult),
                         reads=[bga, bpA], writes=[bga])
                    c.op("dve", lambda e, fx=fx: e.tensor_tensor(out=hT[:, fx, o:o + n], in0=ga[:, :n], in1=pB[:, :n], op=ALU.mult),
                         reads=[bga, bpB], writes=[bhT])
            for ti in range(ntl):
                for oc in range(4):
                    (ps, bps) = ((pC, bpC), (p6, bp6))[(ti * 4 + oc) % 2]
                    for fx in range(4):
                        c.op("pe", lambda e, ps=ps, fx=fx, ti=ti, oc=oc: e.matmul(
                            ps[:, :], lhsT=hT[:, fx, ti * 128:(ti + 1) * 128], rhs=wde[:, fx, oc * 512:(oc + 1) * 512],
                            start=(fx == 0), stop=(fx == 3)),
                            reads=[bhT, bwd], writes=[bps], sig=(fx == 3))
                    c.op("dve", lambda e, ps=ps, ti=ti, oc=oc, ex=ex: e.scalar_tensor_tensor(
                        out=H[:, ti, oc * 512:(oc + 1) * 512], in0=ps[:, :], scalar=Cmb[:, ti, ex:ex + 1],
                        in1=H[:, ti, oc * 512:(oc + 1) * 512], op0=ALU.mult, op1=ALU.add),
                        reads=[bps, bCmb, bH[ti]], writes=[bH[ti]])
        c.barrier()
        c.dma("sp", gv, gf.partition_broadcast(128), writes=[bgv], owner=bgv)
        for ti in range(ntl):
            rms_tile(ti, gv)
            c.dma("sp", yo[t0 + ti * 128:t0 + (ti + 1) * 128, :], u2f, reads=[bu2f], owner=bu2f, is_output=True)
    c.finish()
    return c


def head_consts(h):
    lg = np.log1p(-np.float32(2.0) ** np.float32(-5.0 - h)).astype(np.float32)
    t = np.arange(128, dtype=np.float32)
    rel = t[None, :] - t[:, None]
    dmt = np.where(rel >= 0, np.exp(np.maximum(rel, 0) * lg), 0.0).astype(np.float32)
    gq = np.broadcast_to(np.exp((t + 1.0) * lg)[None, :], (128, 128)).astype(np.float32)
    cf = np.zeros((128, 262), np.float32)
    cf[:, 0:128] = dmt
    cf[:, 128:256] = gq
    for i, nt in enumerate((128, 64, 16)):
        v = np.zeros(128, np.float32)
        v[:nt] = np.exp((nt - 1.0 - t[:nt]) * lg)
        cf[:, 256 + i] = v
        cf[:, 259 + i] = np.exp(np.float32(nt) * lg)
    return cf


def bf_consts():
    cb = np.zeros((128, 512), np.float32)
    i = np.arange(128)
    cb[:, 0:128] = np.eye(128)
    cb[:, 128:256] = np.where(i[:, None] >= i[None, :], NEG, 0.0)
    cb[:, 256:384] = np.where(i[:, None] >= i[None, :], -1.0, 0.0)
    cb[:, 384:512] = -1.0
    return cb


def rope_tables(ntp):
    pos = np.concatenate([np.arange(ntp, dtype=np.float32) - NMETA, PAST + np.arange(DEC_T, dtype=np.float32)])
    inv = (1.0 / (np.float32(10000.0) ** (np.arange(0, 128, 2, dtype=np.float32) / np.float32(128)))).astype(np.float32)
    ang = (pos[:, None] * inv[None, :]).astype(np.float32)
    cs, sn = np.cos(ang).astype(np.float32), np.sin(ang).astype(np.float32)
    s = np.float32(128.0 ** -0.5)
    cc = np.concatenate([cs, cs, cs * s, cs * s], axis=1).astype(np.float32)
    ss = np.concatenate([-sn, sn, -sn * s, sn * s], axis=1).astype(np.float32)
    return np.ascontiguousarray(cc), np.ascontiguousarray(ss)


def wh_cols(h):
    def r(o, w):
        return np.arange(o + h * w, o + (h + 1) * w)
    return np.concatenate([r(1024, 128), r(2048, 128), r(0, 128), r(3072, 128), r(4096, 128),
                           r(5120, 256), r(7168, 256)])


def make_maps(inp, NXT=64, NH=8, NS=NSC, cores=range(8)):
    ntp = NMETA + NXT * 128
    NL = NXT // 4
    cc, ss = rope_tables(ntp)
    cb = bf_consts()
    cf = np.stack([head_consts(h) for h in range(NH)], 0)
    w_in = inp["w_in"][0]
    wh = np.stack([w_in[:, wh_cols(h)] for h in range(NH)], 0)
    c_ = np.ascontiguousarray
    shared = {
        "meta": c_(inp["meta"]), "g1": c_(inp["norm1_g"][0]), "g2": c_(inp["norm2_g"][0]), "gf": c_(inp["normf_g"]),
        "wh": wh, "cstf": cf, "cstb": cb, "ropec": c_(cc[:, 128:256]), "ropes": c_(ss[:, 128:256]),
        "ropesmc": c_(cc[ntp:ntp + DEC_T]), "ropesms": c_(ss[ntp:ntp + DEC_T]),
        "wg": c_(w_in[:, 9216:13312]), "wsbo": c_(inp["w_sb_o"][0][:NH * 128]), "wreto": c_(inp["w_ret_o"][0][:NH * 256]),
        "wout": c_(inp["w_out"][0]),
        "wr": c_(np.concatenate([inp["w_grp"][0], inp["w_exp"][0]], axis=1)),
        "br": c_(np.concatenate([inp["b_grp"][0], inp["b_exp"][0]], axis=0)),
        "wgate": c_(inp["w_gate"][0]), "wup": c_(inp["w_up"][0]), "wdown": c_(inp["w_down"][0]),
    }
    maps = []
    ii = np.arange(128)
    for cid in cores:
        b, j = cid // 4, cid % 4
        tiles = [4 * l + j for l in range(NL)]
        xp = inp["x_prompt"][b]
        xtok = np.concatenate([xp[t * 128:(t + 1) * 128] for t in tiles]
                              + [inp["x_sample"][NSC * cid + s] for s in range(NS)], axis=0)
        rows = np.concatenate([NMETA + t * 128 + ii for t in tiles]) if NL else np.zeros((0,), np.int64)
        mk = np.zeros((128, 4, 128), np.float32)
        for r in range(4):
            if r == j:
                mk[:, r, :] = np.where(ii[:, None] >= ii[None, :], NEG, 0.0)
            elif r > j:
                mk[:, r, :] = NEG
        sel = np.zeros((128, 4), np.float32)
        sel[:, j] = 1.0
        m = dict(shared)
        m.update({
            "xall": c_(xp[:NXT * 128]), "xtok": c_(xtok),
            "st": c_(inp["state_ret"][0, NSC * cid:NSC * cid + NS, :NH]),
            "ropeoc": c_(cc[rows]), "ropeos": c_(ss[rows]),
            "msk": c_(mk.reshape(128, 512)), "sel": sel,
            "ck": c_(inp["cache_sb_k"][0, NSC * cid:NSC * cid + NS, :, :NH].reshape(NS, PAST, NH * 128)),
            "cv": c_(inp["cache_sb_v"][0, NSC * cid:NSC * cid + NS, :, :NH].reshape(NS, PAST, NH * 128)),
        })
        maps.append(m)
    return maps


def kernel(**inputs):
    inp = {k: np.asarray(v) for k, v in inputs.items()}
    nc = bass.Bass("TRN2", target_bir_lowering=False)
    build(nc)
    maps = make_maps(inp)
    res = run_bass_kernel_spmd(nc, maps, core_ids=list(range(8)))
    R = res.results
    NL = 16
    y_p = np.zeros((2, SEQ, D), np.float32)
    y_s = np.zeros((DEC_B, DEC_T, D), np.float32)
    for cid in range(8):
        b, j = cid // 4, cid % 4
        yo = np.asarray(R[cid]["yo"])
        for l in range(NL):
            t = 4 * l + j
            y_p[b, t * 128:(t + 1) * 128] = yo[l * 128:(l + 1) * 128]
        for s in range(NSC):
            y_s[NSC * cid + s] = yo[NL * 128 + s * DEC_T:NL * 128 + (s + 1) * DEC_T]
    kp = np.stack([np.asarray(R[4 * b]["kp"]).reshape(TP, 8, 128) for b in range(2)], 0)[None]
    vp = np.stack([np.asarray(R[4 * b]["vp"]).reshape(TP, 8, 128) for b in range(2)], 0)[None]
    sp = np.stack([np.asarray(R[4 * b]["sp_o"]) for b in range(2)], 0)[None]
    ks = np.concatenate([np.asarray(R[c]["ks"]).reshape(NSC, DEC_T, 8, 128) for c in range(8)], 0)[None]
    vs = np.concatenate([np.asarray(R[c]["vs"]).reshape(NSC, DEC_T, 8, 128) for c in range(8)], 0)[None]
    ss = np.concatenate([np.asarray(R[c]["ss_o"]) for c in range(8)], 0)[None]
    f = lambda a: np.ascontiguousarray(a, dtype=np.float32)
    return (y_p, y_s, f(kp), f(vp), f(sp), f(ks), f(vs), f(ss))
```
